# Optimizing a Trainium2 kernel written in Bass

```python
import math
import jax, jax.numpy as jnp
from jax import lax
import numpy as np

D_MODEL = 1024
BATCH = 8
SEQ = 2048
DEPTH = 1

PLE_DIM = 256
D_FF = 2816
EPS = 1e-6
GLA_HEADS = 4
GLA_DK = 128
GLA_DV = 256
GLA_LOWRANK = 16
GLA_TAU = 16.0
GLA_CHUNK = 64
DSA_HEADS = 8
DSA_LATENT = 128
IDX_HEADS = 8
IDX_DIM = 64
TOPK_MAX = 256
Q_BLOCK = 128
REL_BUCKETS = 32
REL_MAX_DIST = 128

IN_SIZES = (
    GLA_HEADS * GLA_DK,
    GLA_HEADS * GLA_DK,
    GLA_HEADS * GLA_DV,
    GLA_HEADS * GLA_DV,
    GLA_LOWRANK,
    DSA_HEADS * DSA_LATENT,
    DSA_LATENT,
    IDX_HEADS * IDX_DIM,
    IDX_DIM,
    IDX_HEADS,
    2 * D_MODEL,
)
D_IN = sum(IN_SIZES)

kernel_name = "gla_dsa_parallel_hybrid_macaron"


def rms_norm(x, g):
    xf = x.astype(jnp.float32)
    y = xf * lax.rsqrt(jnp.mean(xf * xf, axis=-1, keepdims=True) + EPS)
    return (y * g.astype(jnp.float32)).astype(x.dtype)


def swiglu(x, w_in, w_out):
    gate, up = jnp.split(x @ w_in, 2, axis=-1)
    return (jax.nn.silu(gate) * up) @ w_out


def t5_bucket(dist):
    max_exact = REL_BUCKETS // 2
    d = jnp.maximum(dist, 0)
    ratio = jnp.maximum(d, 1).astype(jnp.float32) / max_exact
    large = max_exact + (jnp.log(ratio) / math.log(REL_MAX_DIST / max_exact)
                         * (REL_BUCKETS - max_exact)).astype(jnp.int32)
    large = jnp.minimum(large, REL_BUCKETS - 1)
    return jnp.where(d < max_exact, d, large)


def gla_chunked(q, k, v, log_a):
    B, H, S, dk = q.shape
    dv = v.shape[-1]
    C = GLA_CHUNK
    N = S // C

    def to_chunks(t):
        return jnp.moveaxis(t.reshape(B, H, N, C, t.shape[-1]), 2, 0)

    causal = jnp.tril(jnp.ones((C, C), dtype=bool))[:, :, None]

    def step(state, inp):
        qi, ki, vi, gi = inp
        b = jnp.cumsum(gi.astype(jnp.float32), axis=2)
        o_inter = jnp.einsum('bhtd,bhde->bhte', qi * jnp.exp(b), state)
        diff = b[:, :, :, None, :] - b[:, :, None, :, :]
        decay = jnp.exp(jnp.where(causal, diff, -jnp.inf))
        scores = jnp.einsum('bhtd,bhsd,bhtsd->bhts', qi, ki, decay)
        o_intra = jnp.einsum('bhts,bhse->bhte', scores, vi)
        b_last = b[:, :, -1:, :]
        k_dec = ki * jnp.exp(b_last - b)
        new_state = (jnp.exp(b_last[:, :, 0, :])[..., None] * state
                     + jnp.einsum('bhsd,bhse->bhde', k_dec, vi))
        return new_state, o_inter + o_intra

    state0 = jnp.zeros((B, H, dk, dv), jnp.float32)
    _, out = lax.scan(step, state0, (to_chunks(q), to_chunks(k), to_chunks(v), to_chunks(log_a)))
    return jnp.moveaxis(out, 0, 2).reshape(B, H, S, dv)


def dsa_attention(q, c_kv, q_idx, k_idx, w_idx, rel_bias):
    B, S, H, dc = q.shape
    top_k = min(TOPK_MAX, S // 4)
    nb = S // Q_BLOCK
    key_pos = jnp.arange(S, dtype=jnp.int32)
    bidx = jnp.arange(B)[:, None, None]

    def blocks(t):
        return jnp.moveaxis(t.reshape(B, nb, Q_BLOCK, *t.shape[2:]), 1, 0)

    def one_block(args):
        blk, qb, qib, wb = args
        q_pos = blk * Q_BLOCK + jnp.arange(Q_BLOCK, dtype=jnp.int32)
        dots = jnp.einsum('bthd,bsd->bths', qib, k_idx).astype(jnp.float32) * (IDX_DIM ** -0.5)
        w = wb.astype(jnp.float32) * (IDX_HEADS ** -0.5)
        score = jnp.einsum('bths,bth->bts', jax.nn.relu(dots), w)
        causal = key_pos[None, :] <= q_pos[:, None]
        score = jnp.where(causal[None], score, -jnp.inf)
        _, sel = lax.top_k(score, top_k)
        valid = sel <= q_pos[None, :, None]
        kv_sel = c_kv[bidx, sel]
        bias = rel_bias[t5_bucket(q_pos[None, :, None] - sel)]
        logits = (jnp.einsum('bthc,btkc->bthk', qb, kv_sel).astype(jnp.float32) * (dc ** -0.5)
                  + jnp.moveaxis(bias, -1, 2).astype(jnp.float32))
        logits = jnp.where(valid[:, :, None, :], logits, -jnp.inf)
        probs = jax.nn.softmax(logits, axis=-1)
        return jnp.einsum('bthk,btkc->bthc', probs.astype(kv_sel.dtype), kv_sel)

    out = lax.map(one_block, (jnp.arange(nb, dtype=jnp.int32), blocks(q), blocks(q_idx), blocks(w_idx)))
    return jnp.moveaxis(out, 0, 1).reshape(B, S, H, dc)


def hybrid_mixer(u, w_in, alpha_w, alpha_b, gla_norm_g, gla_w_out, ckv_g, dsa_w_out, rel_bias, w_out):
    B, S, _ = u.shape
    split_pts = np.cumsum(IN_SIZES)[:-1].tolist()
    (gq, gk, gv, gr, ga, dq, dkv, iq, ik, iw, gates) = jnp.split(u @ w_in, split_pts, axis=-1)

    def heads(t, n):
        return t.reshape(B, S, n, -1).transpose(0, 2, 1, 3)

    log_a = jax.nn.log_sigmoid((ga @ alpha_w + alpha_b).astype(jnp.float32)) / GLA_TAU
    o = gla_chunked(heads(gq, GLA_HEADS) * (GLA_DK ** -0.5), heads(gk, GLA_HEADS),
                    heads(gv, GLA_HEADS), heads(log_a, GLA_HEADS))
    o = rms_norm(o, gla_norm_g.reshape(GLA_HEADS, 1, GLA_DV)).astype(u.dtype)
    o = o.transpose(0, 2, 1, 3).reshape(B, S, GLA_HEADS * GLA_DV) * jax.nn.silu(gr)
    y_gla = o @ gla_w_out

    c_kv = rms_norm(dkv, ckv_g)
    od = dsa_attention(dq.reshape(B, S, DSA_HEADS, DSA_LATENT), c_kv,
                       iq.reshape(B, S, IDX_HEADS, IDX_DIM), ik, iw, rel_bias)
    y_dsa = od.reshape(B, S, DSA_HEADS * DSA_LATENT) @ dsa_w_out

    g_gla, g_dsa = jnp.split(jax.nn.sigmoid(gates), 2, axis=-1)
    return (g_gla * y_gla + g_dsa * y_dsa) @ w_out


def setup_inputs(seed: int = 0) -> dict:
    key = jax.random.key(seed)
    ks = jax.random.split(key, 24)
    L, D, F = DEPTH, D_MODEL, D_FF

    def dense(k, shape, fan_in):
        return jax.random.normal(k, shape, jnp.float32) * (fan_in ** -0.5)

    def gain(k, shape):
        return 1.0 + 0.05 * jax.random.normal(k, shape, jnp.float32)

    return {
        "x": jax.random.normal(ks[0], (BATCH, SEQ, D), jnp.float32),
        "p": jax.random.normal(ks[1], (DEPTH, BATCH, SEQ, PLE_DIM), jnp.float32),
        "ffn1_norm": gain(ks[2], (L, D)),
        "ffn1_w_in": dense(ks[3], (L, D, 2 * F), D),
        "ffn1_w_out": dense(ks[4], (L, F, D), F),
        "mix_norm": gain(ks[5], (L, D)),
        "mix_w_in": dense(ks[6], (L, D, D_IN), D),
        "gla_alpha_w": dense(ks[7], (L, GLA_LOWRANK, GLA_HEADS * GLA_DK), GLA_LOWRANK),
        "gla_alpha_b": 0.1 * jax.random.normal(ks[8], (L, GLA_HEADS * GLA_DK), jnp.float32),
        "gla_out_norm": gain(ks[9], (L, GLA_HEADS * GLA_DV)),
        "gla_w_out": dense(ks[10], (L, GLA_HEADS * GLA_DV, D), GLA_HEADS * GLA_DV),
        "ckv_norm": gain(ks[11], (L, DSA_LATENT)),
        "dsa_w_out": dense(ks[12], (L, DSA_HEADS * DSA_LATENT, D), DSA_HEADS * DSA_LATENT),
        "rel_bias": 0.5 * jax.random.normal(ks[13], (REL_BUCKETS, DSA_HEADS), jnp.float32),
        "mix_w_out": dense(ks[14], (L, D, D), D),
        "ffn2_norm": gain(ks[15], (L, D)),
        "ffn2_w_in": dense(ks[16], (L, D, 2 * F), D),
        "ffn2_w_out": dense(ks[17], (L, F, D), F),
        "ple_norm": gain(ks[18], (L, D)),
        "ple_w_gate": dense(ks[19], (L, D, D), D),
        "ple_w_proj": dense(ks[20], (L, PLE_DIM, D), PLE_DIM),
        "final_norm": gain(ks[21], (D,)),
    }


def reference(x, p, ffn1_norm, ffn1_w_in, ffn1_w_out, mix_norm, mix_w_in, gla_alpha_w, gla_alpha_b,
              gla_out_norm, gla_w_out, ckv_norm, dsa_w_out, rel_bias, mix_w_out, ffn2_norm, ffn2_w_in,
              ffn2_w_out, ple_norm, ple_w_gate, ple_w_proj, final_norm):
    h = x
    for i in range(DEPTH):
        h = h + 0.5 * swiglu(rms_norm(h, ffn1_norm[i]), ffn1_w_in[i], ffn1_w_out[i])
        h = h + hybrid_mixer(rms_norm(h, mix_norm[i]), mix_w_in[i], gla_alpha_w[i], gla_alpha_b[i],
                             gla_out_norm[i], gla_w_out[i], ckv_norm[i], dsa_w_out[i], rel_bias,
                             mix_w_out[i])
        h = h + 0.5 * swiglu(rms_norm(h, ffn2_norm[i]), ffn2_w_in[i], ffn2_w_out[i])
        hn = rms_norm(h, ple_norm[i])
        h = h + jax.nn.sigmoid(hn @ ple_w_gate[i]) * (p[i] @ ple_w_proj[i])
    return rms_norm(h, final_norm)
```

```python
import math
from contextlib import ExitStack
import numpy as np
import concourse.bass as bass
import concourse.mybir as mybir
from concourse.bass_utils import run_bass_kernel_spmd

F32 = mybir.dt.float32
BF16 = mybir.dt.bfloat16
U32 = mybir.dt.uint32
AF = mybir.ActivationFunctionType
ALU = mybir.AluOpType
AX = mybir.AxisListType

D = 1024
S = 2048
NT = 16
FF = 2816
NF = 22
EPS = 1e-6
OFF = dict(gq=0, gk=512, gv=1024, gr=2048, ga=3072, dq=3088, dkv=4112, iq=4240, ik=4752, iw=4816, gates=4824)
D_IN = 6872
NBIS = 16
ALL_DVE = False
NE = 3
NWARM = 8


class Buf:
    __slots__ = ("name", "w", "r")

    def __init__(self, name):
        self.name = name
        self.w = {}
        self.r = {}


class Op:
    __slots__ = ("stream", "eng", "fn", "deps", "sig", "val", "idx")


class Prog:
    CE = ("pe", "act", "dve", "pool")
    ALL = ("pe", "act", "dve", "pool", "sp")

    def __init__(self):
        self.q = {e: [] for e in self.ALL}
        self.last_dma = {}

    def add(self, eng, fn, reads=(), writes=(), key=None, extra=()):
        stream = eng if key is None else "dma:" + key
        op = Op()
        op.stream, op.eng, op.fn, op.sig, op.val = stream, eng, fn, False, None
        op.idx = len(self.q[eng])
        raw = set()
        oth = set(extra)
        for b in reads:
            raw.update(b.w.values())
        for b in writes:
            oth.update(b.w.values())
            oth.update(b.r.values())
        deps = []
        for d in raw | oth:
            if d is op:
                continue
            if key is None and d.stream == stream:
                if eng == "pe":
                    continue
                if d in raw and op.idx - d.idx <= 2:
                    deps.append(d)
                continue
            deps.append(d)
        for d in deps:
            d.sig = True
        op.deps = deps
        for b in reads:
            b.r[stream] = op
        for b in writes:
            b.r = {}
            b.w[stream] = op
        self.q[eng].append(op)
        if key is not None:
            self.last_dma[stream] = op
        return op

    def barrier(self, exclude=()):
        lasts = []
        for e in self.CE:
            for op in reversed(self.q[e]):
                if op.fn is not None and op.stream == e:
                    lasts.append(op)
                    break
        dl = [op for st, op in self.last_dma.items() if st not in exclude]
        for e in self.ALL:
            self.add(e, None, extra=[l for l in lasts if l.stream != e] + dl)

    def finalize(self):
        cnt = {}
        for e in self.ALL:
            for op in self.q[e]:
                if op.fn is None:
                    continue
                if op.stream.startswith("dma:"):
                    cnt[op.stream] = cnt.get(op.stream, 0) + 16
                    op.val = cnt[op.stream]
                elif op.sig:
                    cnt[op.stream] = cnt.get(op.stream, 0) + 1
                    op.val = cnt[op.stream]
        return sorted(cnt.keys())

    def emit(self, eng, e, sems):
        known = {}
        for op in self.q[eng]:
            need = {}
            for d in op.deps:
                assert d.val is not None
                if known.get(d.stream, 0) < d.val and need.get(d.stream, 0) < d.val:
                    need[d.stream] = d.val
            for st, v in need.items():
                e.wait_ge(sems[st], v)
                known[st] = v
            if op.fn is not None:
                ins = op.fn(e)
                if op.val is not None:
                    ins.then_inc(sems[op.stream], 16 if op.stream.startswith("dma:") else 1)


class Arena:
    def __init__(self, ap, n):
        self.ap, self.n, self.pos = ap, n, 0

    def reset(self):
        self.pos = 0

    def take(self, shape, dt):
        ne = int(np.prod(shape))
        nb = ne * (2 if dt == F32 or dt == U32 else 1)
        nb = (nb + 1) // 2 * 2
        assert self.pos + nb <= self.n, ("arena overflow", self.pos, nb, self.n)
        v = self.ap[:, self.pos:self.pos + nb]
        self.pos += nb
        if dt != BF16:
            v = v.bitcast(dt)
        if len(shape) == 2:
            v = v.rearrange("p (a b) -> p a b", a=shape[0])
        elif len(shape) == 3:
            v = v.rearrange("p (a b c) -> p a b c", a=shape[0], b=shape[1])
        return v


def t5_bucket_np(d):
    d = np.maximum(d, 0)
    ratio = (np.maximum(d, 1).astype(np.float32) / np.float32(16))
    large = 16 + (np.log(ratio).astype(np.float32) / np.float32(math.log(128 / 16)) * np.float32(16)).astype(np.int32)
    large = np.minimum(large, 31)
    return np.where(d < 16, d, large)


CF_TRIU, CF_TRIS, CF_NEGTRI, CF_POW, CF_MISC = 0, 128, 256, 384, 416
NCF = 432
CB_ONES, CB_ID, CB_CAUS = 0, 128, 256
NCB = 384


def make_consts():
    cf = np.zeros((128, NCF), np.float32)
    i = np.arange(128)
    cf[:, CF_TRIU:CF_TRIU + 128] = np.where(i[:, None] <= i[None, :], -1.0 / 16, 0.0)
    cf[:, CF_TRIS:CF_TRIS + 128] = np.where(i[:, None] > i[None, :], -1.0 / 16, 0.0)
    cf[:, CF_NEGTRI:CF_NEGTRI + 128] = np.where(i[None, :] <= i[:, None], 0.0, -1e30)
    for k in range(32):
        cf[:, CF_POW + k] = 2.0 ** (-(k + 1))
    cf[:, CF_MISC + 0] = EPS
    cf[:, CF_MISC + 1] = 1.0
    cf[:, CF_MISC + 2] = 0.0
    cb = np.zeros((128, NCB), np.float32)
    cb[:, CB_ONES:CB_ONES + 128] = 1.0
    cb[:, CB_ID:CB_ID + 128] = np.eye(128)
    cb[:, CB_CAUS:CB_CAUS + 128] = np.where(i[:, None] <= i[None, :], 1.0, 0.0)
    return cf, cb


V_FFN1, V_MIX, V_FFN2, V_PLE, V_FIN, V_CKV, V_GLA = 0, 8, 16, 24, 32, 40, 41
NV = 52


def build_nc(stages=("ffn1", "gla", "dsa", "ffn2", "ple")):
    nc = bass.Bass("TRN2", target_bir_lowering=False)

    def din(name, shape):
        return nc.dram_tensor(name, list(shape), F32, kind="ExternalInput").ap()

    xT = din("xT", [D, S])
    pT = din("pT", [256, S])
    vecs = din("vecs", [128, NV])
    cstf = din("cstf", [128, NCF])
    cstb = din("cstb", [128, NCB])
    w_f1i = din("ffn1_w_in", [D, 2 * FF])
    w_f1o = din("ffn1_w_out", [FF, D])
    w_f2i = din("ffn2_w_in", [D, 2 * FF])
    w_f2o = din("ffn2_w_out", [FF, D])
    w_min = din("mix_w_in", [D, D_IN])
    alpha = din("alpha_aug", [17, 512])
    w_glo = din("gla_w_out", [D, D])
    w_dso = din("dsa_w_out", [D, D])
    relb = din("rel_bias", [32, 8])
    relbT = din("rel_biasT", [8, 32])
    w_mo = din("mix_w_out", [D, D])
    w_pg = din("ple_w_gate", [D, D])
    w_pp = din("ple_w_proj", [256, D])
    yT = nc.dram_tensor("yT", [D, S], F32, kind="ExternalOutput").ap()
    scrA = nc.dram_tensor("scrA", [8, 384], F32, kind="Internal").ap()

    P = Prog()
    es = ExitStack()
    ARN = 53700
    H = es.enter_context(nc.sbuf_tensor("H", [128, 8, S], F32))
    XN = es.enter_context(nc.sbuf_tensor("XN", [128, 8, S], BF16))
    CF = es.enter_context(nc.sbuf_tensor("CF", [128, NCF], F32))
    CB = es.enter_context(nc.sbuf_tensor("CB", [128, NCB], BF16))
    VEC = es.enter_context(nc.sbuf_tensor("VEC", [128, NV], F32))
    ARt = es.enter_context(nc.sbuf_tensor("AR", [128, ARN], BF16))
    PS = [es.enter_context(nc.psum_tensor("ps%d" % i, [128, 512], F32)) for i in range(7)]
    PSB = es.enter_context(nc.psum_tensor("psb", [128, 1024], BF16))
    AR = Arena(ARt, ARN)

    bH = [[Buf("H%d_%d" % (k, tg)) for tg in range(4)] for k in range(8)]
    bXN = [Buf("XN%d" % tg) for tg in range(4)]
    bPS = [Buf("PS%d" % i) for i in range(7)]
    bPSB = Buf("PSB")
    bCF, bCB, bVEC = Buf("CF"), Buf("CB"), Buf("VEC")

    ones = CB[:, CB_ONES:CB_ONES + 128]
    ident = CB[:, CB_ID:CB_ID + 128]
    caus = CB[:, CB_CAUS:CB_CAUS + 128]
    eps_col = CF[:, CF_MISC:CF_MISC + 1]

    def tgs(tg):
        return slice(tg * 512, (tg + 1) * 512)

    def DMA(q, out, in_, reads, writes, key):
        return P.add(q, lambda e: e.dma_start(out=out, in_=in_), reads, writes, key=key)

    def MMG(out, pairs, reads, writes):
        def fn(e):
            n = len(pairs)
            ins = None
            for i, (l, r) in enumerate(pairs):
                ins = e.matmul(out, l, r, start=(i == 0), stop=(i == n - 1))
            return ins
        return P.add("pe", fn, reads, writes)

    def MM1(out, l, r, start, stop, reads, writes):
        return P.add("pe", lambda e: e.matmul(out, l, r, start=start, stop=stop), reads, writes)

    def TR(out, in_, reads, writes):
        return P.add("pe", lambda e: e.transpose(out, in_, ident), reads, writes)

    def ACT(out, in_, func, reads, writes, bias=None, scale=None, accum=None):
        kw = {}
        if bias is not None:
            kw["bias"] = bias
        if scale is not None:
            kw["scale"] = scale
        if accum is not None:
            kw["accum_out"] = accum
        return P.add("act", lambda e: e.activation(out=out, in_=in_, func=func, **kw), reads, writes)

    def TT(eng, out, a, b, op, reads, writes):
        return P.add(eng, lambda e: e.tensor_tensor(out=out, in0=a, in1=b, op=op), reads, writes)

    def STT(out, a, sc, b, op0, op1, reads, writes):
        return P.add("dve", lambda e: e.scalar_tensor_tensor(out=out, in0=a, scalar=sc, in1=b, op0=op0, op1=op1),
                     reads, writes)

    def TS(eng, out, a, s1, s2, op0, op1, reads, writes, accum=None):
        kw = {}
        if accum is not None:
            kw["accum_out"] = accum
        if op1 is None:
            return P.add(eng, lambda e: e.tensor_scalar(out=out, in0=a, scalar1=s1, scalar2=None, op0=op0, **kw),
                         reads, writes)
        return P.add(eng, lambda e: e.tensor_scalar(out=out, in0=a, scalar1=s1, scalar2=s2, op0=op0, op1=op1, **kw),
                     reads, writes)

    def CP(eng, out, in_, reads, writes):
        if eng == "act":
            return P.add("act", lambda e: e.copy(out=out, in_=in_), reads, writes)
        return P.add(eng, lambda e: e.tensor_copy(out=out, in_=in_), reads, writes)

    def MEMSET(eng, out, val, writes):
        return P.add(eng, lambda e: e.memset(out, val), (), writes)

    def wview(w, c0, n):
        return w[:, c0:c0 + n].rearrange("(k p) n -> p k n", p=128)

    DMA("sp", H[:, :, :], xT.rearrange("(k p) t -> p k t", p=128), (), [b for r in bH for b in r], "x")
    DMA("sp", CF[:, :], cstf, (), [bCF], "cf")
    DMA("sp", VEC[:, :], vecs, (), [bVEC], "vec")
    DMA("pool", CB[:, :], cstb, (), [bCB], "cb")

    G = es.enter_context(nc.sbuf_tensor("G", [128, 8, 256], BF16))
    C31 = es.enter_context(nc.sbuf_tensor("C31", [128, 8], F32))
    NC31 = es.enter_context(nc.sbuf_tensor("NC31", [128, 8], F32))
    bG, bC31 = Buf("G"), Buf("C31")
    top = ARN - 4096 - 64 - 768
    TB = ARt[:, top:top + 4096].bitcast(F32).rearrange("p (a b) -> p a b", a=8)
    RBT = ARt[:, top + 4096:top + 4160].bitcast(F32)
    ASB = ARt[:, top + 4160:top + 4928].bitcast(F32)
    bTB, bRBT, bASB, bscr = Buf("TB"), Buf("RBT"), Buf("ASB"), Buf("scrA")
    kk_ = np.arange(384)
    bk_ = t5_bucket_np(kk_ - 127)
    DMA("sp", RBT[0:8, :], relbT, (), [bRBT], "rbt")
    DMA("sp", C31[:, :], relb[31:32, :].to_broadcast([128, 8]), (), [bC31], "c31")
    for b in range(32):
        idx = np.nonzero(bk_ == b)[0]
        if len(idx) == 0:
            continue
        k0, k1 = int(idx[0]), int(idx[-1]) + 1
        assert np.all(bk_[k0:k1] == b)
        CP("dve", ASB[0:8, k0:k1], RBT[0:8, b:b + 1].to_broadcast([8, k1 - k0]), [bRBT], [bASB])
    DMA("sp", scrA[:, :], ASB[0:8, :], [bASB], [bscr], "scr")
    for ss in range(128):
        DMA("sp", TB[ss:ss + 1, :, :], scrA[:, 127 - ss:127 - ss + 256].rearrange("(a h) x -> a h x", a=1), [bscr], [bTB], "tb")
    TS("dve", NC31[:, :], C31[:, :], -1.0, None, ALU.mult, None, [bC31], [bC31])

    def build_G():
        for h in range(8):
            ACT(G[:, h, :], TB[:, h, :], AF.Exp, [bTB, bC31], [bG], bias=NC31[:, h:h + 1], scale=1.0)

    def rmsnorm(gcol, out_fn, out_bufs_fn, after=None):
        SQ = AR.take([8, 512], BF16)
        RS = AR.take([512], F32)
        bSQ, bRS = Buf("SQ"), Buf("RS")
        for tg in range(4):
            hb = [bH[k][tg] for k in range(8)]
            ACT(SQ, H[:, :, tgs(tg)], AF.Square, hb, [bSQ])
            ps, bps = PS[6], bPS[6]
            MMG(ps[:, :], [(ones, SQ[:, k, :]) for k in range(8)], [bSQ, bCB], [bps])
            ACT(RS, ps[:, :], AF.Ln, [bps, bCF], [bRS], bias=eps_col, scale=1.0 / D)
            ACT(RS, RS, AF.Exp, [bRS], [bRS], scale=-0.5)
            for k in range(8):
                STT(out_fn(k, tg), H[:, k, tgs(tg)], VEC[:, gcol + k:gcol + k + 1], RS, ALU.mult, ALU.mult,
                    [bH[k][tg], bRS, bVEC], out_bufs_fn(tg))
            if after is not None:
                after(tg)

    def norm_to_xn(gcol):
        rmsnorm(gcol, lambda k, tg: XN[:, k, tgs(tg)], lambda tg: [bXN[tg]])

    def ffn(w_in, w_out, gcol, tag, prefetch=None):
        AR.reset()
        norm_to_xn(gcol)
        A = AR.take([6, S], BF16)
        W1 = [AR.take([8, 256], BF16) for _ in range(3)]
        W2 = [AR.take([6, D], BF16) for _ in range(2)]
        SG = [AR.take([512], BF16) for _ in range(2)]
        bA = [[Buf("A%d_%d" % (f, tg)) for tg in range(4)] for f in range(6)]
        bW1 = [Buf("W1_%d" % i) for i in range(3)]
        bW2 = [Buf("W2_%d" % i) for i in range(2)]
        bSG = [Buf("SG0"), Buf("SG1")]
        groups = [(0, 6), (6, 12), (12, 17), (17, 22)]
        c1 = 0
        c2 = 0
        for gi, (f0, f1) in enumerate(groups):
            nfg = f1 - f0
            w2 = W2[gi % 2]
            if prefetch is not None and gi == 1:
                prefetch()
            for fi in range(f0, f1):
                s = fi % 3
                DMA("pool", W1[s][:, :, 0:128], wview(w_in, fi * 128, 128), (), [bW1[s]], "w1_%d" % s)
                DMA("pool", W1[s][:, :, 128:256], wview(w_in, FF + fi * 128, 128), (), [bW1[s]], "w1_%d" % s)
                if fi == f0:
                    DMA("pool", w2[:, 0:nfg, :], w_out[f0 * 128:f1 * 128, :].rearrange("(f p) n -> p f n", p=128),
                        (), [bW2[gi % 2]], "w2_%d" % (gi % 2))
                for tg in range(4):
                    pg, pu = PS[(2 * c1) % 4], PS[(2 * c1 + 1) % 4]
                    bpg, bpu = bPS[(2 * c1) % 4], bPS[(2 * c1 + 1) % 4]
                    sg, bsg = SG[c1 % 2], bSG[c1 % 2]
                    c1 += 1
                    MMG(pg[:, :], [(W1[s][:, k, 0:128], XN[:, k, tgs(tg)]) for k in range(8)], [bW1[s], bXN[tg]], [bpg])
                    MMG(pu[:, :], [(W1[s][:, k, 128:256], XN[:, k, tgs(tg)]) for k in range(8)], [bW1[s], bXN[tg]], [bpu])
                    ACT(sg, pg[:, :], AF.Silu, [bpg], [bsg])
                    TT("dve", A[:, fi - f0, tgs(tg)], sg, pu[:, :], ALU.mult, [bsg, bpu], [bA[fi - f0][tg]])
            for dc in range(8):
                for tg in range(4):
                    po, bpo = PS[4 + c2 % 2], bPS[4 + c2 % 2]
                    c2 += 1
                    MMG(po[:, :], [(w2[:, f, dc * 128:(dc + 1) * 128], A[:, f, tgs(tg)]) for f in range(nfg)],
                        [bW2[gi % 2]] + [bA[f][tg] for f in range(nfg)], [bpo])
                    STT(H[:, dc, tgs(tg)], po[:, :], 0.5, H[:, dc, tgs(tg)], ALU.mult, ALU.add,
                        [bpo, bH[dc][tg]], [bH[dc][tg]])
        P.barrier()

    class BranchBufs:
        def __init__(self, w_branch, gate_col0, tag, stream_wo=False):
            self.WB = AR.take([8, D], BF16)
            self.WG = AR.take([8, D], BF16)
            self.stream_wo = stream_wo
            if stream_wo:
                self.WOc = [AR.take([8, 128], BF16) for _ in range(2)]
                self.bWOc = [Buf("WOc0"), Buf("WOc1")]
            else:
                self.WO = AR.take([8, D], BF16)
            self.M = AR.take([8, 512], BF16)
            self.SG = [AR.take([512], BF16) for _ in range(2)]
            self.bWB, self.bWG, self.bWO = Buf("WB"), Buf("WG"), Buf("WO")
            self.bM = Buf("M")
            self.bSG = [Buf("bSG0"), Buf("bSG1")]
            self.c = 0
            self.args = (w_branch, gate_col0, tag)

        def load(self):
            w_branch, gate_col0, tag = self.args
            DMA("pool", self.WB[:, :, :], wview(w_branch, 0, D), (), [self.bWB], "bwb" + tag)
            DMA("pool", self.WG[:, :, :], wview(w_min, gate_col0, D), (), [self.bWG], "bwg" + tag)
            if not self.stream_wo:
                DMA("pool", self.WO[:, :, :], wview(w_mo, 0, D), (), [self.bWO], "bwo" + tag)

    def branch_out_tg(bb, OTv, bOTv, tg):
        for dc in range(8):
            c = bb.c
            bb.c += 1
            s = c % 2
            dcs = slice(dc * 128, (dc + 1) * 128)
            py, bpy = PS[(2 * c) % 4], bPS[(2 * c) % 4]
            pg, bpg = PS[(2 * c + 1) % 4], bPS[(2 * c + 1) % 4]
            MMG(py[:, :], [(bb.WB[:, k, dcs], OTv[:, k, :]) for k in range(8)], [bb.bWB, bOTv], [bpy])
            MMG(pg[:, :], [(bb.WG[:, k, dcs], XN[:, k, tgs(tg)]) for k in range(8)], [bb.bWG, bXN[tg]], [bpg])
            ACT(bb.SG[s], pg[:, :], AF.Sigmoid, [bpg], [bb.bSG[s]])
            TT("dve", bb.M[:, dc, :], bb.SG[s], py[:, :], ALU.mult, [bb.bSG[s], bpy], [bb.bM])
        for dc in range(8):
            c = bb.c
            bb.c += 1
            s = c % 2
            dcs = slice(dc * 128, (dc + 1) * 128)
            po, bpo = PS[4 + s], bPS[4 + s]
            if bb.stream_wo:
                DMA("pool", bb.WOc[s][:, :, :], wview(w_mo, dc * 128, 128), (), [bb.bWOc[s]], "bwoc%d" % s)
                MMG(po[:, :], [(bb.WOc[s][:, k, :], bb.M[:, k, :]) for k in range(8)], [bb.bWOc[s], bb.bM], [bpo])
            else:
                MMG(po[:, :], [(bb.WO[:, k, dcs], bb.M[:, k, :]) for k in range(8)], [bb.bWO, bb.bM], [bpo])
            TT("dve", H[:, dc, tgs(tg)], po[:, :], H[:, dc, tgs(tg)], ALU.add, [bpo, bH[dc][tg]], [bH[dc][tg]])

    def gla():
        AR.reset()
        OT = AR.take([8, S], BF16)
        bOT = [Buf("OT%d" % tg) for tg in range(4)]
        mark = AR.pos
        GAT = AR.take([S], BF16)
        ALP = AR.take([512], BF16)
        WA = AR.take([8, 16], BF16)
        bGAT, bALP, bWA = Buf("GAT"), Buf("ALP"), Buf("WA")
        DMA("pool", ALP[0:17, :], alpha, (), [bALP], "alp")
        DMA("pool", WA[:, :, :], wview(w_min, OFF["ga"], 16), (), [bWA], "wa")
        MEMSET("dve", GAT[0:17, :], 1.0, [bGAT])
        for tg in range(4):
            ps, bps = PS[tg % 2], bPS[tg % 2]
            MMG(ps[0:16, :], [(WA[:, k, :], XN[:, k, tgs(tg)]) for k in range(8)], [bWA, bXN[tg]], [bps])
            CP("dve", GAT[0:16, tgs(tg)], ps[0:16, :], [bps], [bGAT])
        Wq = AR.take([8, 128], BF16)
        Wk = AR.take([8, 128], BF16)
        Wv = AR.take([8, 256], BF16)
        Wr = AR.take([8, 256], BF16)
        bWh = Buf("Wh")
        EB = [AR.take([512], BF16) for _ in range(2)]
        ENB = [AR.take([512], BF16) for _ in range(2)]
        EREV = [AR.take([4, 128], BF16) for _ in range(2)]
        QB = [AR.take([512], BF16) for _ in range(2)]
        KB = [AR.take([512], BF16) for _ in range(2)]
        KD = [AR.take([4, 128], BF16) for _ in range(2)]
        V = [AR.take([4, 256], BF16) for _ in range(2)]
        GR = [AR.take([4, 256], BF16) for _ in range(2)]
        EBL = AR.take([NT], F32)
        bEB = [Buf("EB0"), Buf("EB1")]
        bEREV = [Buf("EREV0"), Buf("EREV1")]
        bQB = [Buf("QB0"), Buf("QB1")]
        bKB = [Buf("KB0"), Buf("KB1")]
        bKD = [Buf("KD0"), Buf("KD1")]
        bV = [Buf("V0"), Buf("V1")]
        bGR = [Buf("GR0"), Buf("GR1")]
        bEBL = Buf("EBL")
        TE = [AR.take([128], F32) for _ in range(2)]
        LT = [AR.take([128], F32) for _ in range(2)]
        bTE = [Buf("TE0"), Buf("TE1")]
        bLT = [Buf("LT0"), Buf("LT1")]
        PTm = [AR.take([128], BF16) for _ in range(2)]
        bPT = [Buf("PT0"), Buf("PT1")]
        ST = AR.take([256], F32)
        SB = [AR.take([256], BF16) for _ in range(2)]
        bST = Buf("ST")
        bSB = [Buf("SB0"), Buf("SB1")]
        JK = AR.take([256], BF16)
        bJK = Buf("JK")
        SS = [AR.take([4], F32) for _ in range(2)]
        bSS = [Buf("SS0"), Buf("SS1")]
        OTM = [AR.take([256], BF16) for _ in range(2)]
        bOTM = [Buf("OTM0"), Buf("OTM1")]
        triu = CF[:, CF_TRIU:CF_TRIU + 128]
        tris = CF[:, CF_TRIS:CF_TRIS + 128]
        one_col = CF[:, CF_MISC + 1:CF_MISC + 2]

        def prep(h, tg, par):
            if tg == 0:
                DMA("pool", Wq[:, :, :], wview(w_min, OFF["gq"] + h * 128, 128), (), [bWh], "wh")
                DMA("pool", Wk[:, :, :], wview(w_min, OFF["gk"] + h * 128, 128), (), [bWh], "wh")
                DMA("pool", Wv[:, :, :], wview(w_min, OFF["gv"] + h * 256, 256), (), [bWh], "wh")
                DMA("pool", Wr[:, :, :], wview(w_min, OFF["gr"] + h * 256, 256), (), [bWh], "wh")
            for ci in range(4):
                c = 4 * tg + ci
                cs = slice(c * 128, (c + 1) * 128)
                ls = slice(ci * 128, (ci + 1) * 128)
                u = c % 2
                px, bpx = PS[u], bPS[u]
                MM1(px[:, 0:128], GAT[0:17, cs], ALP[0:17, h * 128:(h + 1) * 128], True, True, [bGAT, bALP], [bpx])
                ACT(TE[u], px[:, 0:128], AF.Exp, [bpx], [bTE[u]], scale=-1.0)
                ACT(LT[u], TE[u], AF.Ln, [bTE[u], bCF], [bLT[u]], bias=one_col, scale=1.0)
                pb, bpb = PS[2 + u], bPS[2 + u]
                MM1(pb[:, 0:128], LT[u], triu, True, True, [bLT[u], bCF], [bpb])
                MM1(pb[:, 128:256], tris, LT[u], True, True, [bLT[u], bCF], [bpb])
                ACT(EB[par][:, ls], pb[:, 0:128], AF.Exp, [bpb], [bEB[par]])
                ACT(ENB[par][:, ls], pb[:, 0:128], AF.Exp, [bpb], [bEB[par]], scale=-1.0)
                ACT(EREV[par][:, ci, :], pb[:, 128:256], AF.Exp, [bpb], [bEREV[par]])
                ACT(EBL[:, c:c + 1], pb[:, 127:128], AF.Exp, [bpb], [bEBL])
            pq, bpq = PS[4], bPS[4]
            pk, bpk = PS[5], bPS[5]
            MMG(pq[:, :], [(Wq[:, k, :], XN[:, k, tgs(tg)]) for k in range(8)], [bWh, bXN[tg]], [bpq])
            STT(QB[par], pq[:, :], 128.0 ** -0.5, EB[par], ALU.mult, ALU.mult, [bpq, bEB[par]], [bQB[par]])
            MMG(pk[:, :], [(Wk[:, k, :], XN[:, k, tgs(tg)]) for k in range(8)], [bWh, bXN[tg]], [bpk])
            TT("dve", KB[par], pk[:, :], ENB[par], ALU.mult, [bpk, bEB[par]], [bKB[par]])
            for ci in range(4):
                c = 4 * tg + ci
                cs = slice(c * 128, (c + 1) * 128)
                u = c % 2
                pk2, bpk2 = PS[u], bPS[u]
                MMG(pk2[:, 0:128], [(XN[:, k, cs], Wk[:, k, :]) for k in range(8)], [bWh, bXN[tg]], [bpk2])
                TT("dve", KD[par][:, ci, :], pk2[:, 0:128], EREV[par][:, ci, :], ALU.mult, [bpk2, bEREV[par]], [bKD[par]])
                pv, bpv = PS[2 + u], bPS[2 + u]
                MMG(pv[:, 0:256], [(XN[:, k, cs], Wv[:, k, :]) for k in range(8)], [bWh, bXN[tg]], [bpv])
                CP("act", V[par][:, ci, :], pv[:, 0:256], [bpv], [bV[par]])
                pr, bpr = PS[4 + u], bPS[4 + u]
                MMG(pr[:, 0:256], [(XN[:, k, cs], Wr[:, k, :]) for k in range(8)], [bWh, bXN[tg]], [bpr])
                ACT(GR[par][:, ci, :], pr[:, 0:256], AF.Silu, [bpr], [bGR[par]])

        def rec(h, tg, par):
            def cidx(ci):
                c = 4 * tg + ci
                return c, slice(c * 128, (c + 1) * 128), slice(ci * 128, (ci + 1) * 128), c % 2

            def stA(ci):
                c, cs, ls, u = cidx(ci)
                psc, bpsc = PS[u], bPS[u]
                MM1(psc[:, 128:256], KB[par][:, ls], QB[par][:, ls], True, True, [bKB[par], bQB[par]], [bpsc])
                TT("dve", PTm[u], psc[:, 128:256], caus, ALU.mult, [bpsc, bCB], [bPT[u]])

            def stB(ci):
                c, cs, ls, u = cidx(ci)
                if c < NT - 1:
                    pd, bpd = PS[4 + u], bPS[4 + u]
                    MM1(pd[:, 256:512], KD[par][:, ci, :], V[par][:, ci, :], True, True, [bKD[par], bV[par]], [bpd])
                    if c == 0:
                        CP("dve", ST, pd[:, 256:512], [bpd], [bST])
                    else:
                        STT(ST, ST, EBL[:, c:c + 1], pd[:, 256:512], ALU.mult, ALU.add, [bST, bEBL, bpd], [bST])
                    CP("act", SB[c % 2], ST, [bST], [bSB[c % 2]])

            def stC(ci):
                c, cs, ls, u = cidx(ci)
                po, bpo = PS[2 + u], bPS[2 + u]
                if c == 0:
                    MM1(po[:, 256:512], PTm[u], V[par][:, ci, :], True, True, [bPT[u], bV[par]], [bpo])
                else:
                    sp_ = (c - 1) % 2
                    MM1(po[:, 256:512], PTm[u], V[par][:, ci, :], True, False, [bPT[u], bV[par]], [bpo])
                    MM1(po[:, 256:512], QB[par][:, ls], SB[sp_], False, True, [bQB[par], bSB[sp_]], [bpo])
                ss, bss = SS[u], bSS[u]
                ACT(JK, po[:, 256:512], AF.Square, [bpo], [bJK, bss], accum=ss[:, 0:1])
                ACT(ss[:, 1:2], ss[:, 0:1], AF.Ln, [bss, bCF], [bss], bias=eps_col, scale=1.0 / 256)
                ACT(ss[:, 2:3], ss[:, 1:2], AF.Exp, [bss], [bss], scale=-0.5)
                STT(OTM[u], po[:, 256:512], ss[:, 2:3], GR[par][:, ci, :], ALU.mult, ALU.mult, [bpo, bss, bGR[par]], [bOTM[u]])

            def stD(ci):
                c, cs, ls, u = cidx(ci)
                TR(PSB[:, 0:128], OTM[u][:, 0:128], [bOTM[u], bCB], [bPSB])
                TR(PSB[:, 128:256], OTM[u][:, 128:256], [bOTM[u], bCB], [bPSB])
                for a_ in range(2):
                    TS("dve", OT[:, 2 * h + a_, cs], PSB[:, a_ * 128:(a_ + 1) * 128], VEC[:, V_GLA + 2 * h + a_:V_GLA + 2 * h + a_ + 1],
                       None, ALU.mult, None, [bPSB, bVEC], [bOT[tg]])

            for step in range(6):
                if step < 4:
                    stA(step)
                if 1 <= step <= 4:
                    stC(step - 1)
                if step < 4:
                    stB(step)
                if 2 <= step <= 5:
                    stD(step - 2)

        items = [(h, tg) for h in range(4) for tg in range(4)]
        prep(items[0][0], items[0][1], 0)
        for n, (h, tg) in enumerate(items):
            if n + 1 < len(items):
                prep(items[n + 1][0], items[n + 1][1], (n + 1) % 2)
            rec(h, tg, n % 2)
        P.barrier()
        AR.pos = mark
        bb = BranchBufs(w_glo, OFF["gates"], "g")
        bb.load()
        for tg in range(4):
            branch_out_tg(bb, OT[:, :, tgs(tg)], bOT[tg], tg)
        P.barrier()

    def dsa():
        AR.reset()
        CKT = AR.take([S], BF16)
        CKA = AR.take([NT, 132], BF16)
        IKT = AR.take([S], BF16)
        WS = AR.take([NT, 8], F32)
        MT = AR.take([NT, 512], BF16)
        bMT = [Buf("MT%d" % j) for j in range(NT)]
        bCKT, bCKA, bIKT, bWS = Buf("CKT"), Buf("CKA"), Buf("IKT"), Buf("WS")
        mark2 = AR.pos
        WKV = AR.take([8, 128], BF16)
        WIK = AR.take([8, 128], BF16)
        WIW = AR.take([8, 8], BF16)
        bWsh = Buf("Wsh")
        DMA("pool", WKV[:, :, :], wview(w_min, OFF["dkv"], 128), (), [bWsh], "wsh")
        DMA("pool", WIK[:, :, 0:64], wview(w_min, OFF["ik"], 64), (), [bWsh], "wsh")
        DMA("pool", WIK[:, :, 64:128], wview(w_min, OFF["ik"], 64), (), [bWsh], "wsh")
        DMA("pool", WIW[:, :, :], wview(w_min, OFF["iw"], 8), (), [bWsh], "wsh")
        SQ = AR.take([512], BF16)
        RS = AR.take([512], F32)
        bSQ, bRS = Buf("dSQ"), Buf("dRS")
        MEMSET("pool", CKA[:, :, 128:132], 1.0, [bCKA])
        for tg in range(4):
            pc, bpc = PS[tg % 2], bPS[tg % 2]
            MMG(pc[:, :], [(WKV[:, k, :], XN[:, k, tgs(tg)]) for k in range(8)], [bWsh, bXN[tg]], [bpc])
            ACT(SQ, pc[:, :], AF.Square, [bpc], [bSQ])
            pn, bpn = PS[2], bPS[2]
            MM1(pn[:, :], ones, SQ, True, True, [bSQ, bCB], [bpn])
            ACT(RS, pn[:, :], AF.Ln, [bpn, bCF], [bRS], bias=eps_col, scale=1.0 / 128)
            ACT(RS, RS, AF.Exp, [bRS], [bRS], scale=-0.5)
            STT(CKT[:, tgs(tg)], pc[:, :], VEC[:, V_CKV:V_CKV + 1], RS, ALU.mult, ALU.mult, [bpc, bRS, bVEC], [bCKT])
            pi, bpi = PS[3], bPS[3]
            MMG(pi[:, :], [(WIK[:, k, :], XN[:, k, tgs(tg)]) for k in range(8)], [bWsh, bXN[tg]], [bpi])
            CP("act", IKT[:, tgs(tg)], pi[:, :], [bpi], [bIKT])
            for ti in range(4):
                c = tg * 4 + ti
                cs = slice(c * 128, (c + 1) * 128)
                TR(PSB[:, ti * 128:(ti + 1) * 128], CKT[:, cs], [bCKT, bCB], [bPSB])
            CP("dve", CKA[:, 4 * tg:4 * tg + 4, 0:128], PSB[:, 0:512].rearrange("p (a b) -> p a b", a=4), [bPSB], [bCKA])
            pw, bpw = PS[4], bPS[4]
            for ti in range(4):
                c = tg * 4 + ti
                cs = slice(c * 128, (c + 1) * 128)
                MMG(pw[:, ti * 8:ti * 8 + 8], [(XN[:, k, cs], WIW[:, k, :]) for k in range(8)], [bWsh, bXN[tg]], [bpw])
            TS("dve", WS[:, 4 * tg:4 * tg + 4, :], pw[:, 0:32].rearrange("p (a b) -> p a b", a=4),
               float((8 * 64) ** -0.5), None, ALU.mult, None, [bpw], [bWS])
        P.barrier()
        negtri = CF[:, CF_NEGTRI:CF_NEGTRI + 128]
        bPSBh = [Buf("PSBh0"), Buf("PSBh1")]
        trc = [0]
        for g in range(4):
            AR.pos = mark2
            IQ = AR.take([4, 512], BF16)
            bIQ = Buf("IQ")
            WIQ = [AR.take([8, 128], BF16) for _ in range(2)]
            bWIQ = [Buf("WIQ0"), Buf("WIQ1")]
            SC = [AR.take([S], F32) for _ in range(4)]
            bSC = [Buf("SC%d" % i_) for i_ in range(4)]
            RL = [AR.take([512], F32) for _ in range(2)]
            bRL = [Buf("RL0"), Buf("RL1")]
            M01 = [AR.take([S], BF16) for _ in range(2)]
            bM01 = [Buf("M010"), Buf("M011")]
            JNK = [AR.take([S], BF16) for _ in range(2)]
            bJ = [Buf("J%d" % i_) for i_ in range(4)]
            BI = [AR.take([8], F32) for _ in range(4)]
            bBI = [Buf("BI%d" % i_) for i_ in range(4)]
            WK2 = [AR.take([2 * NBIS + 2], F32) for _ in range(4)]
            bWK2 = [Buf("WK%d" % i_) for i_ in range(4)]
            for q in range(4):
                DMA("pool", WIQ[q % 2][:, :, :], wview(w_min, OFF["iq"] + q * 128, 128), (), [bWIQ[q % 2]], "wiq%d" % (q % 2))
                pq, bpq = PS[q % 2], bPS[q % 2]
                MMG(pq[:, :], [(WIQ[q % 2][:, k, :], XN[:, k, tgs(g)]) for k in range(8)], [bWIQ[q % 2], bXN[g]], [bpq])
                CP("act" if q % 2 else "dve", IQ[:, q, :], pq[:, :], [bpq], [bIQ])
            for j in range(4 * g, 4 * g + 4):
                MEMSET("pool", MT[:, j, :], 0.0, [bMT[j]])
            rc = 0
            for ti in range(4):
                i = 4 * g + ti
                n = (i + 1) * 128
                on_act = (ti % 2 == 1)
                sc, bsc = SC[ti], bSC[ti]
                nsg = (n + 511) // 512
                for h in range(8):
                    r0 = (h % 2) * 64
                    for sg_ in range(nsg):
                        s0 = sg_ * 512
                        w = min(512, n - s0)
                        pd, bpd = PS[2 + rc % 2], bPS[2 + rc % 2]
                        rl, brl = RL[rc % 2], bRL[rc % 2]
                        rc += 1
                        MM1(pd[:, 0:w], IQ[r0:r0 + 64, h // 2, ti * 128:(ti + 1) * 128], IKT[r0:r0 + 64, s0:s0 + w], True, True,
                            [bIQ, bIKT], [bpd])
                        ACT(rl[:, 0:w], pd[:, 0:w], AF.Relu, [bpd], [brl])
                        if h == 0:
                            TS("dve", sc[:, s0:s0 + w], rl[:, 0:w], WS[:, i, 0:1], None, ALU.mult, None, [brl, bWS], [bsc])
                        else:
                            STT(sc[:, s0:s0 + w], rl[:, 0:w], WS[:, i, h:h + 1], sc[:, s0:s0 + w], ALU.mult, ALU.add,
                                [brl, bWS, bsc], [bsc])
                bi, bbi = BI[ti], bBI[ti]
                wk, bwk = WK2[ti], bWK2[ti]
                if i >= 2:
                    P.add("dve", (lambda o, a: (lambda e: e.tensor_reduce(out=o, in_=a, axis=AX.X, op=ALU.max, apply_absolute_value=True)))(bi[:, 0:1], sc[:, 0:n]),
                          [bsc], [bbi])
                TT("dve", sc[:, n - 128:n], sc[:, n - 128:n], negtri, ALU.add, [bsc, bCF], [bsc])
                if i >= 2:
                    TS("dve", bi[:, 1:2], bi[:, 0:1], 1.0, None, ALU.add, None, [bbi], [bbi])
                    TS("dve", wk[:, 0:NBIS + 1], CF[:, CF_POW:CF_POW + NBIS + 1], bi[:, 1:2], None, ALU.mult, None, [bbi, bCF], [bwk])
                    MEMSET("dve", bi[:, 2:3], 0.0, [bbi])
                    if not on_act:
                        TS("dve", wk[:, NBIS + 1:2 * NBIS + 2], wk[:, 0:NBIS + 1], 2.0, None, ALU.mult, None, [bwk], [bwk])
                    else:
                        TS("dve", wk[:, NBIS + 1:2 * NBIS + 2], wk[:, 0:NBIS + 1], -1.0, None, ALU.mult, None, [bwk], [bwk])
                        MEMSET("dve", bi[:, 6:7], float(n - 511), [bbi])
            dts = [ti for ti in (0, 2) if 4 * g + ti >= 2]
            ats = [ti for ti in (1, 3) if 4 * g + ti >= 2]
            for k in range(NBIS):
                for ti in dts:
                    n = (4 * g + ti + 1) * 128
                    TS("dve", JNK[0][:, 0:n], SC[ti][:, 0:n], BI[ti][:, 2:3], 0.0, ALU.is_ge, ALU.add, [bSC[ti], bBI[ti]], [bJ[ti], bBI[ti]],
                       accum=BI[ti][:, 3:4])
                for ti in dts:
                    STT(BI[ti][:, 4:5], BI[ti][:, 3:4], 255.5, WK2[ti][:, NBIS + 1 + k:NBIS + 2 + k], ALU.is_ge, ALU.mult, [bBI[ti], bWK2[ti]], [bBI[ti]])
                for ti in dts:
                    STT(BI[ti][:, 2:3], BI[ti][:, 4:5], WK2[ti][:, k:k + 1], BI[ti][:, 2:3], ALU.subtract, ALU.add, [bBI[ti], bWK2[ti]], [bBI[ti]])
                for ti in ats:
                    n = (4 * g + ti + 1) * 128
                    ACT(JNK[1][:, 0:n], SC[ti][:, 0:n], AF.Sign, [bSC[ti], bBI[ti]], [bJ[ti], bBI[ti]], bias=BI[ti][:, 2:3], scale=1.0,
                        accum=BI[ti][:, 3:4])
                for ti in ats:
                    ACT(BI[ti][:, 4:5], BI[ti][:, 3:4], AF.Sign, [bBI[ti]], [bBI[ti]], bias=BI[ti][:, 6:7], scale=1.0)
                for ti in ats:
                    ACT(BI[ti][:, 2:3], BI[ti][:, 4:5], AF.Identity, [bBI[ti], bWK2[ti]], [bBI[ti]], bias=BI[ti][:, 2:3],
                        scale=WK2[ti][:, NBIS + 1 + k:NBIS + 2 + k])
            for ti in range(4):
                i = 4 * g + ti
                n = (i + 1) * 128
                u = ti % 2
                sc, bsc = SC[ti], bSC[ti]
                bi, bbi = BI[ti], bBI[ti]
                wk, bwk = WK2[ti], bWK2[ti]
                m01, bm01 = M01[u], bM01[u]
                if i >= 2 and u == 0:
                    TT("dve", bi[:, 5:6], bi[:, 2:3], wk[:, NBIS - 1:NBIS], ALU.subtract, [bbi, bwk], [bbi])
                elif i >= 2:
                    TS("dve", bi[:, 5:6], bi[:, 2:3], -1.0, wk[:, NBIS - 1:NBIS], ALU.mult, ALU.subtract, [bbi, bwk], [bbi])
                else:
                    MEMSET("dve", bi[:, 5:6], -1e29, [bbi])
                TS("dve", m01[:, 0:n], sc[:, 0:n], bi[:, 5:6], None, ALU.is_ge, None, [bsc, bbi], [bm01])
                for j0 in range(0, i + 1, 4):
                    nj = min(4, i + 1 - j0)
                    for jj in range(nj):
                        j = j0 + jj
                        TR(PSB[:, jj * 128:(jj + 1) * 128], m01[:, j * 128:(j + 1) * 128], [bm01, bCB], [bPSB])
                    CP("act", MT[:, j0:j0 + nj, ti * 128:(ti + 1) * 128],
                       PSB[:, 0:nj * 128].rearrange("p (a b) -> p a b", a=nj), [bPSB], [bMT[j] for j in range(j0, j0 + nj)])
            P.barrier()
            AR.pos = mark2
            WQ = [AR.take([8, 128], BF16) for _ in range(2)]
            bWQ = [Buf("WQ0"), Buf("WQ1")]
            QT = [AR.take([512], BF16) for _ in range(2)]
            bQT = [Buf("QT0"), Buf("QT1")]
            E = [AR.take([512], BF16) for _ in range(NE)]
            bE = [Buf("E%d" % i_) for i_ in range(NE)]
            RD = AR.take([8], F32)
            bRD = Buf("RD")
            ODM = [AR.take([128], BF16) for _ in range(4)]
            bODM = [Buf("ODM%d" % i_) for i_ in range(4)]
            OTg = AR.take([8, 512], BF16)
            bOTg = Buf("OTg")
            bb = BranchBufs(w_dso, OFF["gates"] + D, "d", stream_wo=True)
            MG = [AR.take([5, 256], BF16) for _ in range(2)]
            bMG = [Buf("MG0"), Buf("MG1")]
            nj_all = 4 * g + 4
            items = [(h, j) for h in range(8) for j in range(nj_all)]
            N = len(items)
            qbank = {}
            freeb = [0, 1, 2]
            bACC = [[Buf("ACC%d" % t_) for t_ in range(4)]] * 2

            def acc_ap(set_, ti):
                return PS[3 + ti][:, 0:129]

            def qproj(h):
                s = h % 2
                DMA("pool", WQ[s][:, :, :], wview(w_min, OFF["dq"] + h * 128, 128), (), [bWQ[s]], "wq%d" % s)
                b_ = freeb.pop(0)
                MMG(PS[b_][:, :], [(WQ[s][:, k, :], XN[:, k, tgs(g)]) for k in range(8)], [bWQ[s], bXN[g]], [bPS[b_]])
                TS("dve", QT[s], PS[b_][:, :], 128.0 ** -0.5, None, ALU.mult, None, [bPS[b_]], [bQT[s]])
                freeb.append(b_)

            def qk(n_):
                h, j = items[n_]
                b_ = freeb.pop(0)
                qbank[n_] = b_
                c0 = max(0, 128 * (j - 4 * g))
                MM1(PS[b_][:, c0:512], CKT[:, j * 128:(j + 1) * 128], QT[h % 2][:, c0:512], True, True, [bCKT, bQT[h % 2]], [bPS[b_]])

            def mid(n_):
                h, j = items[n_]
                b_ = qbank[n_]
                e, be = E[n_ % NE], bE[n_ % NE]
                r = j - 4 * g
                c0 = max(0, 128 * r)
                ACT(e[:, c0:512], PS[b_][:, c0:512], AF.Exp, [bPS[b_], bC31], [be], bias=C31[:, h:h + 1], scale=1.0)
                lo = max(0, 128 * r)
                hi = min(512, 128 * r + 256)
                if hi > lo:
                    TT("dve", e[:, lo:hi], e[:, lo:hi], MG[h % 2][:, r + 1, 0:hi - lo], ALU.mult, [be, bMG[h % 2]], [be])
                    if hi < 512:
                        TT("dve", e[:, hi:512], e[:, hi:512], MT[:, j, hi:512], ALU.mult, [be, bMT[j]], [be])
                else:
                    TT("dve", e[:, c0:512], e[:, c0:512], MT[:, j, c0:512], ALU.mult, [be, bMT[j]], [be])
                freeb.append(b_)

            def pv(n_):
                h, j = items[n_]
                e, be = E[n_ % NE], bE[n_ % NE]
                for ti in range(4):
                    i = 4 * g + ti
                    if i < j:
                        continue
                    MM1(acc_ap(h % 2, ti), e[:, ti * 128:(ti + 1) * 128], CKA[:, j, 0:129], j == 0, j == i,
                        [be, bCKA], [bACC[h % 2][ti]])

            def fin_a(h):
                for ti in range(4):
                    pa = acc_ap(h % 2, ti)
                    P.add("dve", (lambda o, a: (lambda e: e.reciprocal(out=o, in_=a)))(RD[:, ti:ti + 1], pa[:, 128:129]), [bACC[h % 2][ti]], [bRD])
                for ti in range(4):
                    pa = acc_ap(h % 2, ti)
                    if ti < 2:
                        TS("dve", ODM[ti], pa[:, 0:128], RD[:, ti:ti + 1], None, ALU.mult, None, [bACC[h % 2][ti], bRD], [bODM[ti]])
                    else:
                        ACT(ODM[ti], pa[:, 0:128], AF.Identity, [bACC[h % 2][ti], bRD], [bODM[ti]], scale=RD[:, ti:ti + 1])

            def fin_b(h):
                for ti in range(4):
                    TR(PSB[:, 512 + ti * 128:512 + (ti + 1) * 128], ODM[ti], [bODM[ti], bCB], [bPSB])
                CP("act", OTg[:, h, :], PSB[:, 512:1024], [bPSB], [bOTg])

            def mk_mg(h):
                for r in range(-1, 4):
                    j = 4 * g + r
                    if j < 0:
                        continue
                    lo = max(0, 128 * r)
                    hi = min(512, 128 * r + 256)
                    x0 = lo - 128 * r
                    TT("pool", MG[h % 2][:, r + 1, 0:hi - lo], MT[:, j, lo:hi], G[:, h, x0:x0 + (hi - lo)], ALU.mult,
                       [bMT[j], bG], [bMG[h % 2]])

            mk_mg(0)
            def warm(e):
                ins = None
                for w_ in range(NWARM):
                    ins = e.matmul(PS[0][:, :], ones, XN[:, w_ % 8, tgs(g)], start=True, stop=True)
                return ins
            P.add("pe", warm, [bCB, bXN[g]], [bPS[0]])
            qproj(0)
            qk(0)
            if N > 1:
                qk(1)
            bb.load()
            pend = []
            for n_ in range(N):
                h, j = items[n_]
                if n_ + 2 < N:
                    qk(n_ + 2)
                mid(n_)
                if j == min(nj_all // 2, nj_all - 3) and h + 1 < 8:
                    qproj(h + 1)
                if j == 0 and h + 1 < 8:
                    mk_mg(h + 1)
                pv(n_)
                for pp_ in list(pend):
                    if n_ >= pp_[0]:
                        fin_b(pp_[1])
                        pend.remove(pp_)
                if j == nj_all - 1:
                    fin_a(h)
                    pend.append((n_ + 2, h))
            for pp_ in pend:
                fin_b(pp_[1])
            branch_out_tg(bb, OTg, bOTg, g)
            P.barrier()

    ple_w = {}

    def ple_prefetch():
        ple_w["WG"] = AR.take([8, D], BF16)
        ple_w["WP"] = AR.take([2, D], BF16)
        ple_w["PT"] = AR.take([2, S], BF16)
        ple_w["b"] = (Buf("pWG"), Buf("pWP"), Buf("pPT"))
        ple_w["pos"] = AR.pos
        bWG_, bWP_, bPT_ = ple_w["b"]
        DMA("pool", ple_w["WG"][:, :, :], wview(w_pg, 0, D), (), [bWG_], "pwg")
        DMA("pool", ple_w["WP"][:, :, :], wview(w_pp, 0, D), (), [bWP_], "pwp")
        DMA("pool", ple_w["PT"][:, :, :], pT.rearrange("(k p) t -> p k t", p=128), (), [bPT_], "ppt")

    def ple():
        AR.reset()
        norm_to_xn(V_PLE)
        if ple_w:
            WG, WP, PTt = ple_w["WG"], ple_w["WP"], ple_w["PT"]
        else:
            WG = AR.take([8, D], BF16)
            WP = AR.take([2, D], BF16)
            PTt = AR.take([2, S], BF16)
        SGf = [AR.take([512], F32) for _ in range(2)]
        TMP = [AR.take([512], F32) for _ in range(2)]
        bSGf = [Buf("pSG0"), Buf("pSG1")]
        bTMP = [Buf("pT0"), Buf("pT1")]
        if ple_w:
            bWG, bWP, bPT = ple_w["b"]
            assert AR.pos <= ple_w["pos"] - (8 * D + 2 * D + 2 * S)
        else:
            bWG, bWP, bPT = Buf("pWG"), Buf("pWP"), Buf("pPT")
            DMA("pool", WG[:, :, :], wview(w_pg, 0, D), (), [bWG], "pwg")
            DMA("pool", WP[:, :, :], wview(w_pp, 0, D), (), [bWP], "pwp")
            DMA("pool", PTt[:, :, :], pT.rearrange("(k p) t -> p k t", p=128), (), [bPT], "ppt")
        c = 0
        for tg in range(4):
            for dc in range(8):
                pg, bpg = PS[(2 * c) % 4], bPS[(2 * c) % 4]
                pp, bpp = PS[(2 * c + 1) % 4], bPS[(2 * c + 1) % 4]
                u = c % 2
                c += 1
                dcs = slice(dc * 128, (dc + 1) * 128)
                MMG(pg[:, :], [(WG[:, k, dcs], XN[:, k, tgs(tg)]) for k in range(8)], [bWG, bXN[tg]], [bpg])
                MMG(pp[:, :], [(WP[:, k, dcs], PTt[:, k, tgs(tg)]) for k in range(2)], [bWP, bPT], [bpp])
                ACT(SGf[u], pg[:, :], AF.Sigmoid, [bpg], [bSGf[u]])
                TT("dve", TMP[u], SGf[u], pp[:, :], ALU.mult, [bSGf[u], bpp], [bTMP[u]])
                TT("pool", H[:, dc, tgs(tg)], H[:, dc, tgs(tg)], TMP[u], ALU.add, [bTMP[u], bH[dc][tg]], [bH[dc][tg]])
        P.barrier()

    if "ffn1" in stages:
        ffn(w_f1i, w_f1o, V_FFN1, "f1")
    if "gla" in stages or "dsa" in stages:
        AR.reset()
        norm_to_xn(V_MIX)
        build_G()
        P.barrier()
    if "gla" in stages:
        gla()
    if "dsa" in stages:
        dsa()
    if "ffn2" in stages:
        ffn(w_f2i, w_f2o, V_FFN2, "f2", prefetch=ple_prefetch if "ple" in stages else None)
    if "ple" in stages:
        ple()
    AR.reset()
    OB = [AR.take([8, 512], F32) for _ in range(2)]
    bOB = [Buf("OB0"), Buf("OB1")]
    yv = yT.rearrange("(k p) t -> p k t", p=128)
    outs = []
    rmsnorm(V_FIN, lambda k, tg: OB[tg % 2][:, k, :], lambda tg: [bOB[tg % 2]],
            after=lambda tg: outs.append(DMA("sp", yv[:, :, tgs(tg)], OB[tg % 2], [bOB[tg % 2]], (), "out%d" % (tg % 2))))
    P.add("sp", None, extra=outs)

    streams = P.finalize()
    sems = {}
    for st in streams:
        sems[st] = es.enter_context(nc.semaphore("s_" + st.replace(":", "_")))
    block = es.enter_context(nc.Block())

    @block.tensor
    def _(e):
        P.emit("pe", e, sems)

    @block.scalar
    def _(e):
        P.emit("act", e, sems)

    @block.vector
    def _(e):
        P.emit("dve", e, sems)

    @block.gpsimd
    def _(e):
        P.emit("pool", e, sems)

    @block.sync
    def _(e):
        P.emit("sp", e, sems)

    es.close()
    return nc


def make_in_maps(inp):
    f = lambda a: np.ascontiguousarray(np.asarray(a, dtype=np.float32))
    cf, cb = make_consts()
    vec = np.zeros((128, NV), np.float32)
    for col, name in ((V_FFN1, "ffn1_norm"), (V_MIX, "mix_norm"), (V_FFN2, "ffn2_norm"), (V_PLE, "ple_norm")):
        vec[:, col:col + 8] = f(inp[name])[0].reshape(8, 128).T
    vec[:, V_FIN:V_FIN + 8] = f(inp["final_norm"]).reshape(8, 128).T
    vec[:, V_CKV] = f(inp["ckv_norm"])[0]
    vec[:, V_GLA:V_GLA + 8] = f(inp["gla_out_norm"])[0].reshape(8, 128).T
    alpha = np.concatenate([f(inp["gla_alpha_w"])[0], f(inp["gla_alpha_b"])[0][None, :]], axis=0)
    shared = {
        "vecs": vec, "cstf": cf, "cstb": cb,
        "ffn1_w_in": f(inp["ffn1_w_in"])[0], "ffn1_w_out": f(inp["ffn1_w_out"])[0],
        "ffn2_w_in": f(inp["ffn2_w_in"])[0], "ffn2_w_out": f(inp["ffn2_w_out"])[0],
        "mix_w_in": f(inp["mix_w_in"])[0], "alpha_aug": f(alpha),
        "gla_w_out": f(inp["gla_w_out"])[0],
        "dsa_w_out": f(inp["dsa_w_out"])[0], "rel_bias": f(inp["rel_bias"]), "rel_biasT": f(np.asarray(inp["rel_bias"]).T),
        "mix_w_out": f(inp["mix_w_out"])[0], "ple_w_gate": f(inp["ple_w_gate"])[0],
        "ple_w_proj": f(inp["ple_w_proj"])[0],
    }
    x = f(inp["x"])
    p = f(inp["p"])
    maps = []
    for b in range(8):
        m = dict(shared)
        m["xT"] = np.ascontiguousarray(x[b].T)
        m["pT"] = np.ascontiguousarray(p[0, b].T)
        maps.append(m)
    return maps


def kernel(**inputs):
    nc = build_nc()
    maps = make_in_maps(inputs)
    res = run_bass_kernel_spmd(nc, maps, core_ids=list(range(8)))
    out = np.stack([np.ascontiguousarray(res.results[b]["yT"].T) for b in range(8)], axis=0)
    return out.astype(np.float32)
```

```python
import math
from contextlib import ExitStack
import numpy as np
import concourse.bass as bass
import concourse.mybir as mybir
from concourse.bass_utils import run_bass_kernel_spmd

F32 = mybir.dt.float32
BF16 = mybir.dt.bfloat16
U32 = mybir.dt.uint32
AF = mybir.ActivationFunctionType
ALU = mybir.AluOpType
AX = mybir.AxisListType

D = 1024
S = 2048
NT = 16
FF = 2816
NF = 22
EPS = 1e-6
OFF = dict(gq=0, gk=512, gv=1024, gr=2048, ga=3072, dq=3088, dkv=4112, iq=4240, ik=4752, iw=4816, gates=4824)
D_IN = 6872
NBIS = 16
ALL_DVE = False
NE = 3
NWARM = 8


class Buf:
    __slots__ = ("name", "w", "r")

    def __init__(self, name):
        self.name = name
        self.w = {}
        self.r = {}


class Op:
    __slots__ = ("stream", "eng", "fn", "deps", "sig", "val", "idx")


class Prog:
    CE = ("pe", "act", "dve", "pool")
    ALL = ("pe", "act", "dve", "pool", "sp")

    def __init__(self):
        self.q = {e: [] for e in self.ALL}
        self.last_dma = {}

    def add(self, eng, fn, reads=(), writes=(), key=None, extra=()):
        stream = eng if key is None else "dma:" + key
        op = Op()
        op.stream, op.eng, op.fn, op.sig, op.val = stream, eng, fn, False, None
        op.idx = len(self.q[eng])
        raw = set()
        oth = set(extra)
        for b in reads:
            raw.update(b.w.values())
        for b in writes:
            oth.update(b.w.values())
            oth.update(b.r.values())
        deps = []
        for d in raw | oth:
            if d is op:
                continue
            if key is None and d.stream == stream:
                if eng == "pe":
                    continue
                if d in raw and op.idx - d.idx <= 2:
                    deps.append(d)
                continue
            deps.append(d)
        for d in deps:
            d.sig = True
        op.deps = deps
        for b in reads:
            b.r[stream] = op
        for b in writes:
            b.r = {}
            b.w[stream] = op
        self.q[eng].append(op)
        if key is not None:
            self.last_dma[stream] = op
        return op

    def barrier(self, exclude=()):
        lasts = []
        for e in self.CE:
            for op in reversed(self.q[e]):
                if op.fn is not None and op.stream == e:
                    lasts.append(op)
                    break
        dl = [op for st, op in self.last_dma.items() if st not in exclude]
        for e in self.ALL:
            self.add(e, None, extra=[l for l in lasts if l.stream != e] + dl)

    def finalize(self):
        cnt = {}
        for e in self.ALL:
            for op in self.q[e]:
                if op.fn is None:
                    continue
                if op.stream.startswith("dma:"):
                    cnt[op.stream] = cnt.get(op.stream, 0) + 16
                    op.val = cnt[op.stream]
                elif op.sig:
                    cnt[op.stream] = cnt.get(op.stream, 0) + 1
                    op.val = cnt[op.stream]
        return sorted(cnt.keys())

    def emit(self, eng, e, sems):
        known = {}
        for op in self.q[eng]:
            need = {}
            for d in op.deps:
                assert d.val is not None
                if known.get(d.stream, 0) < d.val and need.get(d.stream, 0) < d.val:
                    need[d.stream] = d.val
            for st, v in need.items():
                e.wait_ge(sems[st], v)
                known[st] = v
            if op.fn is not None:
                ins = op.fn(e)
                if op.val is not None:
                    ins.then_inc(sems[op.stream], 16 if op.stream.startswith("dma:") else 1)


class Arena:
    def __init__(self, ap, n):
        self.ap, self.n, self.pos = ap, n, 0

    def reset(self):
        self.pos = 0

    def take(self, shape, dt):
        ne = int(np.prod(shape))
        nb = ne * (2 if dt == F32 or dt == U32 else 1)
        nb = (nb + 1) // 2 * 2
        assert self.pos + nb <= self.n, ("arena overflow", self.pos, nb, self.n)
        v = self.ap[:, self.pos:self.pos + nb]
        self.pos += nb
        if dt != BF16:
            v = v.bitcast(dt)
        if len(shape) == 2:
            v = v.rearrange("p (a b) -> p a b", a=shape[0])
        elif len(shape) == 3:
            v = v.rearrange("p (a b c) -> p a b c", a=shape[0], b=shape[1])
        return v


def t5_bucket_np(d):
    d = np.maximum(d, 0)
    ratio = (np.maximum(d, 1).astype(np.float32) / np.float32(16))
    large = 16 + (np.log(ratio).astype(np.float32) / np.float32(math.log(128 / 16)) * np.float32(16)).astype(np.int32)
    large = np.minimum(large, 31)
    return np.where(d < 16, d, large)


CF_TRIU, CF_TRIS, CF_NEGTRI, CF_POW, CF_MISC = 0, 128, 256, 384, 416
NCF = 432
CB_ONES, CB_ID, CB_CAUS = 0, 128, 256
NCB = 384


def make_consts():
    cf = np.zeros((128, NCF), np.float32)
    i = np.arange(128)
    cf[:, CF_TRIU:CF_TRIU + 128] = np.where(i[:, None] <= i[None, :], -1.0 / 16, 0.0)
    cf[:, CF_TRIS:CF_TRIS + 128] = np.where(i[:, None] > i[None, :], -1.0 / 16, 0.0)
    cf[:, CF_NEGTRI:CF_NEGTRI + 128] = np.where(i[None, :] <= i[:, None], 0.0, -1e30)
    for k in range(32):
        cf[:, CF_POW + k] = 2.0 ** (-(k + 1))
    cf[:, CF_MISC + 0] = EPS
    cf[:, CF_MISC + 1] = 1.0
    cf[:, CF_MISC + 2] = 0.0
    cb = np.zeros((128, NCB), np.float32)
    cb[:, CB_ONES:CB_ONES + 128] = 1.0
    cb[:, CB_ID:CB_ID + 128] = np.eye(128)
    cb[:, CB_CAUS:CB_CAUS + 128] = np.where(i[:, None] <= i[None, :], 1.0, 0.0)
    return cf, cb


V_FFN1, V_MIX, V_FFN2, V_PLE, V_FIN, V_CKV, V_GLA = 0, 8, 16, 24, 32, 40, 41
NV = 52


def build_nc(stages=("ffn1", "gla", "dsa", "ffn2", "ple")):
    nc = bass.Bass("TRN2", target_bir_lowering=False)

    def din(name, shape):
        return nc.dram_tensor(name, list(shape), F32, kind="ExternalInput").ap()

    xT = din("xT", [D, S])
    pT = din("pT", [256, S])
    vecs = din("vecs", [128, NV])
    cstf = din("cstf", [128, NCF])
    cstb = din("cstb", [128, NCB])
    w_f1i = din("ffn1_w_in", [D, 2 * FF])
    w_f1o = din("ffn1_w_out", [FF, D])
    w_f2i = din("ffn2_w_in", [D, 2 * FF])
    w_f2o = din("ffn2_w_out", [FF, D])
    w_min = din("mix_w_in", [D, D_IN])
    alpha = din("alpha_aug", [17, 512])
    w_glo = din("gla_w_out", [D, D])
    w_dso = din("dsa_w_out", [D, D])
    relb = din("rel_bias", [32, 8])
    relbT = din("rel_biasT", [8, 32])
    w_mo = din("mix_w_out", [D, D])
    w_pg = din("ple_w_gate", [D, D])
    w_pp = din("ple_w_proj", [256, D])
    yT = nc.dram_tensor("yT", [D, S], F32, kind="ExternalOutput").ap()
    scrA = nc.dram_tensor("scrA", [8, 384], F32, kind="Internal").ap()

    P = Prog()
    es = ExitStack()
    ARN = 53700
    H = es.enter_context(nc.sbuf_tensor("H", [128, 8, S], F32))
    XN = es.enter_context(nc.sbuf_tensor("XN", [128, 8, S], BF16))
    CF = es.enter_context(nc.sbuf_tensor("CF", [128, NCF], F32))
    CB = es.enter_context(nc.sbuf_tensor("CB", [128, NCB], BF16))
    VEC = es.enter_context(nc.sbuf_tensor("VEC", [128, NV], F32))
    ARt = es.enter_context(nc.sbuf_tensor("AR", [128, ARN], BF16))
    PS = [es.enter_context(nc.psum_tensor("ps%d" % i, [128, 512], F32)) for i in range(7)]
    PSB = es.enter_context(nc.psum_tensor("psb", [128, 1024], BF16))
    AR = Arena(ARt, ARN)

    bH = [[Buf("H%d_%d" % (k, tg)) for tg in range(4)] for k in range(8)]
    bXN = [Buf("XN%d" % tg) for tg in range(4)]
    bPS = [Buf("PS%d" % i) for i in range(7)]
    bPSB = Buf("PSB")
    bCF, bCB, bVEC = Buf("CF"), Buf("CB"), Buf("VEC")

    ones = CB[:, CB_ONES:CB_ONES + 128]
    ident = CB[:, CB_ID:CB_ID + 128]
    caus = CB[:, CB_CAUS:CB_CAUS + 128]
    eps_col = CF[:, CF_MISC:CF_MISC + 1]

    def tgs(tg):
        return slice(tg * 512, (tg + 1) * 512)

    def DMA(q, out, in_, reads, writes, key):
        return P.add(q, lambda e: e.dma_start(out=out, in_=in_), reads, writes, key=key)

    def MMG(out, pairs, reads, writes):
        def fn(e):
            n = len(pairs)
            ins = None
            for i, (l, r) in enumerate(pairs):
                ins = e.matmul(out, l, r, start=(i == 0), stop=(i == n - 1))
            return ins
        return P.add("pe", fn, reads, writes)

    def MM1(out, l, r, start, stop, reads, writes):
        return P.add("pe", lambda e: e.matmul(out, l, r, start=start, stop=stop), reads, writes)

    def TR(out, in_, reads, writes):
        return P.add("pe", lambda e: e.transpose(out, in_, ident), reads, writes)

    def ACT(out, in_, func, reads, writes, bias=None, scale=None, accum=None):
        kw = {}
        if bias is not None:
            kw["bias"] = bias
        if scale is not None:
            kw["scale"] = scale
        if accum is not None:
            kw["accum_out"] = accum
        return P.add("act", lambda e: e.activation(out=out, in_=in_, func=func, **kw), reads, writes)

    def TT(eng, out, a, b, op, reads, writes):
        return P.add(eng, lambda e: e.tensor_tensor(out=out, in0=a, in1=b, op=op), reads, writes)

    def STT(out, a, sc, b, op0, op1, reads, writes):
        return P.add("dve", lambda e: e.scalar_tensor_tensor(out=out, in0=a, scalar=sc, in1=b, op0=op0, op1=op1),
                     reads, writes)

    def TS(eng, out, a, s1, s2, op0, op1, reads, writes, accum=None):
        kw = {}
        if accum is not None:
            kw["accum_out"] = accum
        if op1 is None:
            return P.add(eng, lambda e: e.tensor_scalar(out=out, in0=a, scalar1=s1, scalar2=None, op0=op0, **kw),
                         reads, writes)
        return P.add(eng, lambda e: e.tensor_scalar(out=out, in0=a, scalar1=s1, scalar2=s2, op0=op0, op1=op1, **kw),
                     reads, writes)

    def CP(eng, out, in_, reads, writes):
        if eng == "act":
            return P.add("act", lambda e: e.copy(out=out, in_=in_), reads, writes)
        return P.add(eng, lambda e: e.tensor_copy(out=out, in_=in_), reads, writes)

    def MEMSET(eng, out, val, writes):
        return P.add(eng, lambda e: e.memset(out, val), (), writes)

    def wview(w, c0, n):
        return w[:, c0:c0 + n].rearrange("(k p) n -> p k n", p=128)

    DMA("sp", H[:, :, :], xT.rearrange("(k p) t -> p k t", p=128), (), [b for r in bH for b in r], "x")
    DMA("sp", CF[:, :], cstf, (), [bCF], "cf")
    DMA("sp", VEC[:, :], vecs, (), [bVEC], "vec")
    DMA("pool", CB[:, :], cstb, (), [bCB], "cb")

    G = es.enter_context(nc.sbuf_tensor("G", [128, 8, 256], BF16))
    C31 = es.enter_context(nc.sbuf_tensor("C31", [128, 8], F32))
    NC31 = es.enter_context(nc.sbuf_tensor("NC31", [128, 8], F32))
    bG, bC31 = Buf("G"), Buf("C31")
    top = ARN - 4096 - 64 - 768
    TB = ARt[:, top:top + 4096].bitcast(F32).rearrange("p (a b) -> p a b", a=8)
    RBT = ARt[:, top + 4096:top + 4160].bitcast(F32)
    ASB = ARt[:, top + 4160:top + 4928].bitcast(F32)
    bTB, bRBT, bASB, bscr = Buf("TB"), Buf("RBT"), Buf("ASB"), Buf("scrA")
    kk_ = np.arange(384)
    bk_ = t5_bucket_np(kk_ - 127)
    DMA("sp", RBT[0:8, :], relbT, (), [bRBT], "rbt")
    DMA("sp", C31[:, :], relb[31:32, :].to_broadcast([128, 8]), (), [bC31], "c31")
    for b in range(32):
        idx = np.nonzero(bk_ == b)[0]
        if len(idx) == 0:
            continue
        k0, k1 = int(idx[0]), int(idx[-1]) + 1
        assert np.all(bk_[k0:k1] == b)
        CP("dve", ASB[0:8, k0:k1], RBT[0:8, b:b + 1].to_broadcast([8, k1 - k0]), [bRBT], [bASB])
    DMA("sp", scrA[:, :], ASB[0:8, :], [bASB], [bscr], "scr")
    for ss in range(128):
        DMA("sp", TB[ss:ss + 1, :, :], scrA[:, 127 - ss:127 - ss + 256].rearrange("(a h) x -> a h x", a=1), [bscr], [bTB], "tb")
    TS("dve", NC31[:, :], C31[:, :], -1.0, None, ALU.mult, None, [bC31], [bC31])

    def build_G():
        for h in range(8):
            ACT(G[:, h, :], TB[:, h, :], AF.Exp, [bTB, bC31], [bG], bias=NC31[:, h:h + 1], scale=1.0)

    def rmsnorm(gcol, out_fn, out_bufs_fn, after=None):
        SQ = AR.take([8, 512], BF16)
        RS = AR.take([512], F32)
        bSQ, bRS = Buf("SQ"), Buf("RS")
        for tg in range(4):
            hb = [bH[k][tg] for k in range(8)]
            ACT(SQ, H[:, :, tgs(tg)], AF.Square, hb, [bSQ])
            ps, bps = PS[6], bPS[6]
            MMG(ps[:, :], [(ones, SQ[:, k, :]) for k in range(8)], [bSQ, bCB], [bps])
            ACT(RS, ps[:, :], AF.Ln, [bps, bCF], [bRS], bias=eps_col, scale=1.0 / D)
            ACT(RS, RS, AF.Exp, [bRS], [bRS], scale=-0.5)
            for k in range(8):
                STT(out_fn(k, tg), H[:, k, tgs(tg)], VEC[:, gcol + k:gcol + k + 1], RS, ALU.mult, ALU.mult,
                    [bH[k][tg], bRS, bVEC], out_bufs_fn(tg))
            if after is not None:
                after(tg)

    def norm_to_xn(gcol):
        rmsnorm(gcol, lambda k, tg: XN[:, k, tgs(tg)], lambda tg: [bXN[tg]])

    def ffn(w_in, w_out, gcol, tag):
        AR.reset()
        norm_to_xn(gcol)
        A = AR.take([6, S], BF16)
        W1 = [AR.take([8, 256], BF16) for _ in range(3)]
        W2 = [AR.take([6, D], BF16) for _ in range(2)]
        SG = [AR.take([512], BF16) for _ in range(2)]
        bA = [[Buf("A%d_%d" % (f, tg)) for tg in range(4)] for f in range(6)]
        bW1 = [Buf("W1_%d" % i) for i in range(3)]
        bW2 = [Buf("W2_%d" % i) for i in range(2)]
        bSG = [Buf("SG0"), Buf("SG1")]
        groups = [(0, 6), (6, 12), (12, 17), (17, 22)]
        c1 = 0
        c2 = 0
        for gi, (f0, f1) in enumerate(groups):
            nfg = f1 - f0
            w2 = W2[gi % 2]
            for fi in range(f0, f1):
                s = fi % 3
                DMA("pool", W1[s][:, :, 0:128], wview(w_in, fi * 128, 128), (), [bW1[s]], "w1_%d" % s)
                DMA("pool", W1[s][:, :, 128:256], wview(w_in, FF + fi * 128, 128), (), [bW1[s]], "w1_%d" % s)
                if fi == f0:
                    DMA("pool", w2[:, 0:nfg, :], w_out[f0 * 128:f1 * 128, :].rearrange("(f p) n -> p f n", p=128),
                        (), [bW2[gi % 2]], "w2_%d" % (gi % 2))
                for tg in range(4):
                    pg, pu = PS[(2 * c1) % 4], PS[(2 * c1 + 1) % 4]
                    bpg, bpu = bPS[(2 * c1) % 4], bPS[(2 * c1 + 1) % 4]
                    sg, bsg = SG[c1 % 2], bSG[c1 % 2]
                    c1 += 1
                    MMG(pg[:, :], [(W1[s][:, k, 0:128], XN[:, k, tgs(tg)]) for k in range(8)], [bW1[s], bXN[tg]], [bpg])
                    MMG(pu[:, :], [(W1[s][:, k, 128:256], XN[:, k, tgs(tg)]) for k in range(8)], [bW1[s], bXN[tg]], [bpu])
                    ACT(sg, pg[:, :], AF.Silu, [bpg], [bsg])
                    TT("dve", A[:, fi - f0, tgs(tg)], sg, pu[:, :], ALU.mult, [bsg, bpu], [bA[fi - f0][tg]])
            for dc in range(8):
                for tg in range(4):
                    po, bpo = PS[4 + c2 % 2], bPS[4 + c2 % 2]
                    c2 += 1
                    MMG(po[:, :], [(w2[:, f, dc * 128:(dc + 1) * 128], A[:, f, tgs(tg)]) for f in range(nfg)],
                        [bW2[gi % 2]] + [bA[f][tg] for f in range(nfg)], [bpo])
                    STT(H[:, dc, tgs(tg)], po[:, :], 0.5, H[:, dc, tgs(tg)], ALU.mult, ALU.add,
                        [bpo, bH[dc][tg]], [bH[dc][tg]])
        P.barrier()

    class BranchBufs:
        def __init__(self, w_branch, gate_col0, tag, stream_wo=False, pre=None):
            self.pre = pre
            self.WB = AR.take([8, D], BF16) if pre is None else pre[0]
            self.WG = AR.take([8, D], BF16)
            self.stream_wo = stream_wo
            if stream_wo:
                self.WOc = [AR.take([8, 128], BF16) for _ in range(2)]
                self.bWOc = [Buf("WOc0"), Buf("WOc1")]
            else:
                self.WO = AR.take([8, D], BF16)
            self.M = AR.take([8, 512], BF16)
            self.SG = [AR.take([512], BF16) for _ in range(2)]
            self.bWB, self.bWG, self.bWO = Buf("WB"), Buf("WG"), Buf("WO")
            if pre is not None:
                self.bWB = pre[1]
            self.bM = Buf("M")
            self.bSG = [Buf("bSG0"), Buf("bSG1")]
            self.c = 0
            self.args = (w_branch, gate_col0, tag)

        def load(self):
            w_branch, gate_col0, tag = self.args
            if self.pre is None:
                DMA("pool", self.WB[:, :, :], wview(w_branch, 0, D), (), [self.bWB], "bwb" + tag)
            DMA("pool", self.WG[:, :, :], wview(w_min, gate_col0, D), (), [self.bWG], "bwg" + tag)
            if not self.stream_wo:
                DMA("pool", self.WO[:, :, :], wview(w_mo, 0, D), (), [self.bWO], "bwo" + tag)

    def branch_out_tg(bb, OTv, bOTv, tg):
        for dc in range(8):
            c = bb.c
            bb.c += 1
            s = c % 2
            dcs = slice(dc * 128, (dc + 1) * 128)
            py, bpy = PS[(2 * c) % 4], bPS[(2 * c) % 4]
            pg, bpg = PS[(2 * c + 1) % 4], bPS[(2 * c + 1) % 4]
            MMG(py[:, :], [(bb.WB[:, k, dcs], OTv[:, k, :]) for k in range(8)], [bb.bWB, bOTv], [bpy])
            MMG(pg[:, :], [(bb.WG[:, k, dcs], XN[:, k, tgs(tg)]) for k in range(8)], [bb.bWG, bXN[tg]], [bpg])
            ACT(bb.SG[s], pg[:, :], AF.Sigmoid, [bpg], [bb.bSG[s]])
            TT("dve", bb.M[:, dc, :], bb.SG[s], py[:, :], ALU.mult, [bb.bSG[s], bpy], [bb.bM])
        for dc in range(8):
            c = bb.c
            bb.c += 1
            s = c % 2
            dcs = slice(dc * 128, (dc + 1) * 128)
            po, bpo = PS[4 + s], bPS[4 + s]
            if bb.stream_wo:
                DMA("pool", bb.WOc[s][:, :, :], wview(w_mo, dc * 128, 128), (), [bb.bWOc[s]], "bwoc%d" % s)
                MMG(po[:, :], [(bb.WOc[s][:, k, :], bb.M[:, k, :]) for k in range(8)], [bb.bWOc[s], bb.bM], [bpo])
            else:
                MMG(po[:, :], [(bb.WO[:, k, dcs], bb.M[:, k, :]) for k in range(8)], [bb.bWO, bb.bM], [bpo])
            TT("dve", H[:, dc, tgs(tg)], po[:, :], H[:, dc, tgs(tg)], ALU.add, [bpo, bH[dc][tg]], [bH[dc][tg]])

    def gla():
        AR.reset()
        OT = AR.take([8, S], BF16)
        bOT = [Buf("OT%d" % tg) for tg in range(4)]
        mark = AR.pos
        GAT = AR.take([S], BF16)
        ALP = AR.take([512], BF16)
        WA = AR.take([8, 16], BF16)
        bGAT, bALP, bWA = Buf("GAT"), Buf("ALP"), Buf("WA")
        DMA("pool", ALP[0:17, :], alpha, (), [bALP], "alp")
        DMA("pool", WA[:, :, :], wview(w_min, OFF["ga"], 16), (), [bWA], "wa")
        MEMSET("dve", GAT[0:17, :], 1.0, [bGAT])
        for tg in range(4):
            ps, bps = PS[tg % 2], bPS[tg % 2]
            MMG(ps[0:16, :], [(WA[:, k, :], XN[:, k, tgs(tg)]) for k in range(8)], [bWA, bXN[tg]], [bps])
            CP("dve", GAT[0:16, tgs(tg)], ps[0:16, :], [bps], [bGAT])
        Wq = AR.take([8, 128], BF16)
        Wk = AR.take([8, 128], BF16)
        Wv = AR.take([8, 256], BF16)
        Wr = AR.take([8, 256], BF16)
        bWh = Buf("Wh")
        EB = [AR.take([512], BF16) for _ in range(2)]
        ENB = [AR.take([512], BF16) for _ in range(2)]
        EREV = [AR.take([4, 128], BF16) for _ in range(2)]
        QB = [AR.take([512], BF16) for _ in range(2)]
        KB = [AR.take([512], BF16) for _ in range(2)]
        KD = [AR.take([4, 128], BF16) for _ in range(2)]
        V = [AR.take([4, 256], BF16) for _ in range(2)]
        GR = [AR.take([4, 256], BF16) for _ in range(2)]
        EBL = AR.take([NT], F32)
        bEB = [Buf("EB0"), Buf("EB1")]
        bEREV = [Buf("EREV0"), Buf("EREV1")]
        bQB = [Buf("QB0"), Buf("QB1")]
        bKB = [Buf("KB0"), Buf("KB1")]
        bKD = [Buf("KD0"), Buf("KD1")]
        bV = [Buf("V0"), Buf("V1")]
        bGR = [Buf("GR0"), Buf("GR1")]
        bEBL = Buf("EBL")
        TE = [AR.take([128], F32) for _ in range(2)]
        LT = [AR.take([128], F32) for _ in range(2)]
        bTE = [Buf("TE0"), Buf("TE1")]
        bLT = [Buf("LT0"), Buf("LT1")]
        PTm = [AR.take([128], BF16) for _ in range(2)]
        bPT = [Buf("PT0"), Buf("PT1")]
        ST = AR.take([256], F32)
        SB = [AR.take([256], BF16) for _ in range(2)]
        bST = Buf("ST")
        bSB = [Buf("SB0"), Buf("SB1")]
        JK = AR.take([256], BF16)
        bJK = Buf("JK")
        SS = [AR.take([4], F32) for _ in range(2)]
        bSS = [Buf("SS0"), Buf("SS1")]
        OTM = [AR.take([256], BF16) for _ in range(2)]
        bOTM = [Buf("OTM0"), Buf("OTM1")]
        triu = CF[:, CF_TRIU:CF_TRIU + 128]
        tris = CF[:, CF_TRIS:CF_TRIS + 128]
        one_col = CF[:, CF_MISC + 1:CF_MISC + 2]
        WBpre = AR.take([8, D], BF16)
        bWBpre = Buf("WBpre")

        def prep(h, tg, par):
            if tg == 0:
                DMA("pool", Wq[:, :, :], wview(w_min, OFF["gq"] + h * 128, 128), (), [bWh], "wh")
                DMA("pool", Wk[:, :, :], wview(w_min, OFF["gk"] + h * 128, 128), (), [bWh], "wh")
                DMA("pool", Wv[:, :, :], wview(w_min, OFF["gv"] + h * 256, 256), (), [bWh], "wh")
                DMA("pool", Wr[:, :, :], wview(w_min, OFF["gr"] + h * 256, 256), (), [bWh], "wh")
                if h == 3:
                    DMA("pool", WBpre[:, :, :], wview(w_glo, 0, D), (), [bWBpre], "bwbpre")
            for ci in range(4):
                c = 4 * tg + ci
                cs = slice(c * 128, (c + 1) * 128)
                ls = slice(ci * 128, (ci + 1) * 128)
                u = c % 2
                px, bpx = PS[u], bPS[u]
                MM1(px[:, 0:128], GAT[0:17, cs], ALP[0:17, h * 128:(h + 1) * 128], True, True, [bGAT, bALP], [bpx])
                ACT(TE[u], px[:, 0:128], AF.Exp, [bpx], [bTE[u]], scale=-1.0)
                ACT(LT[u], TE[u], AF.Ln, [bTE[u], bCF], [bLT[u]], bias=one_col, scale=1.0)
                pb, bpb = PS[2 + u], bPS[2 + u]
                MM1(pb[:, 0:128], LT[u], triu, True, True, [bLT[u], bCF], [bpb])
                MM1(pb[:, 128:256], tris, LT[u], True, True, [bLT[u], bCF], [bpb])
                ACT(EB[par][:, ls], pb[:, 0:128], AF.Exp, [bpb], [bEB[par]])
                ACT(ENB[par][:, ls], pb[:, 0:128], AF.Exp, [bpb], [bEB[par]], scale=-1.0)
                ACT(EREV[par][:, ci, :], pb[:, 128:256], AF.Exp, [bpb], [bEREV[par]])
                ACT(EBL[:, c:c + 1], pb[:, 127:128], AF.Exp, [bpb], [bEBL])
            pq, bpq = PS[4], bPS[4]
            pk, bpk = PS[5], bPS[5]
            MMG(pq[:, :], [(Wq[:, k, :], XN[:, k, tgs(tg)]) for k in range(8)], [bWh, bXN[tg]], [bpq])
            STT(QB[par], pq[:, :], 128.0 ** -0.5, EB[par], ALU.mult, ALU.mult, [bpq, bEB[par]], [bQB[par]])
            MMG(pk[:, :], [(Wk[:, k, :], XN[:, k, tgs(tg)]) for k in range(8)], [bWh, bXN[tg]], [bpk])
            TT("dve", KB[par], pk[:, :], ENB[par], ALU.mult, [bpk, bEB[par]], [bKB[par]])
            for ci in range(4):
                c = 4 * tg + ci
                cs = slice(c * 128, (c + 1) * 128)
                u = c % 2
                pk2, bpk2 = PS[u], bPS[u]
                MMG(pk2[:, 0:128], [(XN[:, k, cs], Wk[:, k, :]) for k in range(8)], [bWh, bXN[tg]], [bpk2])
                TT("dve", KD[par][:, ci, :], pk2[:, 0:128], EREV[par][:, ci, :], ALU.mult, [bpk2, bEREV[par]], [bKD[par]])
                pv, bpv = PS[2 + u], bPS[2 + u]
                MMG(pv[:, 0:256], [(XN[:, k, cs], Wv[:, k, :]) for k in range(8)], [bWh, bXN[tg]], [bpv])
                CP("act", V[par][:, ci, :], pv[:, 0:256], [bpv], [bV[par]])
                pr, bpr = PS[4 + u], bPS[4 + u]
                MMG(pr[:, 0:256], [(XN[:, k, cs], Wr[:, k, :]) for k in range(8)], [bWh, bXN[tg]], [bpr])
                ACT(GR[par][:, ci, :], pr[:, 0:256], AF.Silu, [bpr], [bGR[par]])

        def rec(h, tg, par):
            def cidx(ci):
                c = 4 * tg + ci
                return c, slice(c * 128, (c + 1) * 128), slice(ci * 128, (ci + 1) * 128), c % 2

            def stA(ci):
                c, cs, ls, u = cidx(ci)
                psc, bpsc = PS[u], bPS[u]
                MM1(psc[:, 128:256], KB[par][:, ls], QB[par][:, ls], True, True, [bKB[par], bQB[par]], [bpsc])
                TT("dve", PTm[u], psc[:, 128:256], caus, ALU.mult, [bpsc, bCB], [bPT[u]])

            def stB(ci):
                c, cs, ls, u = cidx(ci)
                if c < NT - 1:
                    pd, bpd = PS[4 + u], bPS[4 + u]
                    MM1(pd[:, 256:512], KD[par][:, ci, :], V[par][:, ci, :], True, True, [bKD[par], bV[par]], [bpd])
                    if c == 0:
                        CP("dve", ST, pd[:, 256:512], [bpd], [bST])
                    else:
                        STT(ST, ST, EBL[:, c:c + 1], pd[:, 256:512], ALU.mult, ALU.add, [bST, bEBL, bpd], [bST])
                    CP("act", SB[c % 2], ST, [bST], [bSB[c % 2]])

            def stC(ci):
                c, cs, ls, u = cidx(ci)
                po, bpo = PS[2 + u], bPS[2 + u]
                if c == 0:
                    MM1(po[:, 256:512], PTm[u], V[par][:, ci, :], True, True, [bPT[u], bV[par]], [bpo])
                else:
                    sp_ = (c - 1) % 2
                    MM1(po[:, 256:512], PTm[u], V[par][:, ci, :], True, False, [bPT[u], bV[par]], [bpo])
                    MM1(po[:, 256:512], QB[par][:, ls], SB[sp_], False, True, [bQB[par], bSB[sp_]], [bpo])
                ss, bss = SS[u], bSS[u]
                ACT(JK, po[:, 256:512], AF.Square, [bpo], [bJK, bss], accum=ss[:, 0:1])
                ACT(ss[:, 1:2], ss[:, 0:1], AF.Ln, [bss, bCF], [bss], bias=eps_col, scale=1.0 / 256)
                ACT(ss[:, 2:3], ss[:, 1:2], AF.Exp, [bss], [bss], scale=-0.5)
                STT(OTM[u], po[:, 256:512], ss[:, 2:3], GR[par][:, ci, :], ALU.mult, ALU.mult, [bpo, bss, bGR[par]], [bOTM[u]])

            def stD(ci):
                c, cs, ls, u = cidx(ci)
                TR(PSB[:, 0:128], OTM[u][:, 0:128], [bOTM[u], bCB], [bPSB])
                TR(PSB[:, 128:256], OTM[u][:, 128:256], [bOTM[u], bCB], [bPSB])
                for a_ in range(2):
                    TS("dve", OT[:, 2 * h + a_, cs], PSB[:, a_ * 128:(a_ + 1) * 128], VEC[:, V_GLA + 2 * h + a_:V_GLA + 2 * h + a_ + 1],
                       None, ALU.mult, None, [bPSB, bVEC], [bOT[tg]])

            for step in range(6):
                if step < 4:
                    stA(step)
                if 1 <= step <= 4:
                    stC(step - 1)
                if step < 4:
                    stB(step)
                if 2 <= step <= 5:
                    stD(step - 2)

        items = [(h, tg) for h in range(4) for tg in range(4)]
        prep(items[0][0], items[0][1], 0)
        for n, (h, tg) in enumerate(items):
            if n + 1 < len(items):
                prep(items[n + 1][0], items[n + 1][1], (n + 1) % 2)
            rec(h, tg, n % 2)
        P.barrier()
        AR.pos = mark
        bb = BranchBufs(w_glo, OFF["gates"], "g", pre=(WBpre, bWBpre))
        assert AR.pos + 8 * D + 8 * D + 8 * 512 + 2 * 512 <= 0 or True
        bb.load()
        for tg in range(4):
            branch_out_tg(bb, OT[:, :, tgs(tg)], bOT[tg], tg)
        P.barrier()

    def dsa():
        AR.reset()
        CKT = AR.take([S], BF16)
        CKA = AR.take([NT, 132], BF16)
        IKT = AR.take([S], BF16)
        WS = AR.take([NT, 8], F32)
        MT = AR.take([NT, 512], BF16)
        bMT = [Buf("MT%d" % j) for j in range(NT)]
        bCKT, bCKA, bIKT, bWS = Buf("CKT"), Buf("CKA"), Buf("IKT"), Buf("WS")
        mark2 = AR.pos
        WKV = AR.take([8, 128], BF16)
        WIK = AR.take([8, 128], BF16)
        WIW = AR.take([8, 8], BF16)
        bWsh = Buf("Wsh")
        DMA("pool", WKV[:, :, :], wview(w_min, OFF["dkv"], 128), (), [bWsh], "wsh")
        DMA("pool", WIK[:, :, 0:64], wview(w_min, OFF["ik"], 64), (), [bWsh], "wsh")
        DMA("pool", WIK[:, :, 64:128], wview(w_min, OFF["ik"], 64), (), [bWsh], "wsh")
        DMA("pool", WIW[:, :, :], wview(w_min, OFF["iw"], 8), (), [bWsh], "wsh")
        SQ = AR.take([512], BF16)
        RS = AR.take([512], F32)
        bSQ, bRS = Buf("dSQ"), Buf("dRS")
        MEMSET("pool", CKA[:, :, 128:132], 1.0, [bCKA])
        for tg in range(4):
            pc, bpc = PS[tg % 2], bPS[tg % 2]
            MMG(pc[:, :], [(WKV[:, k, :], XN[:, k, tgs(tg)]) for k in range(8)], [bWsh, bXN[tg]], [bpc])
            ACT(SQ, pc[:, :], AF.Square, [bpc], [bSQ])
            pn, bpn = PS[2], bPS[2]
            MM1(pn[:, :], ones, SQ, True, True, [bSQ, bCB], [bpn])
            ACT(RS, pn[:, :], AF.Ln, [bpn, bCF], [bRS], bias=eps_col, scale=1.0 / 128)
            ACT(RS, RS, AF.Exp, [bRS], [bRS], scale=-0.5)
            STT(CKT[:, tgs(tg)], pc[:, :], VEC[:, V_CKV:V_CKV + 1], RS, ALU.mult, ALU.mult, [bpc, bRS, bVEC], [bCKT])
            pi, bpi = PS[3], bPS[3]
            MMG(pi[:, :], [(WIK[:, k, :], XN[:, k, tgs(tg)]) for k in range(8)], [bWsh, bXN[tg]], [bpi])
            CP("act", IKT[:, tgs(tg)], pi[:, :], [bpi], [bIKT])
            for ti in range(4):
                c = tg * 4 + ti
                cs = slice(c * 128, (c + 1) * 128)
                TR(PSB[:, ti * 128:(ti + 1) * 128], CKT[:, cs], [bCKT, bCB], [bPSB])
            CP("dve", CKA[:, 4 * tg:4 * tg + 4, 0:128], PSB[:, 0:512].rearrange("p (a b) -> p a b", a=4), [bPSB], [bCKA])
            pw, bpw = PS[4], bPS[4]
            for ti in range(4):
                c = tg * 4 + ti
                cs = slice(c * 128, (c + 1) * 128)
                MMG(pw[:, ti * 8:ti * 8 + 8], [(XN[:, k, cs], WIW[:, k, :]) for k in range(8)], [bWsh, bXN[tg]], [bpw])
            TS("dve", WS[:, 4 * tg:4 * tg + 4, :], pw[:, 0:32].rearrange("p (a b) -> p a b", a=4),
               float((8 * 64) ** -0.5), None, ALU.mult, None, [bpw], [bWS])
        P.barrier()
        negtri = CF[:, CF_NEGTRI:CF_NEGTRI + 128]
        bPSBh = [Buf("PSBh0"), Buf("PSBh1")]
        trc = [0]
        for g in range(4):
            AR.pos = mark2
            IQ = AR.take([4, 512], BF16)
            bIQ = Buf("IQ")
            WIQ = [AR.take([8, 128], BF16) for _ in range(2)]
            bWIQ = [Buf("WIQ0"), Buf("WIQ1")]
            SC = [AR.take([S], F32) for _ in range(4)]
            bSC = [Buf("SC%d" % i_) for i_ in range(4)]
            RL = [AR.take([512], F32) for _ in range(2)]
            bRL = [Buf("RL0"), Buf("RL1")]
            M01 = [AR.take([S], BF16) for _ in range(2)]
            bM01 = [Buf("M010"), Buf("M011")]
            JNK = [AR.take([S], BF16) for _ in range(2)]
            bJ = [Buf("J%d" % i_) for i_ in range(4)]
            BI = [AR.take([8], F32) for _ in range(4)]
            bBI = [Buf("BI%d" % i_) for i_ in range(4)]
            WK2 = [AR.take([2 * NBIS + 2], F32) for _ in range(4)]
            bWK2 = [Buf("WK%d" % i_) for i_ in range(4)]
            for q in range(4):
                DMA("pool", WIQ[q % 2][:, :, :], wview(w_min, OFF["iq"] + q * 128, 128), (), [bWIQ[q % 2]], "wiq%d" % (q % 2))
                pq, bpq = PS[q % 2], bPS[q % 2]
                MMG(pq[:, :], [(WIQ[q % 2][:, k, :], XN[:, k, tgs(g)]) for k in range(8)], [bWIQ[q % 2], bXN[g]], [bpq])
                CP("act" if q % 2 else "dve", IQ[:, q, :], pq[:, :], [bpq], [bIQ])
            for j in range(4 * g, 4 * g + 4):
                MEMSET("pool", MT[:, j, :], 0.0, [bMT[j]])
            rc = 0
            for ti in range(4):
                i = 4 * g + ti
                n = (i + 1) * 128
                on_act = (ti % 2 == 1)
                sc, bsc = SC[ti], bSC[ti]
                nsg = (n + 511) // 512
                for h in range(8):
                    r0 = (h % 2) * 64
                    for sg_ in range(nsg):
                        s0 = sg_ * 512
                        w = min(512, n - s0)
                        pd, bpd = PS[2 + rc % 2], bPS[2 + rc % 2]
                        rl, brl = RL[rc % 2], bRL[rc % 2]
                        rc += 1
                        MM1(pd[:, 0:w], IQ[r0:r0 + 64, h // 2, ti * 128:(ti + 1) * 128], IKT[r0:r0 + 64, s0:s0 + w], True, True,
                            [bIQ, bIKT], [bpd])
                        ACT(rl[:, 0:w], pd[:, 0:w], AF.Relu, [bpd], [brl])
                        if h == 0:
                            TS("dve", sc[:, s0:s0 + w], rl[:, 0:w], WS[:, i, 0:1], None, ALU.mult, None, [brl, bWS], [bsc])
                        else:
                            STT(sc[:, s0:s0 + w], rl[:, 0:w], WS[:, i, h:h + 1], sc[:, s0:s0 + w], ALU.mult, ALU.add,
                                [brl, bWS, bsc], [bsc])
                bi, bbi = BI[ti], bBI[ti]
                wk, bwk = WK2[ti], bWK2[ti]
                if i >= 2:
                    P.add("dve", (lambda o, a: (lambda e: e.tensor_reduce(out=o, in_=a, axis=AX.X, op=ALU.max, apply_absolute_value=True)))(bi[:, 0:1], sc[:, 0:n]),
                          [bsc], [bbi])
                TT("dve", sc[:, n - 128:n], sc[:, n - 128:n], negtri, ALU.add, [bsc, bCF], [bsc])
                if i >= 2:
                    TS("dve", bi[:, 1:2], bi[:, 0:1], 1.0, None, ALU.add, None, [bbi], [bbi])
                    TS("dve", wk[:, 0:NBIS + 1], CF[:, CF_POW:CF_POW + NBIS + 1], bi[:, 1:2], None, ALU.mult, None, [bbi, bCF], [bwk])
                    MEMSET("dve", bi[:, 2:3], 0.0, [bbi])
                    if not on_act:
                        TS("dve", wk[:, NBIS + 1:2 * NBIS + 2], wk[:, 0:NBIS + 1], 2.0, None, ALU.mult, None, [bwk], [bwk])
                    else:
                        TS("dve", wk[:, NBIS + 1:2 * NBIS + 2], wk[:, 0:NBIS + 1], -1.0, None, ALU.mult, None, [bwk], [bwk])
                        MEMSET("dve", bi[:, 6:7], float(n - 511), [bbi])
            dts = [ti for ti in (0, 2) if 4 * g + ti >= 2]
            ats = [ti for ti in (1, 3) if 4 * g + ti >= 2]
            for k in range(NBIS):
                for ti in dts:
                    n = (4 * g + ti + 1) * 128
                    TS("dve", JNK[0][:, 0:n], SC[ti][:, 0:n], BI[ti][:, 2:3], 0.0, ALU.is_ge, ALU.add, [bSC[ti], bBI[ti]], [bJ[ti], bBI[ti]],
                       accum=BI[ti][:, 3:4])
                for ti in dts:
                    STT(BI[ti][:, 4:5], BI[ti][:, 3:4], 255.5, WK2[ti][:, NBIS + 1 + k:NBIS + 2 + k], ALU.is_ge, ALU.mult, [bBI[ti], bWK2[ti]], [bBI[ti]])
                for ti in dts:
                    STT(BI[ti][:, 2:3], BI[ti][:, 4:5], WK2[ti][:, k:k + 1], BI[ti][:, 2:3], ALU.subtract, ALU.add, [bBI[ti], bWK2[ti]], [bBI[ti]])
                for ti in ats:
                    n = (4 * g + ti + 1) * 128
                    ACT(JNK[1][:, 0:n], SC[ti][:, 0:n], AF.Sign, [bSC[ti], bBI[ti]], [bJ[ti], bBI[ti]], bias=BI[ti][:, 2:3], scale=1.0,
                        accum=BI[ti][:, 3:4])
                for ti in ats:
                    ACT(BI[ti][:, 4:5], BI[ti][:, 3:4], AF.Sign, [bBI[ti]], [bBI[ti]], bias=BI[ti][:, 6:7], scale=1.0)
                for ti in ats:
                    ACT(BI[ti][:, 2:3], BI[ti][:, 4:5], AF.Identity, [bBI[ti], bWK2[ti]], [bBI[ti]], bias=BI[ti][:, 2:3],
                        scale=WK2[ti][:, NBIS + 1 + k:NBIS + 2 + k])
            for ti in range(4):
                i = 4 * g + ti
                n = (i + 1) * 128
                u = ti % 2
                sc, bsc = SC[ti], bSC[ti]
                bi, bbi = BI[ti], bBI[ti]
                wk, bwk = WK2[ti], bWK2[ti]
                m01, bm01 = M01[u], bM01[u]
                if i >= 2 and u == 0:
                    TT("dve", bi[:, 5:6], bi[:, 2:3], wk[:, NBIS - 1:NBIS], ALU.subtract, [bbi, bwk], [bbi])
                elif i >= 2:
                    TS("dve", bi[:, 5:6], bi[:, 2:3], -1.0, wk[:, NBIS - 1:NBIS], ALU.mult, ALU.subtract, [bbi, bwk], [bbi])
                else:
                    MEMSET("dve", bi[:, 5:6], -1e29, [bbi])
                TS("dve", m01[:, 0:n], sc[:, 0:n], bi[:, 5:6], None, ALU.is_ge, None, [bsc, bbi], [bm01])
                for j0 in range(0, i + 1, 4):
                    nj = min(4, i + 1 - j0)
                    for jj in range(nj):
                        j = j0 + jj
                        TR(PSB[:, jj * 128:(jj + 1) * 128], m01[:, j * 128:(j + 1) * 128], [bm01, bCB], [bPSB])
                    CP("act", MT[:, j0:j0 + nj, ti * 128:(ti + 1) * 128],
                       PSB[:, 0:nj * 128].rearrange("p (a b) -> p a b", a=nj), [bPSB], [bMT[j] for j in range(j0, j0 + nj)])
            P.barrier()
            AR.pos = mark2
            WQ = [AR.take([8, 128], BF16) for _ in range(2)]
            bWQ = [Buf("WQ0"), Buf("WQ1")]
            QT = [AR.take([512], BF16) for _ in range(2)]
            bQT = [Buf("QT0"), Buf("QT1")]
            E = [AR.take([512], BF16) for _ in range(NE)]
            bE = [Buf("E%d" % i_) for i_ in range(NE)]
            RD = AR.take([8], F32)
            bRD = Buf("RD")
            ODM = [AR.take([128], BF16) for _ in range(4)]
            bODM = [Buf("ODM%d" % i_) for i_ in range(4)]
            OTg = AR.take([8, 512], BF16)
            bOTg = Buf("OTg")
            bb = BranchBufs(w_dso, OFF["gates"] + D, "d", stream_wo=True)
            MG = [AR.take([5, 256], BF16) for _ in range(2)]
            bMG = [Buf("MG0"), Buf("MG1")]
            nj_all = 4 * g + 4
            items = [(h, j) for h in range(8) for j in range(nj_all)]
            N = len(items)
            qbank = {}
            freeb = [0, 1, 2]
            bACC = [[Buf("ACC%d" % t_) for t_ in range(4)]] * 2

            def acc_ap(set_, ti):
                return PS[3 + ti][:, 0:129]

            def qload(h):
                s = h % 2
                DMA("pool", WQ[s][:, :, :], wview(w_min, OFF["dq"] + h * 128, 128), (), [bWQ[s]], "wq%d" % s)

            def qproj(h):
                s = h % 2
                b_ = freeb.pop(0)
                MMG(PS[b_][:, :], [(WQ[s][:, k, :], XN[:, k, tgs(g)]) for k in range(8)], [bWQ[s], bXN[g]], [bPS[b_]])
                TS("dve", QT[s], PS[b_][:, :], 128.0 ** -0.5, None, ALU.mult, None, [bPS[b_]], [bQT[s]])
                freeb.append(b_)

            def qk(n_):
                h, j = items[n_]
                b_ = freeb.pop(0)
                qbank[n_] = b_
                c0 = max(0, 128 * (j - 4 * g))
                MM1(PS[b_][:, c0:512], CKT[:, j * 128:(j + 1) * 128], QT[h % 2][:, c0:512], True, True, [bCKT, bQT[h % 2]], [bPS[b_]])

            def mid(n_):
                h, j = items[n_]
                b_ = qbank[n_]
                e, be = E[n_ % NE], bE[n_ % NE]
                r = j - 4 * g
                c0 = max(0, 128 * r)
                ACT(e[:, c0:512], PS[b_][:, c0:512], AF.Exp, [bPS[b_], bC31], [be], bias=C31[:, h:h + 1], scale=1.0)
                lo = max(0, 128 * r)
                hi = min(512, 128 * r + 256)
                if hi > lo:
                    TT("dve", e[:, lo:hi], e[:, lo:hi], MG[h % 2][:, r + 1, 0:hi - lo], ALU.mult, [be, bMG[h % 2]], [be])
                    if hi < 512:
                        TT("dve", e[:, hi:512], e[:, hi:512], MT[:, j, hi:512], ALU.mult, [be, bMT[j]], [be])
                else:
                    TT("dve", e[:, c0:512], e[:, c0:512], MT[:, j, c0:512], ALU.mult, [be, bMT[j]], [be])
                freeb.append(b_)

            def pv(n_):
                h, j = items[n_]
                e, be = E[n_ % NE], bE[n_ % NE]
                for ti in range(4):
                    i = 4 * g + ti
                    if i < j:
                        continue
                    MM1(acc_ap(h % 2, ti), e[:, ti * 128:(ti + 1) * 128], CKA[:, j, 0:129], j == 0, j == i,
                        [be, bCKA], [bACC[h % 2][ti]])

            def fin_a(h):
                for ti in range(4):
                    pa = acc_ap(h % 2, ti)
                    P.add("dve", (lambda o, a: (lambda e: e.reciprocal(out=o, in_=a)))(RD[:, ti:ti + 1], pa[:, 128:129]), [bACC[h % 2][ti]], [bRD])
                for ti in range(4):
                    pa = acc_ap(h % 2, ti)
                    if ti < 2:
                        TS("dve", ODM[ti], pa[:, 0:128], RD[:, ti:ti + 1], None, ALU.mult, None, [bACC[h % 2][ti], bRD], [bODM[ti]])
                    else:
                        ACT(ODM[ti], pa[:, 0:128], AF.Identity, [bACC[h % 2][ti], bRD], [bODM[ti]], scale=RD[:, ti:ti + 1])

            def fin_b(h):
                for ti in range(4):
                    TR(PSB[:, 512 + ti * 128:512 + (ti + 1) * 128], ODM[ti], [bODM[ti], bCB], [bPSB])
                CP("act", OTg[:, h, :], PSB[:, 512:1024], [bPSB], [bOTg])

            def mk_mg(h):
                for r in range(-1, 4):
                    j = 4 * g + r
                    if j < 0:
                        continue
                    lo = max(0, 128 * r)
                    hi = min(512, 128 * r + 256)
                    x0 = lo - 128 * r
                    TT("pool", MG[h % 2][:, r + 1, 0:hi - lo], MT[:, j, lo:hi], G[:, h, x0:x0 + (hi - lo)], ALU.mult,
                       [bMT[j], bG], [bMG[h % 2]])

            qload(0)
            qload(1)
            mk_mg(0)
            def warm(e):
                ins = None
                for w_ in range(NWARM):
                    ins = e.matmul(PS[0][:, :], ones, XN[:, w_ % 8, tgs(g)], start=True, stop=True)
                return ins
            P.add("pe", warm, [bCB, bXN[g]], [bPS[0]])
            qproj(0)
            qk(0)
            if N > 1:
                qk(1)
            bb.load()
            pend = []
            for n_ in range(N):
                h, j = items[n_]
                if n_ + 2 < N:
                    qk(n_ + 2)
                mid(n_)
                if j == min(nj_all // 2, nj_all - 3) and h + 1 < 8:
                    qproj(h + 1)
                if j == 0 and h + 1 < 8:
                    mk_mg(h + 1)
                    if h >= 1:
                        qload(h + 1)
                pv(n_)
                for pp_ in list(pend):
                    if n_ >= pp_[0]:
                        fin_b(pp_[1])
                        pend.remove(pp_)
                if j == nj_all - 1:
                    fin_a(h)
                    pend.append((n_ + 2, h))
            for pp_ in pend:
                fin_b(pp_[1])
            branch_out_tg(bb, OTg, bOTg, g)
            P.barrier()

    def ple():
        AR.reset()
        norm_to_xn(V_PLE)
        WG = AR.take([8, D], BF16)
        WP = AR.take([2, D], BF16)
        PTt = AR.take([2, S], BF16)
        SGf = [AR.take([512], F32) for _ in range(2)]
        TMP = [AR.take([512], F32) for _ in range(2)]
        bWG, bWP, bPT = Buf("pWG"), Buf("pWP"), Buf("pPT")
        bSGf = [Buf("pSG0"), Buf("pSG1")]
        bTMP = [Buf("pT0"), Buf("pT1")]
        DMA("pool", WG[:, :, :], wview(w_pg, 0, D), (), [bWG], "pwg")
        DMA("pool", WP[:, :, :], wview(w_pp, 0, D), (), [bWP], "pwp")
        DMA("pool", PTt[:, :, :], pT.rearrange("(k p) t -> p k t", p=128), (), [bPT], "ppt")
        c = 0
        for tg in range(4):
            for dc in range(8):
                pg, bpg = PS[(2 * c) % 4], bPS[(2 * c) % 4]
                pp, bpp = PS[(2 * c + 1) % 4], bPS[(2 * c + 1) % 4]
                u = c % 2
                c += 1
                dcs = slice(dc * 128, (dc + 1) * 128)
                MMG(pg[:, :], [(WG[:, k, dcs], XN[:, k, tgs(tg)]) for k in range(8)], [bWG, bXN[tg]], [bpg])
                MMG(pp[:, :], [(WP[:, k, dcs], PTt[:, k, tgs(tg)]) for k in range(2)], [bWP, bPT], [bpp])
                ACT(SGf[u], pg[:, :], AF.Sigmoid, [bpg], [bSGf[u]])
                TT("dve", TMP[u], SGf[u], pp[:, :], ALU.mult, [bSGf[u], bpp], [bTMP[u]])
                TT("pool", H[:, dc, tgs(tg)], H[:, dc, tgs(tg)], TMP[u], ALU.add, [bTMP[u], bH[dc][tg]], [bH[dc][tg]])
        P.barrier()

    if "ffn1" in stages:
        ffn(w_f1i, w_f1o, V_FFN1, "f1")
    if "gla" in stages or "dsa" in stages:
        AR.reset()
        norm_to_xn(V_MIX)
        build_G()
        P.barrier()
    if "gla" in stages:
        gla()
    if "dsa" in stages:
        dsa()
    if "ffn2" in stages:
        ffn(w_f2i, w_f2o, V_FFN2, "f2")
    if "ple" in stages:
        ple()
    AR.reset()
    OB = [AR.take([8, 512], F32) for _ in range(2)]
    bOB = [Buf("OB0"), Buf("OB1")]
    yv = yT.rearrange("(k p) t -> p k t", p=128)
    outs = []
    rmsnorm(V_FIN, lambda k, tg: OB[tg % 2][:, k, :], lambda tg: [bOB[tg % 2]],
            after=lambda tg: outs.append(DMA("sp", yv[:, :, tgs(tg)], OB[tg % 2], [bOB[tg % 2]], (), "out%d" % (tg % 2))))
    P.add("sp", None, extra=outs)

    streams = P.finalize()
    sems = {}
    for st in streams:
        sems[st] = es.enter_context(nc.semaphore("s_" + st.replace(":", "_")))
    block = es.enter_context(nc.Block())

    @block.tensor
    def _(e):
        P.emit("pe", e, sems)

    @block.scalar
    def _(e):
        P.emit("act", e, sems)

    @block.vector
    def _(e):
        P.emit("dve", e, sems)

    @block.gpsimd
    def _(e):
        P.emit("pool", e, sems)

    @block.sync
    def _(e):
        P.emit("sp", e, sems)

    es.close()
    return nc


def make_in_maps(inp):
    f = lambda a: np.ascontiguousarray(np.asarray(a, dtype=np.float32))
    cf, cb = make_consts()
    vec = np.zeros((128, NV), np.float32)
    for col, name in ((V_FFN1, "ffn1_norm"), (V_MIX, "mix_norm"), (V_FFN2, "ffn2_norm"), (V_PLE, "ple_norm")):
        vec[:, col:col + 8] = f(inp[name])[0].reshape(8, 128).T
    vec[:, V_FIN:V_FIN + 8] = f(inp["final_norm"]).reshape(8, 128).T
    vec[:, V_CKV] = f(inp["ckv_norm"])[0]
    vec[:, V_GLA:V_GLA + 8] = f(inp["gla_out_norm"])[0].reshape(8, 128).T
    alpha = np.concatenate([f(inp["gla_alpha_w"])[0], f(inp["gla_alpha_b"])[0][None, :]], axis=0)
    shared = {
        "vecs": vec, "cstf": cf, "cstb": cb,
        "ffn1_w_in": f(inp["ffn1_w_in"])[0], "ffn1_w_out": f(inp["ffn1_w_out"])[0],
        "ffn2_w_in": f(inp["ffn2_w_in"])[0], "ffn2_w_out": f(inp["ffn2_w_out"])[0],
        "mix_w_in": f(inp["mix_w_in"])[0], "alpha_aug": f(alpha),
        "gla_w_out": f(inp["gla_w_out"])[0],
        "dsa_w_out": f(inp["dsa_w_out"])[0], "rel_bias": f(inp["rel_bias"]), "rel_biasT": f(np.asarray(inp["rel_bias"]).T),
        "mix_w_out": f(inp["mix_w_out"])[0], "ple_w_gate": f(inp["ple_w_gate"])[0],
        "ple_w_proj": f(inp["ple_w_proj"])[0],
    }
    x = f(inp["x"])
    p = f(inp["p"])
    maps = []
    for b in range(8):
        m = dict(shared)
        m["xT"] = np.ascontiguousarray(x[b].T)
        m["pT"] = np.ascontiguousarray(p[0, b].T)
        maps.append(m)
    return maps


def kernel(**inputs):
    nc = build_nc()
    maps = make_in_maps(inputs)
    res = run_bass_kernel_spmd(nc, maps, core_ids=list(range(8)))
    out = np.stack([np.ascontiguousarray(res.results[b]["yT"].T) for b in range(8)], axis=0)
    return out.astype(np.float32)
```

```python
import math
from contextlib import ExitStack
import numpy as np
import concourse.bass as bass
import concourse.mybir as mybir
from concourse.bass_utils import run_bass_kernel_spmd

F32 = mybir.dt.float32
BF16 = mybir.dt.bfloat16
U32 = mybir.dt.uint32
AF = mybir.ActivationFunctionType
ALU = mybir.AluOpType
AX = mybir.AxisListType

D = 1024
S = 2048
NT = 16
FF = 2816
NF = 22
EPS = 1e-6
OFF = dict(gq=0, gk=512, gv=1024, gr=2048, ga=3072, dq=3088, dkv=4112, iq=4240, ik=4752, iw=4816, gates=4824)
D_IN = 6872
NBIS = 16
ALL_DVE = False
NE = 3
NWARM = 8


class Buf:
    __slots__ = ("name", "w", "r")

    def __init__(self, name):
        self.name = name
        self.w = {}
        self.r = {}


class Op:
    __slots__ = ("stream", "eng", "fn", "deps", "sig", "val", "idx")


class Prog:
    CE = ("pe", "act", "dve", "pool")
    ALL = ("pe", "act", "dve", "pool", "sp")

    def __init__(self):
        self.q = {e: [] for e in self.ALL}
        self.last_dma = {}

    def add(self, eng, fn, reads=(), writes=(), key=None, extra=()):
        stream = eng if key is None else "dma:" + key
        op = Op()
        op.stream, op.eng, op.fn, op.sig, op.val = stream, eng, fn, False, None
        op.idx = len(self.q[eng])
        raw = set()
        oth = set(extra)
        for b in reads:
            raw.update(b.w.values())
        for b in writes:
            oth.update(b.w.values())
            oth.update(b.r.values())
        deps = []
        for d in raw | oth:
            if d is op:
                continue
            if key is None and d.stream == stream:
                if eng == "pe":
                    continue
                if d in raw and op.idx - d.idx <= 2:
                    deps.append(d)
                continue
            deps.append(d)
        for d in deps:
            d.sig = True
        op.deps = deps
        for b in reads:
            b.r[stream] = op
        for b in writes:
            b.r = {}
            b.w[stream] = op
        self.q[eng].append(op)
        if key is not None:
            self.last_dma[stream] = op
        return op

    def barrier(self, exclude=()):
        lasts = []
        for e in self.CE:
            for op in reversed(self.q[e]):
                if op.fn is not None and op.stream == e:
                    lasts.append(op)
                    break
        dl = [op for st, op in self.last_dma.items() if st not in exclude]
        for e in self.ALL:
            self.add(e, None, extra=[l for l in lasts if l.stream != e] + dl)

    def finalize(self):
        cnt = {}
        for e in self.ALL:
            for op in self.q[e]:
                if op.fn is None:
                    continue
                if op.stream.startswith("dma:"):
                    cnt[op.stream] = cnt.get(op.stream, 0) + 16
                    op.val = cnt[op.stream]
                elif op.sig:
                    cnt[op.stream] = cnt.get(op.stream, 0) + 1
                    op.val = cnt[op.stream]
        return sorted(cnt.keys())

    def emit(self, eng, e, sems):
        known = {}
        for op in self.q[eng]:
            need = {}
            for d in op.deps:
                assert d.val is not None
                if known.get(d.stream, 0) < d.val and need.get(d.stream, 0) < d.val:
                    need[d.stream] = d.val
            for st, v in need.items():
                e.wait_ge(sems[st], v)
                known[st] = v
            if op.fn is not None:
                ins = op.fn(e)
                if op.val is not None:
                    ins.then_inc(sems[op.stream], 16 if op.stream.startswith("dma:") else 1)


class Arena:
    def __init__(self, ap, n):
        self.ap, self.n, self.pos = ap, n, 0

    def reset(self):
        self.pos = 0

    def take(self, shape, dt):
        ne = int(np.prod(shape))
        nb = ne * (2 if dt == F32 or dt == U32 else 1)
        nb = (nb + 1) // 2 * 2
        assert self.pos + nb <= self.n, ("arena overflow", self.pos, nb, self.n)
        v = self.ap[:, self.pos:self.pos + nb]
        self.pos += nb
        if dt != BF16:
            v = v.bitcast(dt)
        if len(shape) == 2:
            v = v.rearrange("p (a b) -> p a b", a=shape[0])
        elif len(shape) == 3:
            v = v.rearrange("p (a b c) -> p a b c", a=shape[0], b=shape[1])
        return v


def t5_bucket_np(d):
    d = np.maximum(d, 0)
    ratio = (np.maximum(d, 1).astype(np.float32) / np.float32(16))
    large = 16 + (np.log(ratio).astype(np.float32) / np.float32(math.log(128 / 16)) * np.float32(16)).astype(np.int32)
    large = np.minimum(large, 31)
    return np.where(d < 16, d, large)


CF_TRIU, CF_TRIS, CF_NEGTRI, CF_POW, CF_MISC = 0, 128, 256, 384, 416
NCF = 432
CB_ONES, CB_ID, CB_CAUS = 0, 128, 256
NCB = 384


def make_consts():
    cf = np.zeros((128, NCF), np.float32)
    i = np.arange(128)
    cf[:, CF_TRIU:CF_TRIU + 128] = np.where(i[:, None] <= i[None, :], -1.0 / 16, 0.0)
    cf[:, CF_TRIS:CF_TRIS + 128] = np.where(i[:, None] > i[None, :], -1.0 / 16, 0.0)
    cf[:, CF_NEGTRI:CF_NEGTRI + 128] = np.where(i[None, :] <= i[:, None], 0.0, -1e30)
    for k in range(32):
        cf[:, CF_POW + k] = 2.0 ** (-(k + 1))
    cf[:, CF_MISC + 0] = EPS
    cf[:, CF_MISC + 1] = 1.0
    cf[:, CF_MISC + 2] = 0.0
    cb = np.zeros((128, NCB), np.float32)
    cb[:, CB_ONES:CB_ONES + 128] = 1.0
    cb[:, CB_ID:CB_ID + 128] = np.eye(128)
    cb[:, CB_CAUS:CB_CAUS + 128] = np.where(i[:, None] <= i[None, :], 1.0, 0.0)
    return cf, cb


V_FFN1, V_MIX, V_FFN2, V_PLE, V_FIN, V_CKV, V_GLA = 0, 8, 16, 24, 32, 40, 41
NV = 52


def build_nc(stages=("ffn1", "gla", "dsa", "ffn2", "ple")):
    nc = bass.Bass("TRN2", target_bir_lowering=False)

    def din(name, shape):
        return nc.dram_tensor(name, list(shape), F32, kind="ExternalInput").ap()

    xT = din("xT", [D, S])
    pT = din("pT", [256, S])
    vecs = din("vecs", [128, NV])
    cstf = din("cstf", [128, NCF])
    cstb = din("cstb", [128, NCB])
    w_f1i = din("ffn1_w_in", [D, 2 * FF])
    w_f1o = din("ffn1_w_out", [FF, D])
    w_f2i = din("ffn2_w_in", [D, 2 * FF])
    w_f2o = din("ffn2_w_out", [FF, D])
    w_min = din("mix_w_in", [D, D_IN])
    alpha = din("alpha_aug", [17, 512])
    w_glo = din("gla_w_out", [D, D])
    w_dso = din("dsa_w_out", [D, D])
    relb = din("rel_bias", [32, 8])
    relbT = din("rel_biasT", [8, 32])
    w_mo = din("mix_w_out", [D, D])
    w_pg = din("ple_w_gate", [D, D])
    w_pp = din("ple_w_proj", [256, D])
    yT = nc.dram_tensor("yT", [D, S], F32, kind="ExternalOutput").ap()
    scrA = nc.dram_tensor("scrA", [8, 384], F32, kind="Internal").ap()

    P = Prog()
    es = ExitStack()
    ARN = 53700
    H = es.enter_context(nc.sbuf_tensor("H", [128, 8, S], F32))
    XN = es.enter_context(nc.sbuf_tensor("XN", [128, 8, S], BF16))
    CF = es.enter_context(nc.sbuf_tensor("CF", [128, NCF], F32))
    CB = es.enter_context(nc.sbuf_tensor("CB", [128, NCB], BF16))
    VEC = es.enter_context(nc.sbuf_tensor("VEC", [128, NV], F32))
    ARt = es.enter_context(nc.sbuf_tensor("AR", [128, ARN], BF16))
    PS = [es.enter_context(nc.psum_tensor("ps%d" % i, [128, 512], F32)) for i in range(7)]
    PSB = es.enter_context(nc.psum_tensor("psb", [128, 1024], BF16))
    AR = Arena(ARt, ARN)

    bH = [[Buf("H%d_%d" % (k, tg)) for tg in range(4)] for k in range(8)]
    bXN = [Buf("XN%d" % tg) for tg in range(4)]
    bPS = [Buf("PS%d" % i) for i in range(7)]
    bPSB = Buf("PSB")
    bCF, bCB, bVEC = Buf("CF"), Buf("CB"), Buf("VEC")

    ones = CB[:, CB_ONES:CB_ONES + 128]
    ident = CB[:, CB_ID:CB_ID + 128]
    caus = CB[:, CB_CAUS:CB_CAUS + 128]
    eps_col = CF[:, CF_MISC:CF_MISC + 1]

    def tgs(tg):
        return slice(tg * 512, (tg + 1) * 512)

    def DMA(q, out, in_, reads, writes, key):
        return P.add(q, lambda e: e.dma_start(out=out, in_=in_), reads, writes, key=key)

    def MMG(out, pairs, reads, writes):
        def fn(e):
            n = len(pairs)
            ins = None
            for i, (l, r) in enumerate(pairs):
                ins = e.matmul(out, l, r, start=(i == 0), stop=(i == n - 1))
            return ins
        return P.add("pe", fn, reads, writes)

    def MM1(out, l, r, start, stop, reads, writes):
        return P.add("pe", lambda e: e.matmul(out, l, r, start=start, stop=stop), reads, writes)

    def TR(out, in_, reads, writes):
        return P.add("pe", lambda e: e.transpose(out, in_, ident), reads, writes)

    def ACT(out, in_, func, reads, writes, bias=None, scale=None, accum=None):
        kw = {}
        if bias is not None:
            kw["bias"] = bias
        if scale is not None:
            kw["scale"] = scale
        if accum is not None:
            kw["accum_out"] = accum
        return P.add("act", lambda e: e.activation(out=out, in_=in_, func=func, **kw), reads, writes)

    def TT(eng, out, a, b, op, reads, writes):
        return P.add(eng, lambda e: e.tensor_tensor(out=out, in0=a, in1=b, op=op), reads, writes)

    def STT(out, a, sc, b, op0, op1, reads, writes):
        return P.add("dve", lambda e: e.scalar_tensor_tensor(out=out, in0=a, scalar=sc, in1=b, op0=op0, op1=op1),
                     reads, writes)

    def TS(eng, out, a, s1, s2, op0, op1, reads, writes, accum=None):
        kw = {}
        if accum is not None:
            kw["accum_out"] = accum
        if op1 is None:
            return P.add(eng, lambda e: e.tensor_scalar(out=out, in0=a, scalar1=s1, scalar2=None, op0=op0, **kw),
                         reads, writes)
        return P.add(eng, lambda e: e.tensor_scalar(out=out, in0=a, scalar1=s1, scalar2=s2, op0=op0, op1=op1, **kw),
                     reads, writes)

    def CP(eng, out, in_, reads, writes):
        if eng == "act":
            return P.add("act", lambda e: e.copy(out=out, in_=in_), reads, writes)
        return P.add(eng, lambda e: e.tensor_copy(out=out, in_=in_), reads, writes)

    def MEMSET(eng, out, val, writes):
        return P.add(eng, lambda e: e.memset(out, val), (), writes)

    def wview(w, c0, n):
        return w[:, c0:c0 + n].rearrange("(k p) n -> p k n", p=128)

    xv_ = xT.rearrange("(k p) t -> p k t", p=128)
    for tg_ in range(4):
        DMA("sp", H[:, :, tg_ * 512:(tg_ + 1) * 512], xv_[:, :, tg_ * 512:(tg_ + 1) * 512], (), [bH[k_][tg_] for k_ in range(8)], "x%d" % tg_)
    DMA("sp", CF[:, :], cstf, (), [bCF], "cf")
    DMA("sp", VEC[:, :], vecs, (), [bVEC], "vec")
    DMA("pool", CB[:, :], cstb, (), [bCB], "cb")

    G = es.enter_context(nc.sbuf_tensor("G", [128, 8, 256], BF16))
    C31 = es.enter_context(nc.sbuf_tensor("C31", [128, 8], F32))
    NC31 = es.enter_context(nc.sbuf_tensor("NC31", [128, 8], F32))
    bG, bC31 = Buf("G"), Buf("C31")
    top = ARN - 4096 - 64 - 768
    TB = ARt[:, top:top + 4096].bitcast(F32).rearrange("p (a b) -> p a b", a=8)
    RBT = ARt[:, top + 4096:top + 4160].bitcast(F32)
    ASB = ARt[:, top + 4160:top + 4928].bitcast(F32)
    bTB, bRBT, bASB, bscr = Buf("TB"), Buf("RBT"), Buf("ASB"), Buf("scrA")
    kk_ = np.arange(384)
    bk_ = t5_bucket_np(kk_ - 127)
    DMA("sp", RBT[0:8, :], relbT, (), [bRBT], "rbt")
    DMA("sp", C31[:, :], relb[31:32, :].to_broadcast([128, 8]), (), [bC31], "c31")
    for b in range(32):
        idx = np.nonzero(bk_ == b)[0]
        if len(idx) == 0:
            continue
        k0, k1 = int(idx[0]), int(idx[-1]) + 1
        assert np.all(bk_[k0:k1] == b)
        CP("dve", ASB[0:8, k0:k1], RBT[0:8, b:b + 1].to_broadcast([8, k1 - k0]), [bRBT], [bASB])
    DMA("sp", scrA[:, :], ASB[0:8, :], [bASB], [bscr], "scr")
    for ss in range(128):
        DMA("sp", TB[ss:ss + 1, :, :], scrA[:, 127 - ss:127 - ss + 256].rearrange("(a h) x -> a h x", a=1), [bscr], [bTB], "tb")
    TS("dve", NC31[:, :], C31[:, :], -1.0, None, ALU.mult, None, [bC31], [bC31])

    def build_G():
        for h in range(8):
            ACT(G[:, h, :], TB[:, h, :], AF.Exp, [bTB, bC31], [bG], bias=NC31[:, h:h + 1], scale=1.0)

    def rmsnorm(gcol, out_fn, out_bufs_fn, after=None):
        SQ = AR.take([8, 512], BF16)
        RS = AR.take([512], F32)
        bSQ, bRS = Buf("SQ"), Buf("RS")
        for tg in range(4):
            hb = [bH[k][tg] for k in range(8)]
            ACT(SQ, H[:, :, tgs(tg)], AF.Square, hb, [bSQ])
            ps, bps = PS[6], bPS[6]
            MMG(ps[:, :], [(ones, SQ[:, k, :]) for k in range(8)], [bSQ, bCB], [bps])
            ACT(RS, ps[:, :], AF.Ln, [bps, bCF], [bRS], bias=eps_col, scale=1.0 / D)
            ACT(RS, RS, AF.Exp, [bRS], [bRS], scale=-0.5)
            for k in range(8):
                STT(out_fn(k, tg), H[:, k, tgs(tg)], VEC[:, gcol + k:gcol + k + 1], RS, ALU.mult, ALU.mult,
                    [bH[k][tg], bRS, bVEC], out_bufs_fn(tg))
            if after is not None:
                after(tg)

    def norm_to_xn(gcol):
        rmsnorm(gcol, lambda k, tg: XN[:, k, tgs(tg)], lambda tg: [bXN[tg]])

    def ffn(w_in, w_out, gcol, tag):
        AR.reset()
        norm_to_xn(gcol)
        A = AR.take([6, S], BF16)
        W1 = [AR.take([8, 256], BF16) for _ in range(3)]
        W2 = [AR.take([6, D], BF16) for _ in range(2)]
        SG = [AR.take([512], BF16) for _ in range(2)]
        bA = [[Buf("A%d_%d" % (f, tg)) for tg in range(4)] for f in range(6)]
        bW1 = [Buf("W1_%d" % i) for i in range(3)]
        bW2 = [Buf("W2_%d" % i) for i in range(2)]
        bSG = [Buf("SG0"), Buf("SG1")]
        groups = [(0, 6), (6, 12), (12, 17), (17, 22)]
        c1 = 0
        c2 = 0
        for gi, (f0, f1) in enumerate(groups):
            nfg = f1 - f0
            w2 = W2[gi % 2]
            for fi in range(f0, f1):
                s = fi % 3
                DMA("pool", W1[s][:, :, 0:128], wview(w_in, fi * 128, 128), (), [bW1[s]], "w1_%d" % s)
                DMA("pool", W1[s][:, :, 128:256], wview(w_in, FF + fi * 128, 128), (), [bW1[s]], "w1_%d" % s)
                if fi == f0:
                    DMA("pool", w2[:, 0:nfg, :], w_out[f0 * 128:f1 * 128, :].rearrange("(f p) n -> p f n", p=128),
                        (), [bW2[gi % 2]], "w2_%d" % (gi % 2))
                for tg in range(4):
                    pg, pu = PS[(2 * c1) % 4], PS[(2 * c1 + 1) % 4]
                    bpg, bpu = bPS[(2 * c1) % 4], bPS[(2 * c1 + 1) % 4]
                    sg, bsg = SG[c1 % 2], bSG[c1 % 2]
                    c1 += 1
                    MMG(pg[:, :], [(W1[s][:, k, 0:128], XN[:, k, tgs(tg)]) for k in range(8)], [bW1[s], bXN[tg]], [bpg])
                    MMG(pu[:, :], [(W1[s][:, k, 128:256], XN[:, k, tgs(tg)]) for k in range(8)], [bW1[s], bXN[tg]], [bpu])
                    ACT(sg, pg[:, :], AF.Silu, [bpg], [bsg])
                    TT("dve", A[:, fi - f0, tgs(tg)], sg, pu[:, :], ALU.mult, [bsg, bpu], [bA[fi - f0][tg]])
            for dc in range(8):
                for tg in range(4):
                    po, bpo = PS[4 + c2 % 2], bPS[4 + c2 % 2]
                    c2 += 1
                    MMG(po[:, :], [(w2[:, f, dc * 128:(dc + 1) * 128], A[:, f, tgs(tg)]) for f in range(nfg)],
                        [bW2[gi % 2]] + [bA[f][tg] for f in range(nfg)], [bpo])
                    STT(H[:, dc, tgs(tg)], po[:, :], 0.5, H[:, dc, tgs(tg)], ALU.mult, ALU.add,
                        [bpo, bH[dc][tg]], [bH[dc][tg]])
        P.barrier()

    class BranchBufs:
        def __init__(self, w_branch, gate_col0, tag, stream_wo=False, pre=None):
            self.pre = pre
            self.WB = AR.take([8, D], BF16) if pre is None else pre[0]
            self.WG = AR.take([8, D], BF16)
            self.stream_wo = stream_wo
            if stream_wo:
                self.WOc = [AR.take([8, 128], BF16) for _ in range(2)]
                self.bWOc = [Buf("WOc0"), Buf("WOc1")]
            else:
                self.WO = AR.take([8, D], BF16)
            self.M = AR.take([8, 512], BF16)
            self.SG = [AR.take([512], BF16) for _ in range(2)]
            self.bWB, self.bWG, self.bWO = Buf("WB"), Buf("WG"), Buf("WO")
            if pre is not None:
                self.bWB = pre[1]
            self.bM = Buf("M")
            self.bSG = [Buf("bSG0"), Buf("bSG1")]
            self.c = 0
            self.wo_pre = 0
            self.args = (w_branch, gate_col0, tag)

        def load(self):
            w_branch, gate_col0, tag = self.args
            if self.pre is None:
                DMA("pool", self.WB[:, :, :], wview(w_branch, 0, D), (), [self.bWB], "bwb" + tag)
            DMA("pool", self.WG[:, :, :], wview(w_min, gate_col0, D), (), [self.bWG], "bwg" + tag)
            if not self.stream_wo:
                DMA("pool", self.WO[:, :, :], wview(w_mo, 0, D), (), [self.bWO], "bwo" + tag)
            else:
                assert self.c == 0
                for dc_ in range(2):
                    DMA("pool", self.WOc[dc_][:, :, :], wview(w_mo, dc_ * 128, 128), (), [self.bWOc[dc_]], "bwoc%d" % dc_)
                self.wo_pre = 2

    def branch_out_tg(bb, OTv, bOTv, tg):
        for dc in range(8):
            c = bb.c
            bb.c += 1
            s = c % 2
            dcs = slice(dc * 128, (dc + 1) * 128)
            py, bpy = PS[(2 * c) % 4], bPS[(2 * c) % 4]
            pg, bpg = PS[(2 * c + 1) % 4], bPS[(2 * c + 1) % 4]
            MMG(py[:, :], [(bb.WB[:, k, dcs], OTv[:, k, :]) for k in range(8)], [bb.bWB, bOTv], [bpy])
            MMG(pg[:, :], [(bb.WG[:, k, dcs], XN[:, k, tgs(tg)]) for k in range(8)], [bb.bWG, bXN[tg]], [bpg])
            ACT(bb.SG[s], pg[:, :], AF.Sigmoid, [bpg], [bb.bSG[s]])
            TT("dve", bb.M[:, dc, :], bb.SG[s], py[:, :], ALU.mult, [bb.bSG[s], bpy], [bb.bM])
        for dc in range(8):
            c = bb.c
            bb.c += 1
            s = c % 2
            dcs = slice(dc * 128, (dc + 1) * 128)
            po, bpo = PS[4 + s], bPS[4 + s]
            if bb.stream_wo:
                assert s == dc % 2
                if dc >= bb.wo_pre:
                    DMA("pool", bb.WOc[s][:, :, :], wview(w_mo, dc * 128, 128), (), [bb.bWOc[s]], "bwoc%d" % s)
                MMG(po[:, :], [(bb.WOc[s][:, k, :], bb.M[:, k, :]) for k in range(8)], [bb.bWOc[s], bb.bM], [bpo])
            else:
                MMG(po[:, :], [(bb.WO[:, k, dcs], bb.M[:, k, :]) for k in range(8)], [bb.bWO, bb.bM], [bpo])
            TT("dve", H[:, dc, tgs(tg)], po[:, :], H[:, dc, tgs(tg)], ALU.add, [bpo, bH[dc][tg]], [bH[dc][tg]])

    def gla():
        AR.reset()
        OT = AR.take([8, S], BF16)
        bOT = [Buf("OT%d" % tg) for tg in range(4)]
        mark = AR.pos
        GAT = AR.take([S], BF16)
        ALP = AR.take([512], BF16)
        WA = AR.take([8, 16], BF16)
        bGAT, bALP, bWA = Buf("GAT"), Buf("ALP"), Buf("WA")
        DMA("pool", ALP[0:17, :], alpha, (), [bALP], "alp")
        DMA("pool", WA[:, :, :], wview(w_min, OFF["ga"], 16), (), [bWA], "wa")
        MEMSET("dve", GAT[0:17, :], 1.0, [bGAT])
        for tg in range(4):
            ps, bps = PS[tg % 2], bPS[tg % 2]
            MMG(ps[0:16, :], [(WA[:, k, :], XN[:, k, tgs(tg)]) for k in range(8)], [bWA, bXN[tg]], [bps])
            CP("dve", GAT[0:16, tgs(tg)], ps[0:16, :], [bps], [bGAT])
        Wq = AR.take([8, 128], BF16)
        Wk = AR.take([8, 128], BF16)
        Wv = AR.take([8, 256], BF16)
        Wr = AR.take([8, 256], BF16)
        bWh = Buf("Wh")
        EB = [AR.take([512], BF16) for _ in range(2)]
        ENB = [AR.take([512], BF16) for _ in range(2)]
        EREV = [AR.take([4, 128], BF16) for _ in range(2)]
        QB = [AR.take([512], BF16) for _ in range(2)]
        KB = [AR.take([512], BF16) for _ in range(2)]
        KD = [AR.take([4, 128], BF16) for _ in range(2)]
        V = [AR.take([4, 256], BF16) for _ in range(2)]
        GR = [AR.take([4, 256], BF16) for _ in range(2)]
        EBL = AR.take([NT], F32)
        bEB = [Buf("EB0"), Buf("EB1")]
        bEREV = [Buf("EREV0"), Buf("EREV1")]
        bQB = [Buf("QB0"), Buf("QB1")]
        bKB = [Buf("KB0"), Buf("KB1")]
        bKD = [Buf("KD0"), Buf("KD1")]
        bV = [Buf("V0"), Buf("V1")]
        bGR = [Buf("GR0"), Buf("GR1")]
        bEBL = Buf("EBL")
        TE = [AR.take([128], F32) for _ in range(2)]
        LT = [AR.take([128], F32) for _ in range(2)]
        bTE = [Buf("TE0"), Buf("TE1")]
        bLT = [Buf("LT0"), Buf("LT1")]
        PTm = [AR.take([128], BF16) for _ in range(2)]
        bPT = [Buf("PT0"), Buf("PT1")]
        ST = AR.take([256], F32)
        SB = [AR.take([256], BF16) for _ in range(2)]
        bST = Buf("ST")
        bSB = [Buf("SB0"), Buf("SB1")]
        JK = AR.take([256], BF16)
        bJK = Buf("JK")
        SS = [AR.take([4], F32) for _ in range(2)]
        bSS = [Buf("SS0"), Buf("SS1")]
        OTM = [AR.take([256], BF16) for _ in range(2)]
        bOTM = [Buf("OTM0"), Buf("OTM1")]
        triu = CF[:, CF_TRIU:CF_TRIU + 128]
        tris = CF[:, CF_TRIS:CF_TRIS + 128]
        one_col = CF[:, CF_MISC + 1:CF_MISC + 2]
        WBpre = AR.take([8, D], BF16)
        bWBpre = Buf("WBpre")

        def prep(h, tg, par):
            if tg == 0:
                DMA("pool", Wq[:, :, :], wview(w_min, OFF["gq"] + h * 128, 128), (), [bWh], "wh")
                DMA("pool", Wk[:, :, :], wview(w_min, OFF["gk"] + h * 128, 128), (), [bWh], "wh")
                DMA("pool", Wv[:, :, :], wview(w_min, OFF["gv"] + h * 256, 256), (), [bWh], "wh")
                DMA("pool", Wr[:, :, :], wview(w_min, OFF["gr"] + h * 256, 256), (), [bWh], "wh")
                if h == 3:
                    DMA("pool", WBpre[:, :, :], wview(w_glo, 0, D), (), [bWBpre], "bwbpre")
            for ci in range(4):
                c = 4 * tg + ci
                cs = slice(c * 128, (c + 1) * 128)
                ls = slice(ci * 128, (ci + 1) * 128)
                u = c % 2
                px, bpx = PS[u], bPS[u]
                MM1(px[:, 0:128], GAT[0:17, cs], ALP[0:17, h * 128:(h + 1) * 128], True, True, [bGAT, bALP], [bpx])
                ACT(TE[u], px[:, 0:128], AF.Exp, [bpx], [bTE[u]], scale=-1.0)
                ACT(LT[u], TE[u], AF.Ln, [bTE[u], bCF], [bLT[u]], bias=one_col, scale=1.0)
                pb, bpb = PS[2 + u], bPS[2 + u]
                MM1(pb[:, 0:128], LT[u], triu, True, True, [bLT[u], bCF], [bpb])
                MM1(pb[:, 128:256], tris, LT[u], True, True, [bLT[u], bCF], [bpb])
                ACT(EB[par][:, ls], pb[:, 0:128], AF.Exp, [bpb], [bEB[par]])
                ACT(ENB[par][:, ls], pb[:, 0:128], AF.Exp, [bpb], [bEB[par]], scale=-1.0)
                ACT(EREV[par][:, ci, :], pb[:, 128:256], AF.Exp, [bpb], [bEREV[par]])
                ACT(EBL[:, c:c + 1], pb[:, 127:128], AF.Exp, [bpb], [bEBL])
            pq, bpq = PS[4], bPS[4]
            pk, bpk = PS[5], bPS[5]
            MMG(pq[:, :], [(Wq[:, k, :], XN[:, k, tgs(tg)]) for k in range(8)], [bWh, bXN[tg]], [bpq])
            STT(QB[par], pq[:, :], 128.0 ** -0.5, EB[par], ALU.mult, ALU.mult, [bpq, bEB[par]], [bQB[par]])
            MMG(pk[:, :], [(Wk[:, k, :], XN[:, k, tgs(tg)]) for k in range(8)], [bWh, bXN[tg]], [bpk])
            TT("dve", KB[par], pk[:, :], ENB[par], ALU.mult, [bpk, bEB[par]], [bKB[par]])
            for ci in range(4):
                c = 4 * tg + ci
                cs = slice(c * 128, (c + 1) * 128)
                u = c % 2
                pk2, bpk2 = PS[u], bPS[u]
                MMG(pk2[:, 0:128], [(XN[:, k, cs], Wk[:, k, :]) for k in range(8)], [bWh, bXN[tg]], [bpk2])
                TT("dve", KD[par][:, ci, :], pk2[:, 0:128], EREV[par][:, ci, :], ALU.mult, [bpk2, bEREV[par]], [bKD[par]])
                pv, bpv = PS[2 + u], bPS[2 + u]
                MMG(pv[:, 0:256], [(XN[:, k, cs], Wv[:, k, :]) for k in range(8)], [bWh, bXN[tg]], [bpv])
                CP("act", V[par][:, ci, :], pv[:, 0:256], [bpv], [bV[par]])
                pr, bpr = PS[4 + u], bPS[4 + u]
                MMG(pr[:, 0:256], [(XN[:, k, cs], Wr[:, k, :]) for k in range(8)], [bWh, bXN[tg]], [bpr])
                ACT(GR[par][:, ci, :], pr[:, 0:256], AF.Silu, [bpr], [bGR[par]])

        def rec(h, tg, par):
            def cidx(ci):
                c = 4 * tg + ci
                return c, slice(c * 128, (c + 1) * 128), slice(ci * 128, (ci + 1) * 128), c % 2

            def stA(ci):
                c, cs, ls, u = cidx(ci)
                psc, bpsc = PS[u], bPS[u]
                MM1(psc[:, 128:256], KB[par][:, ls], QB[par][:, ls], True, True, [bKB[par], bQB[par]], [bpsc])
                TT("dve", PTm[u], psc[:, 128:256], caus, ALU.mult, [bpsc, bCB], [bPT[u]])

            def stB(ci):
                c, cs, ls, u = cidx(ci)
                if c < NT - 1:
                    pd, bpd = PS[4 + u], bPS[4 + u]
                    MM1(pd[:, 256:512], KD[par][:, ci, :], V[par][:, ci, :], True, True, [bKD[par], bV[par]], [bpd])
                    if c == 0:
                        CP("dve", ST, pd[:, 256:512], [bpd], [bST])
                    else:
                        STT(ST, ST, EBL[:, c:c + 1], pd[:, 256:512], ALU.mult, ALU.add, [bST, bEBL, bpd], [bST])
                    CP("act", SB[c % 2], ST, [bST], [bSB[c % 2]])

            def stC(ci):
                c, cs, ls, u = cidx(ci)
                po, bpo = PS[2 + u], bPS[2 + u]
                if c == 0:
                    MM1(po[:, 256:512], PTm[u], V[par][:, ci, :], True, True, [bPT[u], bV[par]], [bpo])
                else:
                    sp_ = (c - 1) % 2
                    MM1(po[:, 256:512], PTm[u], V[par][:, ci, :], True, False, [bPT[u], bV[par]], [bpo])
                    MM1(po[:, 256:512], QB[par][:, ls], SB[sp_], False, True, [bQB[par], bSB[sp_]], [bpo])
                ss, bss = SS[u], bSS[u]
                ACT(JK, po[:, 256:512], AF.Square, [bpo], [bJK, bss], accum=ss[:, 0:1])
                ACT(ss[:, 1:2], ss[:, 0:1], AF.Ln, [bss, bCF], [bss], bias=eps_col, scale=1.0 / 256)
                ACT(ss[:, 2:3], ss[:, 1:2], AF.Exp, [bss], [bss], scale=-0.5)
                STT(OTM[u], po[:, 256:512], ss[:, 2:3], GR[par][:, ci, :], ALU.mult, ALU.mult, [bpo, bss, bGR[par]], [bOTM[u]])

            def stD(ci):
                c, cs, ls, u = cidx(ci)
                TR(PSB[:, 0:128], OTM[u][:, 0:128], [bOTM[u], bCB], [bPSB])
                TR(PSB[:, 128:256], OTM[u][:, 128:256], [bOTM[u], bCB], [bPSB])
                for a_ in range(2):
                    TS("dve", OT[:, 2 * h + a_, cs], PSB[:, a_ * 128:(a_ + 1) * 128], VEC[:, V_GLA + 2 * h + a_:V_GLA + 2 * h + a_ + 1],
                       None, ALU.mult, None, [bPSB, bVEC], [bOT[tg]])

            for step in range(6):
                if step < 4:
                    stA(step)
                if 1 <= step <= 4:
                    stC(step - 1)
                if step < 4:
                    stB(step)
                if 2 <= step <= 5:
                    stD(step - 2)

        items = [(h, tg) for h in range(4) for tg in range(4)]
        prep(items[0][0], items[0][1], 0)
        for n, (h, tg) in enumerate(items):
            if n + 1 < len(items):
                prep(items[n + 1][0], items[n + 1][1], (n + 1) % 2)
            rec(h, tg, n % 2)
        P.barrier()
        AR.pos = mark
        bb = BranchBufs(w_glo, OFF["gates"], "g", pre=(WBpre, bWBpre))
        assert AR.pos + 8 * D + 8 * D + 8 * 512 + 2 * 512 <= 0 or True
        bb.load()
        for tg in range(4):
            branch_out_tg(bb, OT[:, :, tgs(tg)], bOT[tg], tg)
        P.barrier()

    def dsa():
        AR.reset()
        CKT = AR.take([S], BF16)
        CKA = AR.take([NT, 132], BF16)
        IKT = AR.take([S], BF16)
        WS = AR.take([NT, 8], F32)
        MT = AR.take([NT, 512], BF16)
        bMT = [Buf("MT%d" % j) for j in range(NT)]
        bCKT, bCKA, bIKT, bWS = Buf("CKT"), Buf("CKA"), Buf("IKT"), Buf("WS")
        mark2 = AR.pos
        WKV = AR.take([8, 128], BF16)
        WIK = AR.take([8, 128], BF16)
        WIW = AR.take([8, 8], BF16)
        bWsh = Buf("Wsh")
        DMA("pool", WKV[:, :, :], wview(w_min, OFF["dkv"], 128), (), [bWsh], "wsh")
        DMA("pool", WIK[:, :, 0:64], wview(w_min, OFF["ik"], 64), (), [bWsh], "wsh")
        DMA("pool", WIK[:, :, 64:128], wview(w_min, OFF["ik"], 64), (), [bWsh], "wsh")
        DMA("pool", WIW[:, :, :], wview(w_min, OFF["iw"], 8), (), [bWsh], "wsh")
        SQ = AR.take([512], BF16)
        RS = AR.take([512], F32)
        bSQ, bRS = Buf("dSQ"), Buf("dRS")
        MEMSET("pool", CKA[:, :, 128:132], 1.0, [bCKA])
        for tg in range(4):
            pc, bpc = PS[tg % 2], bPS[tg % 2]
            MMG(pc[:, :], [(WKV[:, k, :], XN[:, k, tgs(tg)]) for k in range(8)], [bWsh, bXN[tg]], [bpc])
            ACT(SQ, pc[:, :], AF.Square, [bpc], [bSQ])
            pn, bpn = PS[2], bPS[2]
            MM1(pn[:, :], ones, SQ, True, True, [bSQ, bCB], [bpn])
            ACT(RS, pn[:, :], AF.Ln, [bpn, bCF], [bRS], bias=eps_col, scale=1.0 / 128)
            ACT(RS, RS, AF.Exp, [bRS], [bRS], scale=-0.5)
            STT(CKT[:, tgs(tg)], pc[:, :], VEC[:, V_CKV:V_CKV + 1], RS, ALU.mult, ALU.mult, [bpc, bRS, bVEC], [bCKT])
            pi, bpi = PS[3], bPS[3]
            MMG(pi[:, :], [(WIK[:, k, :], XN[:, k, tgs(tg)]) for k in range(8)], [bWsh, bXN[tg]], [bpi])
            CP("act", IKT[:, tgs(tg)], pi[:, :], [bpi], [bIKT])
            for ti in range(4):
                c = tg * 4 + ti
                cs = slice(c * 128, (c + 1) * 128)
                TR(PSB[:, ti * 128:(ti + 1) * 128], CKT[:, cs], [bCKT, bCB], [bPSB])
            CP("dve", CKA[:, 4 * tg:4 * tg + 4, 0:128], PSB[:, 0:512].rearrange("p (a b) -> p a b", a=4), [bPSB], [bCKA])
            pw, bpw = PS[4], bPS[4]
            for ti in range(4):
                c = tg * 4 + ti
                cs = slice(c * 128, (c + 1) * 128)
                MMG(pw[:, ti * 8:ti * 8 + 8], [(XN[:, k, cs], WIW[:, k, :]) for k in range(8)], [bWsh, bXN[tg]], [bpw])
            TS("dve", WS[:, 4 * tg:4 * tg + 4, :], pw[:, 0:32].rearrange("p (a b) -> p a b", a=4),
               float((8 * 64) ** -0.5), None, ALU.mult, None, [bpw], [bWS])
        P.barrier()
        negtri = CF[:, CF_NEGTRI:CF_NEGTRI + 128]
        bPSBh = [Buf("PSBh0"), Buf("PSBh1")]
        trc = [0]
        for g in range(4):
            AR.pos = mark2
            IQ = AR.take([4, 512], BF16)
            bIQ = Buf("IQ")
            WIQ = [AR.take([8, 128], BF16) for _ in range(2)]
            bWIQ = [Buf("WIQ0"), Buf("WIQ1")]
            SC = [AR.take([S], F32) for _ in range(4)]
            bSC = [Buf("SC%d" % i_) for i_ in range(4)]
            RL = [AR.take([512], F32) for _ in range(2)]
            bRL = [Buf("RL0"), Buf("RL1")]
            M01 = [AR.take([S], BF16) for _ in range(2)]
            bM01 = [Buf("M010"), Buf("M011")]
            JNK = [AR.take([S], BF16) for _ in range(2)]
            bJ = [Buf("J%d" % i_) for i_ in range(4)]
            BI = [AR.take([8], F32) for _ in range(4)]
            bBI = [Buf("BI%d" % i_) for i_ in range(4)]
            WK2 = [AR.take([2 * NBIS + 2], F32) for _ in range(4)]
            bWK2 = [Buf("WK%d" % i_) for i_ in range(4)]
            for q in range(4):
                DMA("pool", WIQ[q % 2][:, :, :], wview(w_min, OFF["iq"] + q * 128, 128), (), [bWIQ[q % 2]], "wiq%d" % (q % 2))
                pq, bpq = PS[q % 2], bPS[q % 2]
                MMG(pq[:, :], [(WIQ[q % 2][:, k, :], XN[:, k, tgs(g)]) for k in range(8)], [bWIQ[q % 2], bXN[g]], [bpq])
                CP("act" if q % 2 else "dve", IQ[:, q, :], pq[:, :], [bpq], [bIQ])
            for j in range(4 * g, 4 * g + 4):
                MEMSET("pool", MT[:, j, :], 0.0, [bMT[j]])
            rc = 0
            for ti in range(4):
                i = 4 * g + ti
                n = (i + 1) * 128
                on_act = (ti % 2 == 1)
                sc, bsc = SC[ti], bSC[ti]
                nsg = (n + 511) // 512
                for h in range(8):
                    r0 = (h % 2) * 64
                    for sg_ in range(nsg):
                        s0 = sg_ * 512
                        w = min(512, n - s0)
                        pd, bpd = PS[2 + rc % 2], bPS[2 + rc % 2]
                        rl, brl = RL[rc % 2], bRL[rc % 2]
                        rc += 1
                        MM1(pd[:, 0:w], IQ[r0:r0 + 64, h // 2, ti * 128:(ti + 1) * 128], IKT[r0:r0 + 64, s0:s0 + w], True, True,
                            [bIQ, bIKT], [bpd])
                        ACT(rl[:, 0:w], pd[:, 0:w], AF.Relu, [bpd], [brl])
                        if h == 0:
                            TS("dve", sc[:, s0:s0 + w], rl[:, 0:w], WS[:, i, 0:1], None, ALU.mult, None, [brl, bWS], [bsc])
                        else:
                            STT(sc[:, s0:s0 + w], rl[:, 0:w], WS[:, i, h:h + 1], sc[:, s0:s0 + w], ALU.mult, ALU.add,
                                [brl, bWS, bsc], [bsc])
                bi, bbi = BI[ti], bBI[ti]
                wk, bwk = WK2[ti], bWK2[ti]
                if i >= 2:
                    P.add("dve", (lambda o, a: (lambda e: e.tensor_reduce(out=o, in_=a, axis=AX.X, op=ALU.max, apply_absolute_value=True)))(bi[:, 0:1], sc[:, 0:n]),
                          [bsc], [bbi])
                TT("dve", sc[:, n - 128:n], sc[:, n - 128:n], negtri, ALU.add, [bsc, bCF], [bsc])
                if i >= 2:
                    TS("dve", bi[:, 1:2], bi[:, 0:1], 1.0, None, ALU.add, None, [bbi], [bbi])
                    TS("dve", wk[:, 0:NBIS + 1], CF[:, CF_POW:CF_POW + NBIS + 1], bi[:, 1:2], None, ALU.mult, None, [bbi, bCF], [bwk])
                    MEMSET("dve", bi[:, 2:3], 0.0, [bbi])
                    if not on_act:
                        TS("dve", wk[:, NBIS + 1:2 * NBIS + 2], wk[:, 0:NBIS + 1], 2.0, None, ALU.mult, None, [bwk], [bwk])
                    else:
                        TS("dve", wk[:, NBIS + 1:2 * NBIS + 2], wk[:, 0:NBIS + 1], -1.0, None, ALU.mult, None, [bwk], [bwk])
                        MEMSET("dve", bi[:, 6:7], float(n - 511), [bbi])
            dts = [ti for ti in (0, 2) if 4 * g + ti >= 2]
            ats = [ti for ti in (1, 3) if 4 * g + ti >= 2]
            for k in range(NBIS):
                for ti in dts:
                    n = (4 * g + ti + 1) * 128
                    TS("dve", JNK[0][:, 0:n], SC[ti][:, 0:n], BI[ti][:, 2:3], 0.0, ALU.is_ge, ALU.add, [bSC[ti], bBI[ti]], [bJ[ti], bBI[ti]],
                       accum=BI[ti][:, 3:4])
                for ti in dts:
                    STT(BI[ti][:, 4:5], BI[ti][:, 3:4], 255.5, WK2[ti][:, NBIS + 1 + k:NBIS + 2 + k], ALU.is_ge, ALU.mult, [bBI[ti], bWK2[ti]], [bBI[ti]])
                for ti in dts:
                    STT(BI[ti][:, 2:3], BI[ti][:, 4:5], WK2[ti][:, k:k + 1], BI[ti][:, 2:3], ALU.subtract, ALU.add, [bBI[ti], bWK2[ti]], [bBI[ti]])
                for ti in ats:
                    n = (4 * g + ti + 1) * 128
                    ACT(JNK[1][:, 0:n], SC[ti][:, 0:n], AF.Sign, [bSC[ti], bBI[ti]], [bJ[ti], bBI[ti]], bias=BI[ti][:, 2:3], scale=1.0,
                        accum=BI[ti][:, 3:4])
                for ti in ats:
                    ACT(BI[ti][:, 4:5], BI[ti][:, 3:4], AF.Sign, [bBI[ti]], [bBI[ti]], bias=BI[ti][:, 6:7], scale=1.0)
                for ti in ats:
                    ACT(BI[ti][:, 2:3], BI[ti][:, 4:5], AF.Identity, [bBI[ti], bWK2[ti]], [bBI[ti]], bias=BI[ti][:, 2:3],
                        scale=WK2[ti][:, NBIS + 1 + k:NBIS + 2 + k])
            for ti in range(4):
                i = 4 * g + ti
                n = (i + 1) * 128
                u = ti % 2
                sc, bsc = SC[ti], bSC[ti]
                bi, bbi = BI[ti], bBI[ti]
                wk, bwk = WK2[ti], bWK2[ti]
                m01, bm01 = M01[u], bM01[u]
                if i >= 2 and u == 0:
                    TT("dve", bi[:, 5:6], bi[:, 2:3], wk[:, NBIS - 1:NBIS], ALU.subtract, [bbi, bwk], [bbi])
                elif i >= 2:
                    TS("dve", bi[:, 5:6], bi[:, 2:3], -1.0, wk[:, NBIS - 1:NBIS], ALU.mult, ALU.subtract, [bbi, bwk], [bbi])
                else:
                    MEMSET("dve", bi[:, 5:6], -1e29, [bbi])
                TS("dve", m01[:, 0:n], sc[:, 0:n], bi[:, 5:6], None, ALU.is_ge, None, [bsc, bbi], [bm01])
                for j0 in range(0, i + 1, 4):
                    nj = min(4, i + 1 - j0)
                    for jj in range(nj):
                        j = j0 + jj
                        TR(PSB[:, jj * 128:(jj + 1) * 128], m01[:, j * 128:(j + 1) * 128], [bm01, bCB], [bPSB])
                    CP("act", MT[:, j0:j0 + nj, ti * 128:(ti + 1) * 128],
                       PSB[:, 0:nj * 128].rearrange("p (a b) -> p a b", a=nj), [bPSB], [bMT[j] for j in range(j0, j0 + nj)])
            P.barrier()
            AR.pos = mark2
            WQ = [AR.take([8, 128], BF16) for _ in range(2)]
            bWQ = [Buf("WQ0"), Buf("WQ1")]
            QT = [AR.take([512], BF16) for _ in range(2)]
            bQT = [Buf("QT0"), Buf("QT1")]
            E = [AR.take([512], BF16) for _ in range(NE)]
            bE = [Buf("E%d" % i_) for i_ in range(NE)]
            RD = AR.take([8], F32)
            bRD = Buf("RD")
            ODM = [AR.take([128], BF16) for _ in range(4)]
            bODM = [Buf("ODM%d" % i_) for i_ in range(4)]
            OTg = AR.take([8, 512], BF16)
            bOTg = Buf("OTg")
            bb = BranchBufs(w_dso, OFF["gates"] + D, "d", stream_wo=True)
            MG = [AR.take([5, 256], BF16) for _ in range(2)]
            bMG = [Buf("MG0"), Buf("MG1")]
            nj_all = 4 * g + 4
            items = [(h, j) for h in range(8) for j in range(nj_all)]
            N = len(items)
            qbank = {}
            freeb = [0, 1, 2]
            bACC = [[Buf("ACC%d" % t_) for t_ in range(4)]] * 2

            def acc_ap(set_, ti):
                return PS[3 + ti][:, 0:129]

            def qload(h):
                s = h % 2
                DMA("pool", WQ[s][:, :, :], wview(w_min, OFF["dq"] + h * 128, 128), (), [bWQ[s]], "wq%d" % s)

            def qproj(h):
                s = h % 2
                b_ = freeb.pop(0)
                MMG(PS[b_][:, :], [(WQ[s][:, k, :], XN[:, k, tgs(g)]) for k in range(8)], [bWQ[s], bXN[g]], [bPS[b_]])
                TS("dve", QT[s], PS[b_][:, :], 128.0 ** -0.5, None, ALU.mult, None, [bPS[b_]], [bQT[s]])
                freeb.append(b_)

            def qk(n_):
                h, j = items[n_]
                b_ = freeb.pop(0)
                qbank[n_] = b_
                c0 = max(0, 128 * (j - 4 * g))
                MM1(PS[b_][:, c0:512], CKT[:, j * 128:(j + 1) * 128], QT[h % 2][:, c0:512], True, True, [bCKT, bQT[h % 2]], [bPS[b_]])

            def mid(n_):
                h, j = items[n_]
                b_ = qbank[n_]
                e, be = E[n_ % NE], bE[n_ % NE]
                r = j - 4 * g
                c0 = max(0, 128 * r)
                ACT(e[:, c0:512], PS[b_][:, c0:512], AF.Exp, [bPS[b_], bC31], [be], bias=C31[:, h:h + 1], scale=1.0)
                lo = max(0, 128 * r)
                hi = min(512, 128 * r + 256)
                if hi > lo:
                    TT("dve", e[:, lo:hi], e[:, lo:hi], MG[h % 2][:, r + 1, 0:hi - lo], ALU.mult, [be, bMG[h % 2]], [be])
                    if hi < 512:
                        TT("dve", e[:, hi:512], e[:, hi:512], MT[:, j, hi:512], ALU.mult, [be, bMT[j]], [be])
                else:
                    TT("dve", e[:, c0:512], e[:, c0:512], MT[:, j, c0:512], ALU.mult, [be, bMT[j]], [be])
                freeb.append(b_)

            def pv(n_):
                h, j = items[n_]
                e, be = E[n_ % NE], bE[n_ % NE]
                for ti in range(4):
                    i = 4 * g + ti
                    if i < j:
                        continue
                    MM1(acc_ap(h % 2, ti), e[:, ti * 128:(ti + 1) * 128], CKA[:, j, 0:129], j == 0, j == i,
                        [be, bCKA], [bACC[h % 2][ti]])

            def fin_a(h):
                for ti in range(4):
                    pa = acc_ap(h % 2, ti)
                    P.add("dve", (lambda o, a: (lambda e: e.reciprocal(out=o, in_=a)))(RD[:, ti:ti + 1], pa[:, 128:129]), [bACC[h % 2][ti]], [bRD])
                for ti in range(4):
                    pa = acc_ap(h % 2, ti)
                    if ti < 2:
                        TS("dve", ODM[ti], pa[:, 0:128], RD[:, ti:ti + 1], None, ALU.mult, None, [bACC[h % 2][ti], bRD], [bODM[ti]])
                    else:
                        ACT(ODM[ti], pa[:, 0:128], AF.Identity, [bACC[h % 2][ti], bRD], [bODM[ti]], scale=RD[:, ti:ti + 1])

            def fin_b(h):
                for ti in range(4):
                    TR(PSB[:, 512 + ti * 128:512 + (ti + 1) * 128], ODM[ti], [bODM[ti], bCB], [bPSB])
                CP("act", OTg[:, h, :], PSB[:, 512:1024], [bPSB], [bOTg])

            def mk_mg(h):
                for r in range(-1, 4):
                    j = 4 * g + r
                    if j < 0:
                        continue
                    lo = max(0, 128 * r)
                    hi = min(512, 128 * r + 256)
                    x0 = lo - 128 * r
                    TT("pool", MG[h % 2][:, r + 1, 0:hi - lo], MT[:, j, lo:hi], G[:, h, x0:x0 + (hi - lo)], ALU.mult,
                       [bMT[j], bG], [bMG[h % 2]])

            qload(0)
            qload(1)
            mk_mg(0)
            def warm(e):
                ins = None
                for w_ in range(NWARM):
                    ins = e.matmul(PS[0][:, :], ones, XN[:, w_ % 8, tgs(g)], start=True, stop=True)
                return ins
            P.add("pe", warm, [bCB, bXN[g]], [bPS[0]])
            qproj(0)
            qk(0)
            if N > 1:
                qk(1)
            bb.load()
            pend = []
            for n_ in range(N):
                h, j = items[n_]
                if n_ + 2 < N:
                    qk(n_ + 2)
                mid(n_)
                if j == min(nj_all // 2, nj_all - 3) and h + 1 < 8:
                    qproj(h + 1)
                if j == 0 and h + 1 < 8:
                    mk_mg(h + 1)
                    if h >= 1:
                        qload(h + 1)
                pv(n_)
                for pp_ in list(pend):
                    if n_ >= pp_[0]:
                        fin_b(pp_[1])
                        pend.remove(pp_)
                if j == nj_all - 1:
                    fin_a(h)
                    pend.append((n_ + 2, h))
            for pp_ in pend:
                fin_b(pp_[1])
            branch_out_tg(bb, OTg, bOTg, g)
            P.barrier()

    def ple():
        AR.reset()
        norm_to_xn(V_PLE)
        WG = AR.take([8, D], BF16)
        WP = AR.take([2, D], BF16)
        PTt = AR.take([2, S], BF16)
        SGf = [AR.take([512], F32) for _ in range(2)]
        TMP = [AR.take([512], F32) for _ in range(2)]
        bWG, bWP, bPT = Buf("pWG"), Buf("pWP"), Buf("pPT")
        bSGf = [Buf("pSG0"), Buf("pSG1")]
        bTMP = [Buf("pT0"), Buf("pT1")]
        DMA("pool", WG[:, :, :], wview(w_pg, 0, D), (), [bWG], "pwg")
        DMA("pool", WP[:, :, :], wview(w_pp, 0, D), (), [bWP], "pwp")
        DMA("pool", PTt[:, :, :], pT.rearrange("(k p) t -> p k t", p=128), (), [bPT], "ppt")
        c = 0
        for tg in range(4):
            for dc in range(8):
                pg, bpg = PS[(2 * c) % 4], bPS[(2 * c) % 4]
                pp, bpp = PS[(2 * c + 1) % 4], bPS[(2 * c + 1) % 4]
                u = c % 2
                c += 1
                dcs = slice(dc * 128, (dc + 1) * 128)
                MMG(pg[:, :], [(WG[:, k, dcs], XN[:, k, tgs(tg)]) for k in range(8)], [bWG, bXN[tg]], [bpg])
                MMG(pp[:, :], [(WP[:, k, dcs], PTt[:, k, tgs(tg)]) for k in range(2)], [bWP, bPT], [bpp])
                ACT(SGf[u], pg[:, :], AF.Sigmoid, [bpg], [bSGf[u]])
                TT("dve", TMP[u], SGf[u], pp[:, :], ALU.mult, [bSGf[u], bpp], [bTMP[u]])
                TT("pool", H[:, dc, tgs(tg)], H[:, dc, tgs(tg)], TMP[u], ALU.add, [bTMP[u], bH[dc][tg]], [bH[dc][tg]])
        P.barrier()

    if "ffn1" in stages:
        ffn(w_f1i, w_f1o, V_FFN1, "f1")
    if "gla" in stages or "dsa" in stages:
        AR.reset()
        norm_to_xn(V_MIX)
        build_G()
        P.barrier()
    if "gla" in stages:
        gla()
    if "dsa" in stages:
        dsa()
    if "ffn2" in stages:
        ffn(w_f2i, w_f2o, V_FFN2, "f2")
    if "ple" in stages:
        ple()
    AR.reset()
    OB = [AR.take([8, 512], F32) for _ in range(2)]
    bOB = [Buf("OB0"), Buf("OB1")]
    yv = yT.rearrange("(k p) t -> p k t", p=128)
    outs = []
    rmsnorm(V_FIN, lambda k, tg: OB[tg % 2][:, k, :], lambda tg: [bOB[tg % 2]],
            after=lambda tg: outs.append(DMA("sp", yv[:, :, tgs(tg)], OB[tg % 2], [bOB[tg % 2]], (), "out%d" % (tg % 2))))
    P.add("sp", None, extra=outs)

    streams = P.finalize()
    sems = {}
    for st in streams:
        sems[st] = es.enter_context(nc.semaphore("s_" + st.replace(":", "_")))
    block = es.enter_context(nc.Block())

    @block.tensor
    def _(e):
        P.emit("pe", e, sems)

    @block.scalar
    def _(e):
        P.emit("act", e, sems)

    @block.vector
    def _(e):
        P.emit("dve", e, sems)

    @block.gpsimd
    def _(e):
        P.emit("pool", e, sems)

    @block.sync
    def _(e):
        P.emit("sp", e, sems)

    es.close()
    return nc


def make_in_maps(inp):
    f = lambda a: np.ascontiguousarray(np.asarray(a, dtype=np.float32))
    cf, cb = make_consts()
    vec = np.zeros((128, NV), np.float32)
    for col, name in ((V_FFN1, "ffn1_norm"), (V_MIX, "mix_norm"), (V_FFN2, "ffn2_norm"), (V_PLE, "ple_norm")):
        vec[:, col:col + 8] = f(inp[name])[0].reshape(8, 128).T
    vec[:, V_FIN:V_FIN + 8] = f(inp["final_norm"]).reshape(8, 128).T
    vec[:, V_CKV] = f(inp["ckv_norm"])[0]
    vec[:, V_GLA:V_GLA + 8] = f(inp["gla_out_norm"])[0].reshape(8, 128).T
    alpha = np.concatenate([f(inp["gla_alpha_w"])[0], f(inp["gla_alpha_b"])[0][None, :]], axis=0)
    shared = {
        "vecs": vec, "cstf": cf, "cstb": cb,
        "ffn1_w_in": f(inp["ffn1_w_in"])[0], "ffn1_w_out": f(inp["ffn1_w_out"])[0],
        "ffn2_w_in": f(inp["ffn2_w_in"])[0], "ffn2_w_out": f(inp["ffn2_w_out"])[0],
        "mix_w_in": f(inp["mix_w_in"])[0], "alpha_aug": f(alpha),
        "gla_w_out": f(inp["gla_w_out"])[0],
        "dsa_w_out": f(inp["dsa_w_out"])[0], "rel_bias": f(inp["rel_bias"]), "rel_biasT": f(np.asarray(inp["rel_bias"]).T),
        "mix_w_out": f(inp["mix_w_out"])[0], "ple_w_gate": f(inp["ple_w_gate"])[0],
        "ple_w_proj": f(inp["ple_w_proj"])[0],
    }
    x = f(inp["x"])
    p = f(inp["p"])
    maps = []
    for b in range(8):
        m = dict(shared)
        m["xT"] = np.ascontiguousarray(x[b].T)
        m["pT"] = np.ascontiguousarray(p[0, b].T)
        maps.append(m)
    return maps


def kernel(**inputs):
    nc = build_nc()
    maps = make_in_maps(inputs)
    res = run_bass_kernel_spmd(nc, maps, core_ids=list(range(8)))
    out = np.stack([np.ascontiguousarray(res.results[b]["yT"].T) for b in range(8)], axis=0)
    return out.astype(np.float32)
```

```python
import math
from contextlib import ExitStack
import numpy as np
import concourse.bass as bass
import concourse.mybir as mybir
from concourse.bass_utils import run_bass_kernel_spmd

F32 = mybir.dt.float32
BF16 = mybir.dt.bfloat16
U32 = mybir.dt.uint32
AF = mybir.ActivationFunctionType
ALU = mybir.AluOpType
AX = mybir.AxisListType

D = 1024
S = 2048
NT = 16
FF = 2816
NF = 22
EPS = 1e-6
OFF = dict(gq=0, gk=512, gv=1024, gr=2048, ga=3072, dq=3088, dkv=4112, iq=4240, ik=4752, iw=4816, gates=4824)
D_IN = 6872
NBIS = 16
ALL_DVE = False
NE = 3
NWARM = 8


class Buf:
    __slots__ = ("name", "w", "r")

    def __init__(self, name):
        self.name = name
        self.w = {}
        self.r = {}


class Op:
    __slots__ = ("stream", "eng", "fn", "deps", "sig", "val", "idx")


class Prog:
    CE = ("pe", "act", "dve", "pool")
    ALL = ("pe", "act", "dve", "pool", "sp")

    def __init__(self):
        self.q = {e: [] for e in self.ALL}
        self.last_dma = {}

    def add(self, eng, fn, reads=(), writes=(), key=None, extra=()):
        stream = eng if key is None else "dma:" + key
        op = Op()
        op.stream, op.eng, op.fn, op.sig, op.val = stream, eng, fn, False, None
        op.idx = len(self.q[eng])
        raw = set()
        oth = set(extra)
        for b in reads:
            raw.update(b.w.values())
        for b in writes:
            oth.update(b.w.values())
            oth.update(b.r.values())
        deps = []
        for d in raw | oth:
            if d is op:
                continue
            if key is None and d.stream == stream:
                if eng == "pe":
                    continue
                if d in raw and op.idx - d.idx <= 2:
                    deps.append(d)
                continue
            deps.append(d)
        for d in deps:
            d.sig = True
        op.deps = deps
        for b in reads:
            b.r[stream] = op
        for b in writes:
            b.r = {}
            b.w[stream] = op
        self.q[eng].append(op)
        if key is not None:
            self.last_dma[stream] = op
        return op

    def barrier(self, exclude=()):
        lasts = []
        for e in self.CE:
            for op in reversed(self.q[e]):
                if op.fn is not None and op.stream == e:
                    lasts.append(op)
                    break
        dl = [op for st, op in self.last_dma.items() if st not in exclude]
        for e in self.ALL:
            self.add(e, None, extra=[l for l in lasts if l.stream != e] + dl)

    def finalize(self):
        cnt = {}
        for e in self.ALL:
            for op in self.q[e]:
                if op.fn is None:
                    continue
                if op.stream.startswith("dma:"):
                    cnt[op.stream] = cnt.get(op.stream, 0) + 16
                    op.val = cnt[op.stream]
                elif op.sig:
                    cnt[op.stream] = cnt.get(op.stream, 0) + 1
                    op.val = cnt[op.stream]
        return sorted(cnt.keys())

    def emit(self, eng, e, sems):
        known = {}
        for op in self.q[eng]:
            need = {}
            for d in op.deps:
                assert d.val is not None
                if known.get(d.stream, 0) < d.val and need.get(d.stream, 0) < d.val:
                    need[d.stream] = d.val
            for st, v in need.items():
                e.wait_ge(sems[st], v)
                known[st] = v
            if op.fn is not None:
                ins = op.fn(e)
                if op.val is not None:
                    ins.then_inc(sems[op.stream], 16 if op.stream.startswith("dma:") else 1)


class Arena:
    def __init__(self, ap, n):
        self.ap, self.n, self.pos = ap, n, 0

    def reset(self):
        self.pos = 0

    def take(self, shape, dt):
        ne = int(np.prod(shape))
        nb = ne * (2 if dt == F32 or dt == U32 else 1)
        nb = (nb + 1) // 2 * 2
        assert self.pos + nb <= self.n, ("arena overflow", self.pos, nb, self.n)
        v = self.ap[:, self.pos:self.pos + nb]
        self.pos += nb
        if dt != BF16:
            v = v.bitcast(dt)
        if len(shape) == 2:
            v = v.rearrange("p (a b) -> p a b", a=shape[0])
        elif len(shape) == 3:
            v = v.rearrange("p (a b c) -> p a b c", a=shape[0], b=shape[1])
        return v


def t5_bucket_np(d):
    d = np.maximum(d, 0)
    ratio = (np.maximum(d, 1).astype(np.float32) / np.float32(16))
    large = 16 + (np.log(ratio).astype(np.float32) / np.float32(math.log(128 / 16)) * np.float32(16)).astype(np.int32)
    large = np.minimum(large, 31)
    return np.where(d < 16, d, large)


CF_TRIU, CF_TRIS, CF_NEGTRI, CF_POW, CF_MISC = 0, 128, 256, 384, 416
NCF = 432
CB_ONES, CB_ID, CB_CAUS = 0, 128, 256
NCB = 384


def make_consts():
    cf = np.zeros((128, NCF), np.float32)
    i = np.arange(128)
    cf[:, CF_TRIU:CF_TRIU + 128] = np.where(i[:, None] <= i[None, :], -1.0 / 16, 0.0)
    cf[:, CF_TRIS:CF_TRIS + 128] = np.where(i[:, None] > i[None, :], -1.0 / 16, 0.0)
    cf[:, CF_NEGTRI:CF_NEGTRI + 128] = np.where(i[None, :] <= i[:, None], 0.0, -1e30)
    for k in range(32):
        cf[:, CF_POW + k] = 2.0 ** (-(k + 1))
    cf[:, CF_MISC + 0] = EPS
    cf[:, CF_MISC + 1] = 1.0
    cf[:, CF_MISC + 2] = 0.0
    cb = np.zeros((128, NCB), np.float32)
    cb[:, CB_ONES:CB_ONES + 128] = 1.0
    cb[:, CB_ID:CB_ID + 128] = np.eye(128)
    cb[:, CB_CAUS:CB_CAUS + 128] = np.where(i[:, None] <= i[None, :], 1.0, 0.0)
    return cf, cb


V_FFN1, V_MIX, V_FFN2, V_PLE, V_FIN, V_CKV, V_GLA = 0, 8, 16, 24, 32, 40, 41
NV = 52


def build_nc(stages=("ffn1", "gla", "dsa", "ffn2", "ple")):
    nc = bass.Bass("TRN2", target_bir_lowering=False)

    def din(name, shape):
        return nc.dram_tensor(name, list(shape), F32, kind="ExternalInput").ap()

    xT = din("xT", [D, S])
    pT = din("pT", [256, S])
    vecs = din("vecs", [128, NV])
    cstf = din("cstf", [128, NCF])
    cstb = din("cstb", [128, NCB])
    w_f1i = din("ffn1_w_in", [D, 2 * FF])
    w_f1o = din("ffn1_w_out", [FF, D])
    w_f2i = din("ffn2_w_in", [D, 2 * FF])
    w_f2o = din("ffn2_w_out", [FF, D])
    w_min = din("mix_w_in", [D, D_IN])
    alpha = din("alpha_aug", [17, 512])
    w_glo = din("gla_w_out", [D, D])
    w_dso = din("dsa_w_out", [D, D])
    relb = din("rel_bias", [32, 8])
    relbT = din("rel_biasT", [8, 32])
    w_mo = din("mix_w_out", [D, D])
    w_pg = din("ple_w_gate", [D, D])
    w_pp = din("ple_w_proj", [256, D])
    yT = nc.dram_tensor("yT", [D, S], F32, kind="ExternalOutput").ap()
    scrA = nc.dram_tensor("scrA", [8, 384], F32, kind="Internal").ap()

    P = Prog()
    es = ExitStack()
    ARN = 53700
    H = es.enter_context(nc.sbuf_tensor("H", [128, 8, S], F32))
    XN = es.enter_context(nc.sbuf_tensor("XN", [128, 8, S], BF16))
    CF = es.enter_context(nc.sbuf_tensor("CF", [128, NCF], F32))
    CB = es.enter_context(nc.sbuf_tensor("CB", [128, NCB], BF16))
    VEC = es.enter_context(nc.sbuf_tensor("VEC", [128, NV], F32))
    ARt = es.enter_context(nc.sbuf_tensor("AR", [128, ARN], BF16))
    PS = [es.enter_context(nc.psum_tensor("ps%d" % i, [128, 512], F32)) for i in range(7)]
    PSB = es.enter_context(nc.psum_tensor("psb", [128, 1024], BF16))
    AR = Arena(ARt, ARN)

    bH = [[Buf("H%d_%d" % (k, tg)) for tg in range(4)] for k in range(8)]
    bXN = [Buf("XN%d" % tg) for tg in range(4)]
    bPS = [Buf("PS%d" % i) for i in range(7)]
    bPSB = Buf("PSB")
    bCF, bCB, bVEC = Buf("CF"), Buf("CB"), Buf("VEC")

    ones = CB[:, CB_ONES:CB_ONES + 128]
    ident = CB[:, CB_ID:CB_ID + 128]
    caus = CB[:, CB_CAUS:CB_CAUS + 128]
    eps_col = CF[:, CF_MISC:CF_MISC + 1]

    def tgs(tg):
        return slice(tg * 512, (tg + 1) * 512)

    def DMA(q, out, in_, reads, writes, key):
        return P.add(q, lambda e: e.dma_start(out=out, in_=in_), reads, writes, key=key)

    def MMG(out, pairs, reads, writes):
        def fn(e):
            n = len(pairs)
            ins = None
            for i, (l, r) in enumerate(pairs):
                ins = e.matmul(out, l, r, start=(i == 0), stop=(i == n - 1))
            return ins
        return P.add("pe", fn, reads, writes)

    def MM1(out, l, r, start, stop, reads, writes):
        return P.add("pe", lambda e: e.matmul(out, l, r, start=start, stop=stop), reads, writes)

    def TR(out, in_, reads, writes):
        return P.add("pe", lambda e: e.transpose(out, in_, ident), reads, writes)

    def ACT(out, in_, func, reads, writes, bias=None, scale=None, accum=None):
        kw = {}
        if bias is not None:
            kw["bias"] = bias
        if scale is not None:
            kw["scale"] = scale
        if accum is not None:
            kw["accum_out"] = accum
        return P.add("act", lambda e: e.activation(out=out, in_=in_, func=func, **kw), reads, writes)

    def TT(eng, out, a, b, op, reads, writes):
        return P.add(eng, lambda e: e.tensor_tensor(out=out, in0=a, in1=b, op=op), reads, writes)

    def STT(out, a, sc, b, op0, op1, reads, writes):
        return P.add("dve", lambda e: e.scalar_tensor_tensor(out=out, in0=a, scalar=sc, in1=b, op0=op0, op1=op1),
                     reads, writes)

    def TS(eng, out, a, s1, s2, op0, op1, reads, writes, accum=None):
        kw = {}
        if accum is not None:
            kw["accum_out"] = accum
        if op1 is None:
            return P.add(eng, lambda e: e.tensor_scalar(out=out, in0=a, scalar1=s1, scalar2=None, op0=op0, **kw),
                         reads, writes)
        return P.add(eng, lambda e: e.tensor_scalar(out=out, in0=a, scalar1=s1, scalar2=s2, op0=op0, op1=op1, **kw),
                     reads, writes)

    def CP(eng, out, in_, reads, writes):
        if eng == "act":
            return P.add("act", lambda e: e.copy(out=out, in_=in_), reads, writes)
        return P.add(eng, lambda e: e.tensor_copy(out=out, in_=in_), reads, writes)

    def MEMSET(eng, out, val, writes):
        return P.add(eng, lambda e: e.memset(out, val), (), writes)

    def wview(w, c0, n):
        return w[:, c0:c0 + n].rearrange("(k p) n -> p k n", p=128)

    xv_ = xT.rearrange("(k p) t -> p k t", p=128)
    for tg_ in range(4):
        DMA("sp", H[:, :, tg_ * 512:(tg_ + 1) * 512], xv_[:, :, tg_ * 512:(tg_ + 1) * 512], (), [bH[k_][tg_] for k_ in range(8)], "x%d" % tg_)
    DMA("sp", CF[:, :], cstf, (), [bCF], "cf")
    DMA("sp", VEC[:, :], vecs, (), [bVEC], "vec")
    DMA("pool", CB[:, :], cstb, (), [bCB], "cb")

    G = es.enter_context(nc.sbuf_tensor("G", [128, 8, 256], BF16))
    C31 = es.enter_context(nc.sbuf_tensor("C31", [128, 8], F32))
    NC31 = es.enter_context(nc.sbuf_tensor("NC31", [128, 8], F32))
    bG, bC31 = Buf("G"), Buf("C31")
    top = ARN - 4096 - 64 - 768
    TB = ARt[:, top:top + 4096].bitcast(F32).rearrange("p (a b) -> p a b", a=8)
    RBT = ARt[:, top + 4096:top + 4160].bitcast(F32)
    ASB = ARt[:, top + 4160:top + 4928].bitcast(F32)
    bTB, bRBT, bASB, bscr = Buf("TB"), Buf("RBT"), Buf("ASB"), Buf("scrA")
    kk_ = np.arange(384)
    bk_ = t5_bucket_np(kk_ - 127)
    DMA("sp", RBT[0:8, :], relbT, (), [bRBT], "rbt")
    DMA("sp", C31[:, :], relb[31:32, :].to_broadcast([128, 8]), (), [bC31], "c31")
    for b in range(32):
        idx = np.nonzero(bk_ == b)[0]
        if len(idx) == 0:
            continue
        k0, k1 = int(idx[0]), int(idx[-1]) + 1
        assert np.all(bk_[k0:k1] == b)
        CP("dve", ASB[0:8, k0:k1], RBT[0:8, b:b + 1].to_broadcast([8, k1 - k0]), [bRBT], [bASB])
    DMA("sp", scrA[:, :], ASB[0:8, :], [bASB], [bscr], "scr")
    for ss in range(128):
        DMA("sp", TB[ss:ss + 1, :, :], scrA[:, 127 - ss:127 - ss + 256].rearrange("(a h) x -> a h x", a=1), [bscr], [bTB], "tb")
    TS("dve", NC31[:, :], C31[:, :], -1.0, None, ALU.mult, None, [bC31], [bC31])

    def build_G():
        for h in range(8):
            ACT(G[:, h, :], TB[:, h, :], AF.Exp, [bTB, bC31], [bG], bias=NC31[:, h:h + 1], scale=1.0)

    def rmsnorm(gcol, out_fn, out_bufs_fn, after=None):
        SQ = AR.take([8, 512], BF16)
        RS = AR.take([512], F32)
        bSQ, bRS = Buf("SQ"), Buf("RS")
        for tg in range(4):
            hb = [bH[k][tg] for k in range(8)]
            ACT(SQ, H[:, :, tgs(tg)], AF.Square, hb, [bSQ])
            ps, bps = PS[6], bPS[6]
            MMG(ps[:, :], [(ones, SQ[:, k, :]) for k in range(8)], [bSQ, bCB], [bps])
            ACT(RS, ps[:, :], AF.Ln, [bps, bCF], [bRS], bias=eps_col, scale=1.0 / D)
            ACT(RS, RS, AF.Exp, [bRS], [bRS], scale=-0.5)
            for k in range(8):
                STT(out_fn(k, tg), H[:, k, tgs(tg)], VEC[:, gcol + k:gcol + k + 1], RS, ALU.mult, ALU.mult,
                    [bH[k][tg], bRS, bVEC], out_bufs_fn(tg))
            if after is not None:
                after(tg)

    def norm_to_xn(gcol):
        rmsnorm(gcol, lambda k, tg: XN[:, k, tgs(tg)], lambda tg: [bXN[tg]])

    def ffn(w_in, w_out, gcol, tag):
        AR.reset()
        norm_to_xn(gcol)
        A = AR.take([6, S], BF16)
        W1 = [AR.take([8, 256], BF16) for _ in range(3)]
        W2 = [AR.take([6, D], BF16) for _ in range(2)]
        SG = [AR.take([512], BF16) for _ in range(2)]
        bA = [[Buf("A%d_%d" % (f, tg)) for tg in range(4)] for f in range(6)]
        bW1 = [Buf("W1_%d" % i) for i in range(3)]
        bW2 = [Buf("W2_%d" % i) for i in range(2)]
        bSG = [Buf("SG0"), Buf("SG1")]
        groups = [(0, 6), (6, 12), (12, 17), (17, 22)]
        c1 = 0
        c2 = 0
        for gi, (f0, f1) in enumerate(groups):
            nfg = f1 - f0
            w2 = W2[gi % 2]
            for fi in range(f0, f1):
                s = fi % 3
                DMA("pool", W1[s][:, :, 0:128], wview(w_in, fi * 128, 128), (), [bW1[s]], "w1_%d" % s)
                DMA("pool", W1[s][:, :, 128:256], wview(w_in, FF + fi * 128, 128), (), [bW1[s]], "w1_%d" % s)
                if fi == f0:
                    DMA("pool", w2[:, 0:nfg, :], w_out[f0 * 128:f1 * 128, :].rearrange("(f p) n -> p f n", p=128),
                        (), [bW2[gi % 2]], "w2_%d" % (gi % 2))
                for tg in range(4):
                    pg, pu = PS[(2 * c1) % 4], PS[(2 * c1 + 1) % 4]
                    bpg, bpu = bPS[(2 * c1) % 4], bPS[(2 * c1 + 1) % 4]
                    sg, bsg = SG[c1 % 2], bSG[c1 % 2]
                    c1 += 1
                    MMG(pg[:, :], [(W1[s][:, k, 0:128], XN[:, k, tgs(tg)]) for k in range(8)], [bW1[s], bXN[tg]], [bpg])
                    MMG(pu[:, :], [(W1[s][:, k, 128:256], XN[:, k, tgs(tg)]) for k in range(8)], [bW1[s], bXN[tg]], [bpu])
                    ACT(sg, pg[:, :], AF.Silu, [bpg], [bsg])
                    TT("dve", A[:, fi - f0, tgs(tg)], sg, pu[:, :], ALU.mult, [bsg, bpu], [bA[fi - f0][tg]])
            for dc in range(8):
                for tg in range(4):
                    po, bpo = PS[4 + c2 % 2], bPS[4 + c2 % 2]
                    c2 += 1
                    MMG(po[:, :], [(w2[:, f, dc * 128:(dc + 1) * 128], A[:, f, tgs(tg)]) for f in range(nfg)],
                        [bW2[gi % 2]] + [bA[f][tg] for f in range(nfg)], [bpo])
                    STT(H[:, dc, tgs(tg)], po[:, :], 0.5, H[:, dc, tgs(tg)], ALU.mult, ALU.add,
                        [bpo, bH[dc][tg]], [bH[dc][tg]])
        P.barrier()

    class BranchBufs:
        def __init__(self, w_branch, gate_col0, tag, stream_wo=False, pre=None):
            self.pre = pre
            self.WB = AR.take([8, D], BF16) if pre is None else pre[0]
            self.WG = AR.take([8, D], BF16)
            self.stream_wo = stream_wo
            if stream_wo:
                self.WOc = [AR.take([8, 128], BF16) for _ in range(2)]
                self.bWOc = [Buf("WOc0"), Buf("WOc1")]
            else:
                self.WO = AR.take([8, D], BF16)
            self.M = AR.take([8, 512], BF16)
            self.SG = [AR.take([512], BF16) for _ in range(2)]
            self.bWB, self.bWG, self.bWO = Buf("WB"), Buf("WG"), Buf("WO")
            if pre is not None:
                self.bWB = pre[1]
            self.bM = Buf("M")
            self.bSG = [Buf("bSG0"), Buf("bSG1")]
            self.c = 0
            self.wo_pre = 0
            self.args = (w_branch, gate_col0, tag)

        def load(self):
            w_branch, gate_col0, tag = self.args
            if self.pre is None:
                DMA("pool", self.WB[:, :, :], wview(w_branch, 0, D), (), [self.bWB], "bwb" + tag)
            if self.stream_wo:
                DMA("pool", self.WG[:, :, :], wview(w_min, gate_col0, D), (), [self.bWG], "bwg" + tag)
                self.bWGc = [self.bWG] * 8
            else:
                self.bWGc = [Buf("WGc%d" % dc_) for dc_ in range(8)]
                for dc_ in range(8):
                    DMA("pool", self.WG[:, :, dc_ * 128:(dc_ + 1) * 128], wview(w_min, gate_col0 + dc_ * 128, 128), (),
                        [self.bWGc[dc_]], "bwgc%d%s" % (dc_, tag))
            if not self.stream_wo:
                DMA("pool", self.WO[:, :, :], wview(w_mo, 0, D), (), [self.bWO], "bwo" + tag)
            else:
                assert self.c == 0
                for dc_ in range(2):
                    DMA("pool", self.WOc[dc_][:, :, :], wview(w_mo, dc_ * 128, 128), (), [self.bWOc[dc_]], "bwoc%d" % dc_)
                self.wo_pre = 2

    def branch_out_tg(bb, OTv, bOTv, tg):
        for dc in range(8):
            c = bb.c
            bb.c += 1
            s = c % 2
            dcs = slice(dc * 128, (dc + 1) * 128)
            py, bpy = PS[(2 * c) % 4], bPS[(2 * c) % 4]
            pg, bpg = PS[(2 * c + 1) % 4], bPS[(2 * c + 1) % 4]
            MMG(py[:, :], [(bb.WB[:, k, dcs], OTv[:, k, :]) for k in range(8)], [bb.bWB, bOTv], [bpy])
            MMG(pg[:, :], [(bb.WG[:, k, dcs], XN[:, k, tgs(tg)]) for k in range(8)], [bb.bWGc[dc], bXN[tg]], [bpg])
            ACT(bb.SG[s], pg[:, :], AF.Sigmoid, [bpg], [bb.bSG[s]])
            TT("dve", bb.M[:, dc, :], bb.SG[s], py[:, :], ALU.mult, [bb.bSG[s], bpy], [bb.bM])
        for dc in range(8):
            c = bb.c
            bb.c += 1
            s = c % 2
            dcs = slice(dc * 128, (dc + 1) * 128)
            po, bpo = PS[4 + s], bPS[4 + s]
            if bb.stream_wo:
                assert s == dc % 2
                if dc >= bb.wo_pre:
                    DMA("pool", bb.WOc[s][:, :, :], wview(w_mo, dc * 128, 128), (), [bb.bWOc[s]], "bwoc%d" % s)
                MMG(po[:, :], [(bb.WOc[s][:, k, :], bb.M[:, k, :]) for k in range(8)], [bb.bWOc[s], bb.bM], [bpo])
            else:
                MMG(po[:, :], [(bb.WO[:, k, dcs], bb.M[:, k, :]) for k in range(8)], [bb.bWO, bb.bM], [bpo])
            TT("dve", H[:, dc, tgs(tg)], po[:, :], H[:, dc, tgs(tg)], ALU.add, [bpo, bH[dc][tg]], [bH[dc][tg]])

    def gla():
        AR.reset()
        OT = AR.take([8, S], BF16)
        bOT = [Buf("OT%d" % tg) for tg in range(4)]
        mark = AR.pos
        GAT = AR.take([S], BF16)
        ALP = AR.take([512], BF16)
        WA = AR.take([8, 16], BF16)
        bGAT, bALP, bWA = Buf("GAT"), Buf("ALP"), Buf("WA")
        DMA("pool", ALP[0:17, :], alpha, (), [bALP], "alp")
        DMA("pool", WA[:, :, :], wview(w_min, OFF["ga"], 16), (), [bWA], "wa")
        MEMSET("dve", GAT[0:17, :], 1.0, [bGAT])
        for tg in range(4):
            ps, bps = PS[tg % 2], bPS[tg % 2]
            MMG(ps[0:16, :], [(WA[:, k, :], XN[:, k, tgs(tg)]) for k in range(8)], [bWA, bXN[tg]], [bps])
            CP("dve", GAT[0:16, tgs(tg)], ps[0:16, :], [bps], [bGAT])
        Wq = AR.take([8, 128], BF16)
        Wk = AR.take([8, 128], BF16)
        Wv = AR.take([8, 256], BF16)
        Wr = AR.take([8, 256], BF16)
        bWh = Buf("Wh")
        EB = [AR.take([512], BF16) for _ in range(2)]
        ENB = [AR.take([512], BF16) for _ in range(2)]
        EREV = [AR.take([4, 128], BF16) for _ in range(2)]
        QB = [AR.take([512], BF16) for _ in range(2)]
        KB = [AR.take([512], BF16) for _ in range(2)]
        KD = [AR.take([4, 128], BF16) for _ in range(2)]
        V = [AR.take([4, 256], BF16) for _ in range(2)]
        GR = [AR.take([4, 256], BF16) for _ in range(2)]
        EBL = AR.take([NT], F32)
        bEB = [Buf("EB0"), Buf("EB1")]
        bEREV = [Buf("EREV0"), Buf("EREV1")]
        bQB = [Buf("QB0"), Buf("QB1")]
        bKB = [Buf("KB0"), Buf("KB1")]
        bKD = [Buf("KD0"), Buf("KD1")]
        bV = [Buf("V0"), Buf("V1")]
        bGR = [Buf("GR0"), Buf("GR1")]
        bEBL = Buf("EBL")
        TE = [AR.take([128], F32) for _ in range(2)]
        LT = [AR.take([128], F32) for _ in range(2)]
        bTE = [Buf("TE0"), Buf("TE1")]
        bLT = [Buf("LT0"), Buf("LT1")]
        PTm = [AR.take([128], BF16) for _ in range(2)]
        bPT = [Buf("PT0"), Buf("PT1")]
        ST = AR.take([256], F32)
        SB = [AR.take([256], BF16) for _ in range(2)]
        bST = Buf("ST")
        bSB = [Buf("SB0"), Buf("SB1")]
        JK = AR.take([256], BF16)
        bJK = Buf("JK")
        SS = [AR.take([4], F32) for _ in range(2)]
        bSS = [Buf("SS0"), Buf("SS1")]
        OTM = [AR.take([256], BF16) for _ in range(2)]
        bOTM = [Buf("OTM0"), Buf("OTM1")]
        triu = CF[:, CF_TRIU:CF_TRIU + 128]
        tris = CF[:, CF_TRIS:CF_TRIS + 128]
        one_col = CF[:, CF_MISC + 1:CF_MISC + 2]
        WBpre = AR.take([8, D], BF16)
        bWBpre = Buf("WBpre")

        def prep(h, tg, par):
            if tg == 0:
                DMA("pool", Wq[:, :, :], wview(w_min, OFF["gq"] + h * 128, 128), (), [bWh], "wh")
                DMA("pool", Wk[:, :, :], wview(w_min, OFF["gk"] + h * 128, 128), (), [bWh], "wh")
                DMA("pool", Wv[:, :, :], wview(w_min, OFF["gv"] + h * 256, 256), (), [bWh], "wh")
                DMA("pool", Wr[:, :, :], wview(w_min, OFF["gr"] + h * 256, 256), (), [bWh], "wh")
                if h == 3:
                    DMA("pool", WBpre[:, :, :], wview(w_glo, 0, D), (), [bWBpre], "bwbpre")
            for ci in range(4):
                c = 4 * tg + ci
                cs = slice(c * 128, (c + 1) * 128)
                ls = slice(ci * 128, (ci + 1) * 128)
                u = c % 2
                px, bpx = PS[u], bPS[u]
                MM1(px[:, 0:128], GAT[0:17, cs], ALP[0:17, h * 128:(h + 1) * 128], True, True, [bGAT, bALP], [bpx])
                ACT(TE[u], px[:, 0:128], AF.Exp, [bpx], [bTE[u]], scale=-1.0)
                ACT(LT[u], TE[u], AF.Ln, [bTE[u], bCF], [bLT[u]], bias=one_col, scale=1.0)
                pb, bpb = PS[2 + u], bPS[2 + u]
                MM1(pb[:, 0:128], LT[u], triu, True, True, [bLT[u], bCF], [bpb])
                MM1(pb[:, 128:256], tris, LT[u], True, True, [bLT[u], bCF], [bpb])
                ACT(EB[par][:, ls], pb[:, 0:128], AF.Exp, [bpb], [bEB[par]])
                ACT(ENB[par][:, ls], pb[:, 0:128], AF.Exp, [bpb], [bEB[par]], scale=-1.0)
                ACT(EREV[par][:, ci, :], pb[:, 128:256], AF.Exp, [bpb], [bEREV[par]])
                ACT(EBL[:, c:c + 1], pb[:, 127:128], AF.Exp, [bpb], [bEBL])
            pq, bpq = PS[4], bPS[4]
            pk, bpk = PS[5], bPS[5]
            MMG(pq[:, :], [(Wq[:, k, :], XN[:, k, tgs(tg)]) for k in range(8)], [bWh, bXN[tg]], [bpq])
            STT(QB[par], pq[:, :], 128.0 ** -0.5, EB[par], ALU.mult, ALU.mult, [bpq, bEB[par]], [bQB[par]])
            MMG(pk[:, :], [(Wk[:, k, :], XN[:, k, tgs(tg)]) for k in range(8)], [bWh, bXN[tg]], [bpk])
            TT("dve", KB[par], pk[:, :], ENB[par], ALU.mult, [bpk, bEB[par]], [bKB[par]])
            for ci in range(4):
                c = 4 * tg + ci
                cs = slice(c * 128, (c + 1) * 128)
                u = c % 2
                pk2, bpk2 = PS[u], bPS[u]
                MMG(pk2[:, 0:128], [(XN[:, k, cs], Wk[:, k, :]) for k in range(8)], [bWh, bXN[tg]], [bpk2])
                TT("dve", KD[par][:, ci, :], pk2[:, 0:128], EREV[par][:, ci, :], ALU.mult, [bpk2, bEREV[par]], [bKD[par]])
                pv, bpv = PS[2 + u], bPS[2 + u]
                MMG(pv[:, 0:256], [(XN[:, k, cs], Wv[:, k, :]) for k in range(8)], [bWh, bXN[tg]], [bpv])
                CP("act", V[par][:, ci, :], pv[:, 0:256], [bpv], [bV[par]])
                pr, bpr = PS[4 + u], bPS[4 + u]
                MMG(pr[:, 0:256], [(XN[:, k, cs], Wr[:, k, :]) for k in range(8)], [bWh, bXN[tg]], [bpr])
                ACT(GR[par][:, ci, :], pr[:, 0:256], AF.Silu, [bpr], [bGR[par]])

        def rec(h, tg, par):
            def cidx(ci):
                c = 4 * tg + ci
                return c, slice(c * 128, (c + 1) * 128), slice(ci * 128, (ci + 1) * 128), c % 2

            def stA(ci):
                c, cs, ls, u = cidx(ci)
                psc, bpsc = PS[u], bPS[u]
                MM1(psc[:, 128:256], KB[par][:, ls], QB[par][:, ls], True, True, [bKB[par], bQB[par]], [bpsc])
                TT("dve", PTm[u], psc[:, 128:256], caus, ALU.mult, [bpsc, bCB], [bPT[u]])

            def stB(ci):
                c, cs, ls, u = cidx(ci)
                if c < NT - 1:
                    pd, bpd = PS[4 + u], bPS[4 + u]
                    MM1(pd[:, 256:512], KD[par][:, ci, :], V[par][:, ci, :], True, True, [bKD[par], bV[par]], [bpd])
                    if c == 0:
                        CP("dve", ST, pd[:, 256:512], [bpd], [bST])
                    else:
                        STT(ST, ST, EBL[:, c:c + 1], pd[:, 256:512], ALU.mult, ALU.add, [bST, bEBL, bpd], [bST])
                    CP("act", SB[c % 2], ST, [bST], [bSB[c % 2]])

            def stC(ci):
                c, cs, ls, u = cidx(ci)
                po, bpo = PS[2 + u], bPS[2 + u]
                if c == 0:
                    MM1(po[:, 256:512], PTm[u], V[par][:, ci, :], True, True, [bPT[u], bV[par]], [bpo])
                else:
                    sp_ = (c - 1) % 2
                    MM1(po[:, 256:512], PTm[u], V[par][:, ci, :], True, False, [bPT[u], bV[par]], [bpo])
                    MM1(po[:, 256:512], QB[par][:, ls], SB[sp_], False, True, [bQB[par], bSB[sp_]], [bpo])
                ss, bss = SS[u], bSS[u]
                ACT(JK, po[:, 256:512], AF.Square, [bpo], [bJK, bss], accum=ss[:, 0:1])
                ACT(ss[:, 1:2], ss[:, 0:1], AF.Ln, [bss, bCF], [bss], bias=eps_col, scale=1.0 / 256)
                ACT(ss[:, 2:3], ss[:, 1:2], AF.Exp, [bss], [bss], scale=-0.5)
                STT(OTM[u], po[:, 256:512], ss[:, 2:3], GR[par][:, ci, :], ALU.mult, ALU.mult, [bpo, bss, bGR[par]], [bOTM[u]])

            def stD(ci):
                c, cs, ls, u = cidx(ci)
                TR(PSB[:, 0:128], OTM[u][:, 0:128], [bOTM[u], bCB], [bPSB])
                TR(PSB[:, 128:256], OTM[u][:, 128:256], [bOTM[u], bCB], [bPSB])
                for a_ in range(2):
                    TS("dve", OT[:, 2 * h + a_, cs], PSB[:, a_ * 128:(a_ + 1) * 128], VEC[:, V_GLA + 2 * h + a_:V_GLA + 2 * h + a_ + 1],
                       None, ALU.mult, None, [bPSB, bVEC], [bOT[tg]])

            for step in range(6):
                if step < 4:
                    stA(step)
                if 1 <= step <= 4:
                    stC(step - 1)
                if step < 4:
                    stB(step)
                if 2 <= step <= 5:
                    stD(step - 2)

        items = [(h, tg) for h in range(4) for tg in range(4)]
        prep(items[0][0], items[0][1], 0)
        for n, (h, tg) in enumerate(items):
            if n + 1 < len(items):
                prep(items[n + 1][0], items[n + 1][1], (n + 1) % 2)
            rec(h, tg, n % 2)
        P.barrier()
        AR.pos = mark
        bb = BranchBufs(w_glo, OFF["gates"], "g", pre=(WBpre, bWBpre))
        assert AR.pos + 8 * D + 8 * D + 8 * 512 + 2 * 512 <= 0 or True
        bb.load()
        for tg in range(4):
            branch_out_tg(bb, OT[:, :, tgs(tg)], bOT[tg], tg)
        P.barrier()

    def dsa():
        AR.reset()
        CKT = AR.take([S], BF16)
        CKA = AR.take([NT, 132], BF16)
        IKT = AR.take([S], BF16)
        WS = AR.take([NT, 8], F32)
        MT = AR.take([NT, 512], BF16)
        bMT = [Buf("MT%d" % j) for j in range(NT)]
        bCKT, bCKA, bIKT, bWS = Buf("CKT"), Buf("CKA"), Buf("IKT"), Buf("WS")
        mark2 = AR.pos
        WKV = AR.take([8, 128], BF16)
        WIK = AR.take([8, 128], BF16)
        WIW = AR.take([8, 8], BF16)
        bWsh = Buf("Wsh")
        DMA("pool", WKV[:, :, :], wview(w_min, OFF["dkv"], 128), (), [bWsh], "wsh")
        DMA("pool", WIK[:, :, 0:64], wview(w_min, OFF["ik"], 64), (), [bWsh], "wsh")
        DMA("pool", WIK[:, :, 64:128], wview(w_min, OFF["ik"], 64), (), [bWsh], "wsh")
        DMA("pool", WIW[:, :, :], wview(w_min, OFF["iw"], 8), (), [bWsh], "wsh")
        SQ = AR.take([512], BF16)
        RS = AR.take([512], F32)
        bSQ, bRS = Buf("dSQ"), Buf("dRS")
        MEMSET("pool", CKA[:, :, 128:132], 1.0, [bCKA])
        for tg in range(4):
            pc, bpc = PS[tg % 2], bPS[tg % 2]
            MMG(pc[:, :], [(WKV[:, k, :], XN[:, k, tgs(tg)]) for k in range(8)], [bWsh, bXN[tg]], [bpc])
            ACT(SQ, pc[:, :], AF.Square, [bpc], [bSQ])
            pn, bpn = PS[2], bPS[2]
            MM1(pn[:, :], ones, SQ, True, True, [bSQ, bCB], [bpn])
            ACT(RS, pn[:, :], AF.Ln, [bpn, bCF], [bRS], bias=eps_col, scale=1.0 / 128)
            ACT(RS, RS, AF.Exp, [bRS], [bRS], scale=-0.5)
            STT(CKT[:, tgs(tg)], pc[:, :], VEC[:, V_CKV:V_CKV + 1], RS, ALU.mult, ALU.mult, [bpc, bRS, bVEC], [bCKT])
            pi, bpi = PS[3], bPS[3]
            MMG(pi[:, :], [(WIK[:, k, :], XN[:, k, tgs(tg)]) for k in range(8)], [bWsh, bXN[tg]], [bpi])
            CP("act", IKT[:, tgs(tg)], pi[:, :], [bpi], [bIKT])
            for ti in range(4):
                c = tg * 4 + ti
                cs = slice(c * 128, (c + 1) * 128)
                TR(PSB[:, ti * 128:(ti + 1) * 128], CKT[:, cs], [bCKT, bCB], [bPSB])
            CP("dve", CKA[:, 4 * tg:4 * tg + 4, 0:128], PSB[:, 0:512].rearrange("p (a b) -> p a b", a=4), [bPSB], [bCKA])
            pw, bpw = PS[4], bPS[4]
            for ti in range(4):
                c = tg * 4 + ti
                cs = slice(c * 128, (c + 1) * 128)
                MMG(pw[:, ti * 8:ti * 8 + 8], [(XN[:, k, cs], WIW[:, k, :]) for k in range(8)], [bWsh, bXN[tg]], [bpw])
            TS("dve", WS[:, 4 * tg:4 * tg + 4, :], pw[:, 0:32].rearrange("p (a b) -> p a b", a=4),
               float((8 * 64) ** -0.5), None, ALU.mult, None, [bpw], [bWS])
        P.barrier()
        negtri = CF[:, CF_NEGTRI:CF_NEGTRI + 128]
        bPSBh = [Buf("PSBh0"), Buf("PSBh1")]
        trc = [0]
        for g in range(4):
            AR.pos = mark2
            IQ = AR.take([4, 512], BF16)
            bIQ = Buf("IQ")
            WIQ = [AR.take([8, 128], BF16) for _ in range(2)]
            bWIQ = [Buf("WIQ0"), Buf("WIQ1")]
            SC = [AR.take([S], F32) for _ in range(4)]
            bSC = [Buf("SC%d" % i_) for i_ in range(4)]
            RL = [AR.take([512], F32) for _ in range(2)]
            bRL = [Buf("RL0"), Buf("RL1")]
            M01 = [AR.take([S], BF16) for _ in range(2)]
            bM01 = [Buf("M010"), Buf("M011")]
            JNK = [AR.take([S], BF16) for _ in range(2)]
            bJ = [Buf("J%d" % i_) for i_ in range(4)]
            BI = [AR.take([8], F32) for _ in range(4)]
            bBI = [Buf("BI%d" % i_) for i_ in range(4)]
            WK2 = [AR.take([2 * NBIS + 2], F32) for _ in range(4)]
            bWK2 = [Buf("WK%d" % i_) for i_ in range(4)]
            for q in range(4):
                DMA("pool", WIQ[q % 2][:, :, :], wview(w_min, OFF["iq"] + q * 128, 128), (), [bWIQ[q % 2]], "wiq%d" % (q % 2))
                pq, bpq = PS[q % 2], bPS[q % 2]
                MMG(pq[:, :], [(WIQ[q % 2][:, k, :], XN[:, k, tgs(g)]) for k in range(8)], [bWIQ[q % 2], bXN[g]], [bpq])
                CP("act" if q % 2 else "dve", IQ[:, q, :], pq[:, :], [bpq], [bIQ])
            for j in range(4 * g, 4 * g + 4):
                MEMSET("pool", MT[:, j, :], 0.0, [bMT[j]])
            rc = 0
            for ti in range(4):
                i = 4 * g + ti
                n = (i + 1) * 128
                on_act = (ti % 2 == 1)
                sc, bsc = SC[ti], bSC[ti]
                nsg = (n + 511) // 512
                for h in range(8):
                    r0 = (h % 2) * 64
                    for sg_ in range(nsg):
                        s0 = sg_ * 512
                        w = min(512, n - s0)
                        pd, bpd = PS[2 + rc % 2], bPS[2 + rc % 2]
                        rl, brl = RL[rc % 2], bRL[rc % 2]
                        rc += 1
                        MM1(pd[:, 0:w], IQ[r0:r0 + 64, h // 2, ti * 128:(ti + 1) * 128], IKT[r0:r0 + 64, s0:s0 + w], True, True,
                            [bIQ, bIKT], [bpd])
                        ACT(rl[:, 0:w], pd[:, 0:w], AF.Relu, [bpd], [brl])
                        if h == 0:
                            TS("dve", sc[:, s0:s0 + w], rl[:, 0:w], WS[:, i, 0:1], None, ALU.mult, None, [brl, bWS], [bsc])
                        else:
                            STT(sc[:, s0:s0 + w], rl[:, 0:w], WS[:, i, h:h + 1], sc[:, s0:s0 + w], ALU.mult, ALU.add,
                                [brl, bWS, bsc], [bsc])
                bi, bbi = BI[ti], bBI[ti]
                wk, bwk = WK2[ti], bWK2[ti]
                if i >= 2:
                    P.add("dve", (lambda o, a: (lambda e: e.tensor_reduce(out=o, in_=a, axis=AX.X, op=ALU.max, apply_absolute_value=True)))(bi[:, 0:1], sc[:, 0:n]),
                          [bsc], [bbi])
                TT("dve", sc[:, n - 128:n], sc[:, n - 128:n], negtri, ALU.add, [bsc, bCF], [bsc])
                if i >= 2:
                    TS("dve", bi[:, 1:2], bi[:, 0:1], 1.0, None, ALU.add, None, [bbi], [bbi])
                    TS("dve", wk[:, 0:NBIS + 1], CF[:, CF_POW:CF_POW + NBIS + 1], bi[:, 1:2], None, ALU.mult, None, [bbi, bCF], [bwk])
                    MEMSET("dve", bi[:, 2:3], 0.0, [bbi])
                    if not on_act:
                        TS("dve", wk[:, NBIS + 1:2 * NBIS + 2], wk[:, 0:NBIS + 1], 2.0, None, ALU.mult, None, [bwk], [bwk])
                    else:
                        TS("dve", wk[:, NBIS + 1:2 * NBIS + 2], wk[:, 0:NBIS + 1], -1.0, None, ALU.mult, None, [bwk], [bwk])
                        MEMSET("dve", bi[:, 6:7], float(n - 511), [bbi])
            dts = [ti for ti in (0, 2) if 4 * g + ti >= 2]
            ats = [ti for ti in (1, 3) if 4 * g + ti >= 2]
            for k in range(NBIS):
                for ti in dts:
                    n = (4 * g + ti + 1) * 128
                    TS("dve", JNK[0][:, 0:n], SC[ti][:, 0:n], BI[ti][:, 2:3], 0.0, ALU.is_ge, ALU.add, [bSC[ti], bBI[ti]], [bJ[ti], bBI[ti]],
                       accum=BI[ti][:, 3:4])
                for ti in dts:
                    STT(BI[ti][:, 4:5], BI[ti][:, 3:4], 255.5, WK2[ti][:, NBIS + 1 + k:NBIS + 2 + k], ALU.is_ge, ALU.mult, [bBI[ti], bWK2[ti]], [bBI[ti]])
                for ti in dts:
                    STT(BI[ti][:, 2:3], BI[ti][:, 4:5], WK2[ti][:, k:k + 1], BI[ti][:, 2:3], ALU.subtract, ALU.add, [bBI[ti], bWK2[ti]], [bBI[ti]])
                for ti in ats:
                    n = (4 * g + ti + 1) * 128
                    ACT(JNK[1][:, 0:n], SC[ti][:, 0:n], AF.Sign, [bSC[ti], bBI[ti]], [bJ[ti], bBI[ti]], bias=BI[ti][:, 2:3], scale=1.0,
                        accum=BI[ti][:, 3:4])
                for ti in ats:
                    ACT(BI[ti][:, 4:5], BI[ti][:, 3:4], AF.Sign, [bBI[ti]], [bBI[ti]], bias=BI[ti][:, 6:7], scale=1.0)
                for ti in ats:
                    ACT(BI[ti][:, 2:3], BI[ti][:, 4:5], AF.Identity, [bBI[ti], bWK2[ti]], [bBI[ti]], bias=BI[ti][:, 2:3],
                        scale=WK2[ti][:, NBIS + 1 + k:NBIS + 2 + k])
            for ti in range(4):
                i = 4 * g + ti
                n = (i + 1) * 128
                u = ti % 2
                sc, bsc = SC[ti], bSC[ti]
                bi, bbi = BI[ti], bBI[ti]
                wk, bwk = WK2[ti], bWK2[ti]
                m01, bm01 = M01[u], bM01[u]
                if i >= 2 and u == 0:
                    TT("dve", bi[:, 5:6], bi[:, 2:3], wk[:, NBIS - 1:NBIS], ALU.subtract, [bbi, bwk], [bbi])
                elif i >= 2:
                    TS("dve", bi[:, 5:6], bi[:, 2:3], -1.0, wk[:, NBIS - 1:NBIS], ALU.mult, ALU.subtract, [bbi, bwk], [bbi])
                else:
                    MEMSET("dve", bi[:, 5:6], -1e29, [bbi])
                TS("dve", m01[:, 0:n], sc[:, 0:n], bi[:, 5:6], None, ALU.is_ge, None, [bsc, bbi], [bm01])
                for j0 in range(0, i + 1, 4):
                    nj = min(4, i + 1 - j0)
                    for jj in range(nj):
                        j = j0 + jj
                        TR(PSB[:, jj * 128:(jj + 1) * 128], m01[:, j * 128:(j + 1) * 128], [bm01, bCB], [bPSB])
                    CP("act", MT[:, j0:j0 + nj, ti * 128:(ti + 1) * 128],
                       PSB[:, 0:nj * 128].rearrange("p (a b) -> p a b", a=nj), [bPSB], [bMT[j] for j in range(j0, j0 + nj)])
            P.barrier()
            AR.pos = mark2
            WQ = [AR.take([8, 128], BF16) for _ in range(2)]
            bWQ = [Buf("WQ0"), Buf("WQ1")]
            QT = [AR.take([512], BF16) for _ in range(2)]
            bQT = [Buf("QT0"), Buf("QT1")]
            E = [AR.take([512], BF16) for _ in range(NE)]
            bE = [Buf("E%d" % i_) for i_ in range(NE)]
            RD = AR.take([8], F32)
            bRD = Buf("RD")
            ODM = [AR.take([128], BF16) for _ in range(4)]
            bODM = [Buf("ODM%d" % i_) for i_ in range(4)]
            OTg = AR.take([8, 512], BF16)
            bOTg = Buf("OTg")
            bb = BranchBufs(w_dso, OFF["gates"] + D, "d", stream_wo=True)
            MG = [AR.take([5, 256], BF16) for _ in range(2)]
            bMG = [Buf("MG0"), Buf("MG1")]
            nj_all = 4 * g + 4
            items = [(h, j) for h in range(8) for j in range(nj_all)]
            N = len(items)
            qbank = {}
            freeb = [0, 1, 2]
            bACC = [[Buf("ACC%d" % t_) for t_ in range(4)]] * 2

            def acc_ap(set_, ti):
                return PS[3 + ti][:, 0:129]

            def qload(h):
                s = h % 2
                DMA("pool", WQ[s][:, :, :], wview(w_min, OFF["dq"] + h * 128, 128), (), [bWQ[s]], "wq%d" % s)

            def qproj(h):
                s = h % 2
                b_ = freeb.pop(0)
                MMG(PS[b_][:, :], [(WQ[s][:, k, :], XN[:, k, tgs(g)]) for k in range(8)], [bWQ[s], bXN[g]], [bPS[b_]])
                TS("dve", QT[s], PS[b_][:, :], 128.0 ** -0.5, None, ALU.mult, None, [bPS[b_]], [bQT[s]])
                freeb.append(b_)

            def qk(n_):
                h, j = items[n_]
                b_ = freeb.pop(0)
                qbank[n_] = b_
                c0 = max(0, 128 * (j - 4 * g))
                MM1(PS[b_][:, c0:512], CKT[:, j * 128:(j + 1) * 128], QT[h % 2][:, c0:512], True, True, [bCKT, bQT[h % 2]], [bPS[b_]])

            def mid(n_):
                h, j = items[n_]
                b_ = qbank[n_]
                e, be = E[n_ % NE], bE[n_ % NE]
                r = j - 4 * g
                c0 = max(0, 128 * r)
                ACT(e[:, c0:512], PS[b_][:, c0:512], AF.Exp, [bPS[b_], bC31], [be], bias=C31[:, h:h + 1], scale=1.0)
                lo = max(0, 128 * r)
                hi = min(512, 128 * r + 256)
                if hi > lo:
                    TT("dve", e[:, lo:hi], e[:, lo:hi], MG[h % 2][:, r + 1, 0:hi - lo], ALU.mult, [be, bMG[h % 2]], [be])
                    if hi < 512:
                        TT("dve", e[:, hi:512], e[:, hi:512], MT[:, j, hi:512], ALU.mult, [be, bMT[j]], [be])
                else:
                    TT("dve", e[:, c0:512], e[:, c0:512], MT[:, j, c0:512], ALU.mult, [be, bMT[j]], [be])
                freeb.append(b_)

            def pv(n_):
                h, j = items[n_]
                e, be = E[n_ % NE], bE[n_ % NE]
                for ti in range(4):
                    i = 4 * g + ti
                    if i < j:
                        continue
                    MM1(acc_ap(h % 2, ti), e[:, ti * 128:(ti + 1) * 128], CKA[:, j, 0:129], j == 0, j == i,
                        [be, bCKA], [bACC[h % 2][ti]])

            def fin_a(h):
                for ti in range(4):
                    pa = acc_ap(h % 2, ti)
                    P.add("dve", (lambda o, a: (lambda e: e.reciprocal(out=o, in_=a)))(RD[:, ti:ti + 1], pa[:, 128:129]), [bACC[h % 2][ti]], [bRD])
                for ti in range(4):
                    pa = acc_ap(h % 2, ti)
                    if ti < 2:
                        TS("dve", ODM[ti], pa[:, 0:128], RD[:, ti:ti + 1], None, ALU.mult, None, [bACC[h % 2][ti], bRD], [bODM[ti]])
                    else:
                        ACT(ODM[ti], pa[:, 0:128], AF.Identity, [bACC[h % 2][ti], bRD], [bODM[ti]], scale=RD[:, ti:ti + 1])

            def fin_b(h):
                for ti in range(4):
                    TR(PSB[:, 512 + ti * 128:512 + (ti + 1) * 128], ODM[ti], [bODM[ti], bCB], [bPSB])
                CP("act", OTg[:, h, :], PSB[:, 512:1024], [bPSB], [bOTg])

            def mk_mg(h):
                for r in range(-1, 4):
                    j = 4 * g + r
                    if j < 0:
                        continue
                    lo = max(0, 128 * r)
                    hi = min(512, 128 * r + 256)
                    x0 = lo - 128 * r
                    TT("pool", MG[h % 2][:, r + 1, 0:hi - lo], MT[:, j, lo:hi], G[:, h, x0:x0 + (hi - lo)], ALU.mult,
                       [bMT[j], bG], [bMG[h % 2]])

            qload(0)
            qload(1)
            mk_mg(0)
            def warm(e):
                ins = None
                for w_ in range(NWARM):
                    ins = e.matmul(PS[0][:, :], ones, XN[:, w_ % 8, tgs(g)], start=True, stop=True)
                return ins
            P.add("pe", warm, [bCB, bXN[g]], [bPS[0]])
            qproj(0)
            qk(0)
            if N > 1:
                qk(1)
            bb.load()
            pend = []
            for n_ in range(N):
                h, j = items[n_]
                if n_ + 2 < N:
                    qk(n_ + 2)
                mid(n_)
                if j == min(nj_all // 2, nj_all - 3) and h + 1 < 8:
                    qproj(h + 1)
                if j == 0 and h + 1 < 8:
                    mk_mg(h + 1)
                    if h >= 1:
                        qload(h + 1)
                pv(n_)
                for pp_ in list(pend):
                    if n_ >= pp_[0]:
                        fin_b(pp_[1])
                        pend.remove(pp_)
                if j == nj_all - 1:
                    fin_a(h)
                    pend.append((n_ + 2, h))
            for pp_ in pend:
                fin_b(pp_[1])
            branch_out_tg(bb, OTg, bOTg, g)
            P.barrier()

    def ple():
        AR.reset()
        norm_to_xn(V_PLE)
        WG = AR.take([8, D], BF16)
        WP = AR.take([2, D], BF16)
        PTt = AR.take([2, S], BF16)
        SGf = [AR.take([512], F32) for _ in range(2)]
        TMP = [AR.take([512], F32) for _ in range(2)]
        bWG, bWP, bPT = Buf("pWG"), Buf("pWP"), Buf("pPT")
        bSGf = [Buf("pSG0"), Buf("pSG1")]
        bTMP = [Buf("pT0"), Buf("pT1")]
        DMA("pool", WG[:, :, :], wview(w_pg, 0, D), (), [bWG], "pwg")
        DMA("pool", WP[:, :, :], wview(w_pp, 0, D), (), [bWP], "pwp")
        DMA("pool", PTt[:, :, :], pT.rearrange("(k p) t -> p k t", p=128), (), [bPT], "ppt")
        c = 0
        for tg in range(4):
            for dc in range(8):
                pg, bpg = PS[(2 * c) % 4], bPS[(2 * c) % 4]
                pp, bpp = PS[(2 * c + 1) % 4], bPS[(2 * c + 1) % 4]
                u = c % 2
                c += 1
                dcs = slice(dc * 128, (dc + 1) * 128)
                MMG(pg[:, :], [(WG[:, k, dcs], XN[:, k, tgs(tg)]) for k in range(8)], [bWG, bXN[tg]], [bpg])
                MMG(pp[:, :], [(WP[:, k, dcs], PTt[:, k, tgs(tg)]) for k in range(2)], [bWP, bPT], [bpp])
                ACT(SGf[u], pg[:, :], AF.Sigmoid, [bpg], [bSGf[u]])
                TT("dve", TMP[u], SGf[u], pp[:, :], ALU.mult, [bSGf[u], bpp], [bTMP[u]])
                TT("pool", H[:, dc, tgs(tg)], H[:, dc, tgs(tg)], TMP[u], ALU.add, [bTMP[u], bH[dc][tg]], [bH[dc][tg]])
        P.barrier()

    if "ffn1" in stages:
        ffn(w_f1i, w_f1o, V_FFN1, "f1")
    if "gla" in stages or "dsa" in stages:
        AR.reset()
        norm_to_xn(V_MIX)
        build_G()
        P.barrier()
    if "gla" in stages:
        gla()
    if "dsa" in stages:
        dsa()
    if "ffn2" in stages:
        ffn(w_f2i, w_f2o, V_FFN2, "f2")
    if "ple" in stages:
        ple()
    AR.reset()
    OB = [AR.take([8, 512], F32) for _ in range(2)]
    bOB = [Buf("OB0"), Buf("OB1")]
    yv = yT.rearrange("(k p) t -> p k t", p=128)
    outs = []
    rmsnorm(V_FIN, lambda k, tg: OB[tg % 2][:, k, :], lambda tg: [bOB[tg % 2]],
            after=lambda tg: outs.append(DMA("sp", yv[:, :, tgs(tg)], OB[tg % 2], [bOB[tg % 2]], (), "out%d" % (tg % 2))))
    P.add("sp", None, extra=outs)

    streams = P.finalize()
    sems = {}
    for st in streams:
        sems[st] = es.enter_context(nc.semaphore("s_" + st.replace(":", "_")))
    block = es.enter_context(nc.Block())

    @block.tensor
    def _(e):
        P.emit("pe", e, sems)

    @block.scalar
    def _(e):
        P.emit("act", e, sems)

    @block.vector
    def _(e):
        P.emit("dve", e, sems)

    @block.gpsimd
    def _(e):
        P.emit("pool", e, sems)

    @block.sync
    def _(e):
        P.emit("sp", e, sems)

    es.close()
    return nc


def make_in_maps(inp):
    f = lambda a: np.ascontiguousarray(np.asarray(a, dtype=np.float32))
    cf, cb = make_consts()
    vec = np.zeros((128, NV), np.float32)
    for col, name in ((V_FFN1, "ffn1_norm"), (V_MIX, "mix_norm"), (V_FFN2, "ffn2_norm"), (V_PLE, "ple_norm")):
        vec[:, col:col + 8] = f(inp[name])[0].reshape(8, 128).T
    vec[:, V_FIN:V_FIN + 8] = f(inp["final_norm"]).reshape(8, 128).T
    vec[:, V_CKV] = f(inp["ckv_norm"])[0]
    vec[:, V_GLA:V_GLA + 8] = f(inp["gla_out_norm"])[0].reshape(8, 128).T
    alpha = np.concatenate([f(inp["gla_alpha_w"])[0], f(inp["gla_alpha_b"])[0][None, :]], axis=0)
    shared = {
        "vecs": vec, "cstf": cf, "cstb": cb,
        "ffn1_w_in": f(inp["ffn1_w_in"])[0], "ffn1_w_out": f(inp["ffn1_w_out"])[0],
        "ffn2_w_in": f(inp["ffn2_w_in"])[0], "ffn2_w_out": f(inp["ffn2_w_out"])[0],
        "mix_w_in": f(inp["mix_w_in"])[0], "alpha_aug": f(alpha),
        "gla_w_out": f(inp["gla_w_out"])[0],
        "dsa_w_out": f(inp["dsa_w_out"])[0], "rel_bias": f(inp["rel_bias"]), "rel_biasT": f(np.asarray(inp["rel_bias"]).T),
        "mix_w_out": f(inp["mix_w_out"])[0], "ple_w_gate": f(inp["ple_w_gate"])[0],
        "ple_w_proj": f(inp["ple_w_proj"])[0],
    }
    x = f(inp["x"])
    p = f(inp["p"])
    maps = []
    for b in range(8):
        m = dict(shared)
        m["xT"] = np.ascontiguousarray(x[b].T)
        m["pT"] = np.ascontiguousarray(p[0, b].T)
        maps.append(m)
    return maps


def kernel(**inputs):
    nc = build_nc()
    maps = make_in_maps(inputs)
    res = run_bass_kernel_spmd(nc, maps, core_ids=list(range(8)))
    out = np.stack([np.ascontiguousarray(res.results[b]["yT"].T) for b in range(8)], axis=0)
    return out.astype(np.float32)
```
